# Optimizing a Trainium2 kernel written in Bass

```python
import jax
import jax.numpy as jnp
from jax import lax
import numpy as np

D_MODEL = 2048
BATCH = 2
SEQ = 4096
DEPTH = 1
DEC_BATCH = 32
DEC_SEQ = 8
PAST_LEN = 16384
PAGE_SIZE = 128

D_ATTN = D_MODEL // 2
D_RNN = D_MODEL - D_ATTN
N_HEADS = 8
HEAD_DIM = D_ATTN // N_HEADS
N_KV = 2
GQA = N_HEADS // N_KV
N_BRANCH = 3
CMP_BLOCK = 32
SEL_BLOCK = 64
SEL_PER_CMP = SEL_BLOCK // CMP_BLOCK
TOP_N = 16
WINDOW = 512
Q_BLOCK = 128
FORCE_SCORE = 1e4
RNN_BLOCKS = 16
RNN_BLOCK_DIM = D_RNN // RNN_BLOCKS
CONV_W = 4
LRU_C = 8.0
N_GROUPS = 4
EXP_PER_GROUP = 4
N_EXPERTS = N_GROUPS * EXP_PER_GROUP
TOP_K_IN_GROUP = 2
D_EXPERT = D_MODEL // 4
RMS_EPS = 1e-6
KV_W = N_KV * 2 * HEAD_DIM
GATE_W = N_HEADS * N_BRANCH
D_IN = D_ATTN + 3 * KV_W + GATE_W + 2 * D_RNN
SPLIT_OFFSETS = [D_ATTN, D_ATTN + KV_W, D_ATTN + 2 * KV_W, D_ATTN + 3 * KV_W,
                 D_ATTN + 3 * KV_W + GATE_W, D_ATTN + 3 * KV_W + GATE_W + D_RNN]

kernel_name = 'hymba_nsa_rglru_hmoe_step'


def rms_norm(x, g):
    xf = x.astype(jnp.float32)
    y = xf * lax.rsqrt(jnp.mean(xf * xf, axis=-1, keepdims=True) + RMS_EPS)
    return (y * g.astype(jnp.float32)).astype(x.dtype)


def alibi_slopes():
    h = jnp.arange(1, N_HEADS + 1, dtype=jnp.float32)
    return jnp.exp2(-8.0 * h / N_HEADS).reshape(N_KV, GQA)


def masked_softmax(s, mask):
    s = jnp.where(mask, s.astype(jnp.float32), -jnp.inf)
    m = jnp.max(s, axis=-1, keepdims=True)
    m = jnp.where(jnp.isfinite(m), m, 0.0)
    p = jnp.where(mask, jnp.exp(s - m), 0.0)
    return p / jnp.maximum(jnp.sum(p, axis=-1, keepdims=True), 1e-30)


def compress_blocks(kv, w_cmp):
    b, n = kv.shape[:2]
    blk = kv.reshape(b, n // CMP_BLOCK, CMP_BLOCK, N_KV, 2, HEAD_DIM)
    return jnp.einsum('bnjgcd,jc->bngcd', blk, w_cmp.astype(kv.dtype))


def nsa_block(q, gates, t_pos, cmp_kv, sel_fetch, n_sel, win_kv, win_pos, slopes):
    b, tq = q.shape[:2]
    f32 = jnp.float32
    qg = q.reshape(b, tq, N_KV, GQA, HEAD_DIM) * (HEAD_DIM ** -0.5)
    sl = slopes[None, None, :, :, None]
    nc = cmp_kv.shape[1]
    c_end = (jnp.arange(nc) + 1) * CMP_BLOCK - 1
    dist_c = t_pos[:, None] - c_end[None, :]
    s_c = jnp.einsum('btgrd,bngd->btgrn', qg, cmp_kv[..., 0, :]).astype(f32)
    s_c = s_c - sl * dist_c.astype(f32)[None, :, None, None, :]
    p_c = masked_softmax(s_c, (dist_c >= 0)[None, :, None, None, :])
    o_c = jnp.einsum('btgrn,bngd->btgrd', p_c.astype(cmp_kv.dtype), cmp_kv[..., 1, :])
    imp = jnp.pad(p_c.sum(axis=3), ((0, 0), (0, 0), (0, 0), (0, n_sel * SEL_PER_CMP - nc)))
    imp = imp.reshape(b, tq, N_KV, n_sel, SEL_PER_CMP).sum(-1)
    blk = jnp.arange(n_sel)[None, :]
    cur = (t_pos // SEL_BLOCK)[:, None]
    forced = (blk == 0) | (blk == cur) | (blk == cur - 1)
    imp = jnp.where(forced[None, :, None, :], FORCE_SCORE, imp)
    imp = jnp.where((blk > cur)[None, :, None, :], -jnp.inf, imp)
    k_top = min(TOP_N, n_sel)
    _, idx = lax.top_k(imp, k_top)
    kv_s = sel_fetch(idx).reshape(b, tq, N_KV, k_top * SEL_BLOCK, 2, HEAD_DIM)
    pos_s = (idx[..., None] * SEL_BLOCK + jnp.arange(SEL_BLOCK)).reshape(b, tq, N_KV, k_top * SEL_BLOCK)
    dist_s = t_pos[None, :, None, None] - pos_s
    s_s = jnp.einsum('btgrd,btgkd->btgrk', qg, kv_s[..., 0, :]).astype(f32)
    s_s = s_s - sl * dist_s.astype(f32)[:, :, :, None, :]
    p_s = masked_softmax(s_s, (dist_s >= 0)[:, :, :, None, :])
    o_s = jnp.einsum('btgrk,btgkd->btgrd', p_s.astype(kv_s.dtype), kv_s[..., 1, :])
    dist_w = t_pos[:, None] - win_pos[None, :]
    mask_w = (dist_w >= 0) & (dist_w <= WINDOW) & (win_pos >= 0)[None, :]
    s_w = jnp.einsum('btgrd,bkgd->btgrk', qg, win_kv[..., 0, :]).astype(f32)
    s_w = s_w - sl * dist_w.astype(f32)[None, :, None, None, :]
    p_w = masked_softmax(s_w, mask_w[None, :, None, None, :])
    o_w = jnp.einsum('btgrk,bkgd->btgrd', p_w.astype(win_kv.dtype), win_kv[..., 1, :])
    g = jax.nn.sigmoid(gates.astype(f32)).reshape(b, tq, N_KV, GQA, N_BRANCH)
    o = g[..., 0:1] * o_c + g[..., 1:2] * o_s + g[..., 2:3] * o_w
    return o.reshape(b, tq, D_ATTN).astype(q.dtype)


def nsa_prompt(q, gates, kv_c, kv_s, kv_w, w_cmp, slopes):
    b, s = q.shape[:2]
    cmp_kv = compress_blocks(kv_c, w_cmp)
    n_sel = s // SEL_BLOCK
    sel_blocks = kv_s.reshape(b, n_sel, SEL_BLOCK, N_KV, 2, HEAD_DIM)
    b_idx = jnp.arange(b)[:, None, None, None]
    g_idx = jnp.arange(N_KV)[None, None, :, None]

    def fetch(idx):
        return sel_blocks[b_idx, idx, :, g_idx]

    win_pad = jnp.pad(kv_w, ((0, 0), (WINDOW, 0), (0, 0), (0, 0), (0, 0)))
    nq = s // Q_BLOCK
    q_b = q.reshape(b, nq, Q_BLOCK, N_HEADS, HEAD_DIM).swapaxes(0, 1)
    g_b = gates.reshape(b, nq, Q_BLOCK, N_HEADS, N_BRANCH).swapaxes(0, 1)

    def one_block(args):
        i, q_i, g_i = args
        start = i * Q_BLOCK
        t_pos = start + jnp.arange(Q_BLOCK)
        win = lax.dynamic_slice_in_dim(win_pad, start, WINDOW + Q_BLOCK, axis=1)
        win_pos = start - WINDOW + jnp.arange(WINDOW + Q_BLOCK)
        return nsa_block(q_i, g_i, t_pos, cmp_kv, fetch, n_sel, win, win_pos, slopes)

    out = lax.map(one_block, (jnp.arange(nq), q_b, g_b))
    return out.swapaxes(0, 1).reshape(b, s, D_ATTN)


def nsa_sample(q, gates, kv_c, kv_s, kv_w, cache_c, cache_s, cache_w, page_table, w_cmp, slopes):
    b, t = q.shape[:2]
    past = page_table.shape[1] * PAGE_SIZE
    cmp_kv = compress_blocks(cache_c[page_table].reshape(b, past, N_KV, 2, HEAD_DIM), w_cmp)
    n_new_c = t // CMP_BLOCK
    if n_new_c > 0:
        cmp_kv = jnp.concatenate([cmp_kv, compress_blocks(kv_c[:, :n_new_c * CMP_BLOCK], w_cmp)], axis=1)
    n_sel = -(-(past + t) // SEL_BLOCK)
    n_past_sel = past // SEL_BLOCK
    n_new_sel = n_sel - n_past_sel
    sel_per_page = PAGE_SIZE // SEL_BLOCK
    pool_blocks = cache_s.reshape(-1, SEL_BLOCK, N_KV, 2, HEAD_DIM)
    new_blocks = jnp.pad(kv_s, ((0, 0), (0, n_new_sel * SEL_BLOCK - t), (0, 0), (0, 0), (0, 0)))
    new_blocks = new_blocks.reshape(b, n_new_sel, SEL_BLOCK, N_KV, 2, HEAD_DIM)
    b_idx = jnp.arange(b)[:, None, None, None]
    g_idx = jnp.arange(N_KV)[None, None, :, None]

    def fetch(idx):
        lp = jnp.minimum(idx, n_past_sel - 1)
        phys = page_table[b_idx, lp // sel_per_page] * sel_per_page + lp % sel_per_page
        from_pool = pool_blocks[phys, :, g_idx]
        from_new = new_blocks[b_idx, jnp.clip(idx - n_past_sel, 0, n_new_sel - 1), :, g_idx]
        return jnp.where((idx < n_past_sel)[..., None, None, None], from_pool, from_new)

    n_win = cache_w.shape[1]
    win = jnp.concatenate([cache_w, kv_w], axis=1)
    win_pos = past - n_win + jnp.arange(n_win + t)
    t_pos = past + jnp.arange(t)
    out = nsa_block(q, gates, t_pos, cmp_kv, fetch, n_sel, win, win_pos, slopes)
    return out, win[:, t:]


def causal_conv(x, buf, w, bias):
    t = x.shape[1]
    xp = jnp.concatenate([buf, x], axis=1)
    y = bias + xp[:, 0:t] * w[0]
    for k in range(1, CONV_W):
        y = y + xp[:, k:k + t] * w[k]
    return y, xp[:, t:]


def _lin_comb(e1, e2):
    a1, b1 = e1
    a2, b2 = e2
    return a1 * a2, a2 * b1 + b2


def rg_lru(x, h0, w_a, b_a, w_x, b_x, lam):
    b, t, c = x.shape
    f32 = jnp.float32
    xb = x.reshape(b, t, RNN_BLOCKS, RNN_BLOCK_DIM)
    r = jax.nn.sigmoid((jnp.einsum('btnd,nde->btne', xb, w_a).reshape(b, t, c) + b_a).astype(f32))
    i = jax.nn.sigmoid((jnp.einsum('btnd,nde->btne', xb, w_x).reshape(b, t, c) + b_x).astype(f32))
    log_a = -LRU_C * r * jax.nn.softplus(-lam.astype(f32))
    a = jnp.exp(log_a)
    u = jnp.sqrt(-jnp.expm1(2.0 * log_a)) * (i * x.astype(f32))
    u = u.at[:, 0].add(a[:, 0] * h0.astype(f32))
    _, h = lax.associative_scan(_lin_comb, (a, u), axis=1)
    return h.astype(x.dtype), h[:, -1].astype(x.dtype)


def recurrent_group(xr, xg, conv_buf, h0, conv_w, conv_b, w_a, b_a, w_x, b_x, lam):
    xc, new_buf = causal_conv(xr, conv_buf, conv_w, conv_b)
    h, h_last = rg_lru(xc, h0, w_a, b_a, w_x, b_x, lam)
    return h * jax.nn.gelu(xg), new_buf, h_last


def hier_moe(x, w_rg, b_rg, w_re, b_re, w_gate, w_up, w_down):
    n = x.shape[0]
    f32 = jnp.float32
    rows = jnp.arange(n)
    g_logit = (x @ w_rg).astype(f32) + b_rg.astype(f32)
    g_prob = jax.nn.softmax(g_logit, axis=-1)
    g_star = jnp.argmax(g_logit, axis=-1)
    e_logit = ((x @ w_re).astype(f32) + b_re.astype(f32)).reshape(n, N_GROUPS, EXP_PER_GROUP)
    e_prob = jax.nn.softmax(e_logit[rows, g_star], axis=-1)
    top_p, top_i = lax.top_k(e_prob, TOP_K_IN_GROUP)
    w = top_p / jnp.sum(top_p, axis=-1, keepdims=True) * g_prob[rows, g_star][:, None]
    e_idx = g_star[:, None] * EXP_PER_GROUP + top_i
    comb = jnp.einsum('nk,nke->ne', w, jax.nn.one_hot(e_idx, N_EXPERTS, dtype=f32))
    hg = jnp.einsum('nd,edf->nef', x, w_gate)
    hu = jnp.einsum('nd,edf->nef', x, w_up)
    h = jax.nn.silu(hg) * hu * comb[:, :, None].astype(x.dtype)
    return jnp.einsum('nef,efd->nd', h, w_down)


def project(x, norm_g, w_in):
    b, t, _ = x.shape
    z = rms_norm(x, norm_g) @ w_in
    q, kc, ks, kw, gt, xr, xg = jnp.split(z, SPLIT_OFFSETS, axis=-1)
    kv = lambda a: a.reshape(b, t, N_KV, 2, HEAD_DIM)
    return (q.reshape(b, t, N_HEADS, HEAD_DIM), kv(kc), kv(ks), kv(kw),
            gt.reshape(b, t, N_HEADS, N_BRANCH), xr, xg)


def finish(x, attn_o, rnn_o, w_out, norm_ffn, w_rg, b_rg, w_re, b_re, w_gate, w_up, w_down):
    h = x + jnp.concatenate([attn_o, rnn_o], axis=-1) @ w_out
    b, t, d = h.shape
    ffn = hier_moe(rms_norm(h, norm_ffn).reshape(b * t, d), w_rg, b_rg, w_re, b_re, w_gate, w_up, w_down)
    return h + ffn.reshape(b, t, d).astype(h.dtype)


def setup_inputs(seed: int = 0) -> dict:
    key = jax.random.key(seed)
    ks = jax.random.split(key, 32)
    nrm = jax.random.normal
    n_pages = PAST_LEN // PAGE_SIZE
    n_used = DEC_BATCH * n_pages
    n_pool = n_used + max(1, n_used // 4)
    n_win = min(WINDOW, PAST_LEN)
    page_table = jax.random.permutation(ks[7], n_pool)[:n_used].reshape(DEC_BATCH, n_pages).astype(jnp.int32)
    u = jax.random.uniform(ks[17], (DEPTH, D_RNN), minval=0.9, maxval=0.999)
    s = u ** (1.0 / LRU_C)
    return {
        'x_prompt': nrm(ks[0], (BATCH, SEQ, D_MODEL), jnp.float32),
        'x_sample': nrm(ks[1], (DEC_BATCH, DEC_SEQ, D_MODEL), jnp.float32),
        'cache_cmp_kv': nrm(ks[2], (DEPTH, n_pool, PAGE_SIZE, N_KV, 2, HEAD_DIM), jnp.float32),
        'cache_sel_kv': nrm(ks[3], (DEPTH, n_pool, PAGE_SIZE, N_KV, 2, HEAD_DIM), jnp.float32),
        'cache_win_kv': nrm(ks[4], (DEPTH, DEC_BATCH, n_win, N_KV, 2, HEAD_DIM), jnp.float32),
        'state_conv': nrm(ks[5], (DEPTH, DEC_BATCH, CONV_W - 1, D_RNN), jnp.float32),
        'state_h': 0.5 * nrm(ks[6], (DEPTH, DEC_BATCH, D_RNN), jnp.float32),
        'page_table': page_table,
        'norm_mix': 1.0 + 0.02 * nrm(ks[8], (DEPTH, D_MODEL), jnp.float32),
        'w_in': nrm(ks[9], (DEPTH, D_MODEL, D_IN), jnp.float32) * D_MODEL ** -0.5,
        'cmp_pool_w': (1.0 + 0.1 * nrm(ks[10], (DEPTH, CMP_BLOCK, 2), jnp.float32)) / CMP_BLOCK,
        'conv_w': nrm(ks[11], (DEPTH, CONV_W, D_RNN), jnp.float32) * CONV_W ** -0.5,
        'conv_b': 0.01 * nrm(ks[12], (DEPTH, D_RNN), jnp.float32),
        'lru_wa': nrm(ks[13], (DEPTH, RNN_BLOCKS, RNN_BLOCK_DIM, RNN_BLOCK_DIM), jnp.float32) * RNN_BLOCK_DIM ** -0.5,
        'lru_ba': 0.01 * nrm(ks[14], (DEPTH, D_RNN), jnp.float32),
        'lru_wx': nrm(ks[15], (DEPTH, RNN_BLOCKS, RNN_BLOCK_DIM, RNN_BLOCK_DIM), jnp.float32) * RNN_BLOCK_DIM ** -0.5,
        'lru_bx': 0.01 * nrm(ks[16], (DEPTH, D_RNN), jnp.float32),
        'lru_lambda': jnp.log(s) - jnp.log1p(-s),
        'w_out': nrm(ks[18], (DEPTH, D_MODEL, D_MODEL), jnp.float32) * D_MODEL ** -0.5,
        'norm_ffn': 1.0 + 0.02 * nrm(ks[19], (DEPTH, D_MODEL), jnp.float32),
        'w_router_group': nrm(ks[20], (DEPTH, D_MODEL, N_GROUPS), jnp.float32) * D_MODEL ** -0.5,
        'b_router_group': 0.01 * nrm(ks[21], (DEPTH, N_GROUPS), jnp.float32),
        'w_router_expert': nrm(ks[22], (DEPTH, D_MODEL, N_EXPERTS), jnp.float32) * D_MODEL ** -0.5,
        'b_router_expert': 0.01 * nrm(ks[23], (DEPTH, N_EXPERTS), jnp.float32),
        'w_exp_gate': nrm(ks[24], (DEPTH, N_EXPERTS, D_MODEL, D_EXPERT), jnp.float32) * D_MODEL ** -0.5,
        'w_exp_up': nrm(ks[25], (DEPTH, N_EXPERTS, D_MODEL, D_EXPERT), jnp.float32) * D_MODEL ** -0.5,
        'w_exp_down': nrm(ks[26], (DEPTH, N_EXPERTS, D_EXPERT, D_MODEL), jnp.float32) * D_EXPERT ** -0.5,
        'norm_final': 1.0 + 0.02 * nrm(ks[27], (D_MODEL,), jnp.float32),
    }


def reference(x_prompt, x_sample, cache_cmp_kv, cache_sel_kv, cache_win_kv, state_conv, state_h, page_table,
              norm_mix, w_in, cmp_pool_w, conv_w, conv_b, lru_wa, lru_ba, lru_wx, lru_bx, lru_lambda, w_out,
              norm_ffn, w_router_group, b_router_group, w_router_expert, b_router_expert,
              w_exp_gate, w_exp_up, w_exp_down, norm_final):
    slopes = alibi_slopes()
    hp, hs = x_prompt, x_sample
    bp = x_prompt.shape[0]
    l_cmp_p, l_cmp_s, l_sel_p, l_sel_s, l_win_p, l_win_s = [], [], [], [], [], []
    l_conv_p, l_conv_s, l_h_p, l_h_s = [], [], [], []
    for l in range(DEPTH):
        moe_w = (w_out[l], norm_ffn[l], w_router_group[l], b_router_group[l], w_router_expert[l],
                 b_router_expert[l], w_exp_gate[l], w_exp_up[l], w_exp_down[l])
        lru_w = (conv_w[l], conv_b[l], lru_wa[l], lru_ba[l], lru_wx[l], lru_bx[l], lru_lambda[l])
        q, kc, ks, kw, gt, xr, xg = project(hp, norm_mix[l], w_in[l])
        attn_p = nsa_prompt(q, gt, kc, ks, kw, cmp_pool_w[l], slopes)
        zbuf = jnp.zeros((bp, CONV_W - 1, D_RNN), xr.dtype)
        zh = jnp.zeros((bp, D_RNN), xr.dtype)
        rnn_p, conv_p, h_p = recurrent_group(xr, xg, zbuf, zh, *lru_w)
        hp = finish(hp, attn_p, rnn_p, *moe_w)
        l_cmp_p.append(kc)
        l_sel_p.append(ks)
        l_win_p.append(kw[:, -min(WINDOW, kw.shape[1]):])
        l_conv_p.append(conv_p)
        l_h_p.append(h_p)
        q, kc, ks, kw, gt, xr, xg = project(hs, norm_mix[l], w_in[l])
        attn_s, win_s = nsa_sample(q, gt, kc, ks, kw, cache_cmp_kv[l], cache_sel_kv[l], cache_win_kv[l],
                                   page_table, cmp_pool_w[l], slopes)
        rnn_s, conv_s, h_s = recurrent_group(xr, xg, state_conv[l], state_h[l], *lru_w)
        hs = finish(hs, attn_s, rnn_s, *moe_w)
        l_cmp_s.append(kc)
        l_sel_s.append(ks)
        l_win_s.append(win_s)
        l_conv_s.append(conv_s)
        l_h_s.append(h_s)
    y_prompt = rms_norm(hp, norm_final)
    y_sample = rms_norm(hs, norm_final)
    new_cmp_prompt = jnp.stack(l_cmp_p)
    new_cmp_sample = jnp.stack(l_cmp_s)
    new_sel_prompt = jnp.stack(l_sel_p)
    new_sel_sample = jnp.stack(l_sel_s)
    new_win_prompt = jnp.stack(l_win_p)
    new_win_sample = jnp.stack(l_win_s)
    new_conv_prompt = jnp.stack(l_conv_p)
    new_conv_sample = jnp.stack(l_conv_s)
    new_h_prompt = jnp.stack(l_h_p)
    new_h_sample = jnp.stack(l_h_s)
    return (y_prompt, y_sample, new_cmp_prompt, new_cmp_sample, new_sel_prompt, new_sel_sample,
            new_win_prompt, new_win_sample, new_conv_prompt, new_conv_sample, new_h_prompt, new_h_sample)
```

```python
import numpy as np
import concourse.bass as bass
import concourse.mybir as mybir
from concourse.bass_utils import run_bass_kernel_spmd
from contextlib import ExitStack

F32 = mybir.dt.float32
BF16 = mybir.dt.bfloat16
I32 = mybir.dt.int32
AF = mybir.ActivationFunctionType
ALU = mybir.AluOpType
AX = mybir.AxisListType

D = 2048
NQ = 1024
KVW = 512
D_IN = 4632
C_Q, C_KC, C_KS, C_KW, C_GT, C_XR, C_XG = 0, 1024, 1536, 2048, 2560, 2584, 3608
SEQ = 4096
EPS = 1e-6


class Buf:
    def __init__(self, name):
        self.name = name
        self.w = None
        self.r = {}


class V:
    def __init__(self, ap, buf):
        self.ap = ap
        self.buf = buf


class T:
    def __init__(self, t, name):
        self.t = t
        self.buf = Buf(name)

    def __getitem__(self, idx):
        return V(self.t[idx], self.buf)

    def v(self, ap):
        return V(ap, self.buf)


class Eng:
    def __init__(self, kb, name, e, sem):
        self.kb = kb
        self.name = name
        self.e = e
        self.sem = sem
        self.count = 0
        self.waited = {}
        self.pend_r = []
        self.pend_w = []

    def wait(self, tok):
        if tok is None:
            return
        sem, val, owner = tok
        if owner is self and self.name == "pe":
            return
        key = id(sem)
        if self.waited.get(key, 0) >= val:
            return
        self.e.wait_ge(sem, val)
        self.waited[key] = val


class KB:
    def __init__(self, nc, es):
        self.nc = nc
        self.es = es
        self.eng = {}
        for name, e in (("pe", nc.tensor), ("act", nc.scalar), ("dve", nc.vector), ("pool", nc.gpsimd), ("sp", nc.sync)):
            sem = es.enter_context(nc.semaphore("sem_" + name))
            self.eng[name] = Eng(self, name, e, sem)
        self.dma_sems = []
        for i in range(24):
            self.dma_sems.append([es.enter_context(nc.semaphore("dsem%d" % i)), 0, None])
        self.dma_rr = 0
        self.all_bufs = []
        self.stack = [es]
        self.ntile = 0

    def sb(self, name, shape, dt, glob=False):
        if glob or len(self.stack) == 1:
            t = self.stack[0].enter_context(self.nc.sbuf_tensor(name, shape, dt, side="right"))
        else:
            t = self.stack[-1].enter_context(self.nc.sbuf_tensor(name, shape, dt))
        T_ = T(t, name)
        self.all_bufs.append(T_.buf)
        return T_

    def phase_begin(self):
        es = ExitStack()
        es.__enter__()
        self.stack.append(es)

    def phase_end(self):
        self.barrier()
        es = self.stack.pop()
        es.__exit__(None, None, None)

    def barrier(self):
        toks = []
        for F in self.eng.values():
            if F.count > 0:
                toks.append((F.sem, F.count, F))
        for slot in self.dma_sems:
            if slot[2] is not None:
                toks.append(slot[2])
        for E in self.eng.values():
            for tok in toks:
                if tok[2] is E:
                    continue
                E.wait(tok)

    def ps(self, name, shape, dt=F32):
        t = self.es.enter_context(self.nc.psum_tensor(name, shape, dt))
        T_ = T(t, name)
        self.all_bufs.append(T_.buf)
        return T_

    def dram(self, name, shape, dt, kind="Internal"):
        t = self.nc.dram_tensor(name, shape, dt, kind=kind)
        T_ = T(t.ap(), name)
        self.all_bufs.append(T_.buf)
        return T_

    def _pre(self, E, reads, writes):
        for b in reads:
            E.wait(b.w)
        for b in writes:
            E.wait(b.w)
            for tok in b.r.values():
                E.wait(tok)

    def _post(self, tok, reads, writes):
        for b in reads:
            b.r[id(tok[0])] = tok
        for b in writes:
            b.w = tok
            b.r = {}

    def op(self, eng, fn, reads, writes, signal=True):
        E = self.eng[eng]
        rb = [v.buf for v in reads if v is not None]
        wb = [v.buf for v in writes if v is not None]
        self._pre(E, rb, wb)
        ins = fn(E.e)
        if signal:
            E.count += 1
            ins.then_inc(E.sem, 1)
            tok = (E.sem, E.count, E)
            self._post(tok, rb + E.pend_r, wb + E.pend_w)
            E.pend_r = []
            E.pend_w = []
        else:
            E.pend_r += rb
            E.pend_w += wb
        return ins

    def dma(self, q, out, in_, fn=None):
        E = self.eng[q]
        rb, wb = [in_.buf], [out.buf]
        self._pre(E, rb, wb)
        slot = self.dma_sems[self.dma_rr % len(self.dma_sems)]
        self.dma_rr += 1
        if slot[2] is not None:
            E.wait(slot[2])
        if fn is None:
            ins = E.e.dma_start(out=out.ap, in_=in_.ap)
        else:
            ins = fn(E.e)
        slot[1] += 16
        ins.then_inc(slot[0], 16)
        tok = (slot[0], slot[1], None)
        slot[2] = tok
        self._post(tok, rb, wb)
        return tok

    def finish(self):
        E = self.eng["sp"]
        for b in self.all_bufs:
            E.wait(b.w)
            for tok in b.r.values():
                E.wait(tok)
        for slot in self.dma_sems:
            if slot[2] is not None:
                E.wait(slot[2])

    def mm(self, out, lhsT, rhs, start=True, stop=True, signal=True):
        kw = {}
        try:
            out.ap.base_partition()
        except AssertionError:
            kw["tile_position"] = (0, 96)
        return self.op("pe", lambda e: e.matmul(out.ap, lhsT.ap, rhs.ap, start=start, stop=stop,
                                                 skip_group_check=True, **kw),
                       [lhsT, rhs], [out], signal=signal)

    def tr(self, out, in_, ident, signal=True):
        return self.op("pe", lambda e: e.transpose(out.ap, in_.ap, ident.ap), [in_, ident], [out], signal=signal)

    def act(self, out, in_, func, bias=None, scale=None, accum=None, eng="act"):
        kw = {}
        rd = [in_]
        if bias is not None:
            if isinstance(bias, V):
                kw["bias"] = bias.ap
                rd.append(bias)
            else:
                kw["bias"] = bias
        if scale is not None:
            if isinstance(scale, V):
                kw["scale"] = scale.ap
                rd.append(scale)
            else:
                kw["scale"] = scale
        wr = [out]
        if accum is not None:
            kw["accum_out"] = accum.ap
            wr.append(accum)
        return self.op(eng, lambda e: e.activation(out.ap, in_.ap, func, **kw), rd, wr)

    def copy(self, eng, out, in_):
        if eng == "act":
            return self.op("act", lambda e: e.copy(out.ap, in_.ap), [in_], [out])
        return self.op(eng, lambda e: e.tensor_copy(out=out.ap, in_=in_.ap), [in_], [out])

    def memset(self, eng, out, val):
        return self.op(eng, lambda e: e.memset(out.ap, val), [], [out])

    def tt(self, eng, out, a, b, op):
        return self.op(eng, lambda e: e.tensor_tensor(out=out.ap, in0=a.ap, in1=b.ap, op=op), [a, b], [out])

    def ts(self, eng, out, a, s1, s2, op0, op1=None, accum=None):
        rd = [a]
        x1 = s1.ap if isinstance(s1, V) else s1
        x2 = s2.ap if isinstance(s2, V) else s2
        if isinstance(s1, V):
            rd.append(s1)
        if isinstance(s2, V):
            rd.append(s2)
        kw = {}
        wr = [out]
        if op1 is not None:
            kw["op1"] = op1
        if accum is not None:
            kw["accum_out"] = accum.ap
            wr.append(accum)
        return self.op(eng, lambda e: e.tensor_scalar(out=out.ap, in0=a.ap, scalar1=x1, scalar2=x2, op0=op0, **kw),
                       rd, wr)

    def stt(self, eng, out, a, s, b, op0, op1):
        rd = [a, b]
        x = s.ap if isinstance(s, V) else s
        if isinstance(s, V):
            rd.append(s)
        return self.op(eng, lambda e: e.scalar_tensor_tensor(out=out.ap, in0=a.ap, scalar=x, in1=b.ap, op0=op0, op1=op1),
                       rd, [out])


def build_program():
    nc = bass.Bass("TRN2", target_bir_lowering=False)
    es = ExitStack()
    with es:
        kb = KB(nc, es)
        try:
            _build(nc, kb)
        except StopBuild:
            while len(kb.stack) > 1:
                kb.phase_end()
        kb.finish()
    return nc


class StopBuild(Exception):
    pass


import os
STAGE = int(os.environ.get("KSTAGE", "99"))


def stage_gate(n):
    if STAGE < n:
        raise StopBuild()


SLOPES = [2.0 ** (-(h + 1)) for h in range(8)]
NTOK = 1056


def _build(nc, kb):
    dram_in = lambda name, shape, dt=F32: kb.dram(name, shape, dt, kind="ExternalInput")
    dram_out = lambda name, shape, dt=F32: kb.dram(name, shape, dt, kind="ExternalOutput")
    x_full = dram_in("x_full", [SEQ, D])
    x_own = dram_in("x_own", [1024, D])
    x_s = dram_in("x_s", [32, D])
    w_in = dram_in("w_in", [D, D_IN])
    norm_mix = dram_in("norm_mix", [1, D])
    ident_in = dram_in("ident", [128, 128])
    rnn_par_in = dram_in("rnn_par", [128, 8, 8])
    wa_bd_in = dram_in("wa_bd", [128, 8, 128])
    wx_bd_in = dram_in("wx_bd", [128, 8, 128])
    onehot_in = dram_in("onehot", [128, 4])
    st_h_in = dram_in("st_h", [128, 4, 8])
    st_conv_in = dram_in("st_conv", [128, 4, 8, 3])
    cwin_in = dram_in("cwin", [4, 512, 512])
    poolk_in = dram_in("poolk", [128, 4])
    poolv_in = dram_in("poolv", [128, 252])
    tpos_in = dram_in("tpos", [128, 8])
    curb_in = dram_in("curb", [128, 8])
    iota_in = dram_in("iota4096", [1, 4096])
    iotace_in = dram_in("iota_ce", [1, 128])
    iotablk_in = dram_in("iota_blk", [1, 64])
    negdw_in = dram_in("negdw", [128, 640])
    bandw_in = dram_in("bandw", [128, 640])
    w_out = dram_in("w_out", [D, D])
    norm_ffn = dram_in("norm_ffn", [1, D])
    norm_final = dram_in("norm_final", [1, D])
    w_router = dram_in("w_router", [D, 20])
    b_router = dram_in("b_router", [1, 20])
    w_gate = dram_in("w_gate", [16, D, 512])
    w_up = dram_in("w_up", [16, D, 512])
    w_down = dram_in("w_down", [16, 512, D])
    cache_c = dram_in("cache_c", [5120 * 128, 512])
    cache_s = dram_in("cache_s", [5120 * 128, 512])
    pt_in = dram_in("pt", [1, 512], I32)
    pidx_in = dram_in("pidx", [128, 1])
    cbs_in = dram_in("cb_s", [128, 2, 512])
    slrow_in = dram_in("slope_rows", [128, 2])
    stok_in = dram_in("stok", [128, 2])
    wbs_in = dram_in("wb_s", [128, 2, 520])
    wms_in = dram_in("wm_s", [128, 520])
    nbs_in = dram_in("nb_s", [128, 2, 8])
    nms_in = dram_in("nm_s", [128, 8])
    rsum_in = dram_in("rsum", [128, 128])
    forced_in = dram_in("forced_s", [1, 257])
    iota512_in = dram_in("iota512", [1, 512])
    o_h_p = dram_out("o_h_p", [128, 8])
    o_h_s = dram_out("o_h_s", [128, 4, 8])
    o_conv_s = dram_out("o_conv_s", [128, 4, 8, 3])
    o_win_s = dram_out("o_win_s", [4, 512, 512])
    o_conv_p = dram_out("o_conv_p", [128, 8, 3])
    o_kv_p = dram_out("o_kv_p", [SEQ, 3 * KVW])
    o_kv_s = dram_out("o_kv_s", [32, 3 * KVW])
    o_y = dram_out("o_y", [NTOK, D])
    scr_kT = kb.dram("scr_kT", [2, 2, 128, SEQ], BF16)
    scr_v = kb.dram("scr_v", [2, 32, 128, 2, 128], BF16)
    scr_q = kb.dram("scr_q", [128, 8, NTOK], BF16)
    scr_cat = kb.dram("scr_cat", [128, 16, NTOK], BF16)
    scr_kvs = kb.dram("scr_kvs", [32, 3 * KVW], BF16)
    scr_g = kb.dram("scr_g", [32, 24], F32)

    ident_f = kb.sb("ident_f", [128, 128], F32)
    ident_b = kb.sb("ident_b", [128, 128], BF16)
    kb.dma("sp", ident_f[:], ident_in[:])
    kb.copy("dve", ident_b[:], ident_f[:])
    eps_t = kb.sb("eps_t", [128, 1], F32)
    kb.memset("dve", eps_t[:], EPS)
    one_t = kb.sb("one_t", [128, 1], F32)
    kb.memset("dve", one_t[:], 1.0)
    onehot = kb.sb("onehot_sb", [128, 4], F32)
    kb.dma("sp", onehot[:], onehot_in[:])
    gates_sb = kb.sb("gates_sb", [128, 9, 24], F32)
    h_s_all = kb.sb("h_s_all", [128, 8, 32], F32)
    kcmpT_sb = kb.sb("kcmpT_sb", [128, 2, 128], BF16)
    vcmp_sb = kb.sb("vcmp_sb", [128, 2, 128], BF16)
    ssq = kb.sb("ssq", [128, 1], F32)
    rstd = kb.sb("rstd", [128, 1], F32)
    psb = [kb.ps("psb%d" % i, [128, 512], F32) for i in range(8)]

    def bview(pb):
        return pb.t[:].bitcast(BF16)

    kb.phase_begin()
    gmix = kb.sb("gmix", [128, D], F32)
    kb.dma("sp", gmix[:], norm_mix.v(norm_mix.t[0:1, :].partition_broadcast(128)))
    xs = kb.sb("xs", [128, D], F32)
    xn = kb.sb("xn", [128, D], BF16)
    xnT = kb.sb("xnT", [128, 16, 512], BF16)
    own_h = kb.sb("own_h", [128, 8, 1024], BF16)
    kb.memset("pool", own_h[:], 0.0)

    def rms_rows(src_tile, nrows, gam, dst_bf, junk):
        kb.act(junk.v(junk.t[0:nrows, :]), src_tile.v(src_tile.t[0:nrows, :]), AF.Square, accum=ssq.v(ssq.t[0:nrows, :]))
        kb.act(rstd.v(rstd.t[0:nrows, :]), ssq.v(ssq.t[0:nrows, :]), AF.Sqrt, bias=eps_t.v(eps_t.t[0:nrows, :]), scale=1.0 / D)
        kb.op("dve", lambda e: e.reciprocal(out=rstd.t[0:nrows, :], in_=rstd.t[0:nrows, :]), [rstd[:]], [rstd[:]])
        kb.stt("dve", dst_bf.v(dst_bf.t[0:nrows, :]), src_tile.v(src_tile.t[0:nrows, :]), rstd.v(rstd.t[0:nrows, 0:1]),
               gam.v(gam.t[0:nrows, :]), ALU.mult, ALU.mult)

    def transpose16(src_bf, nrows, dstT, col0):
        for q4 in range(4):
            pb = psb[q4]
            pbv = bview(pb)
            for kk in range(4):
                k = q4 * 4 + kk
                kb.tr(pb.v(pbv[:, kk * 128: kk * 128 + nrows]), src_bf.v(src_bf.t[0:nrows, k * 128:(k + 1) * 128]),
                      ident_b.v(ident_b.t[0:nrows, 0:nrows]), signal=(kk == 3))
            src = pbv[:, 0:512].rearrange("p (a b) -> p a b", a=4)[:, :, 0:nrows]
            dst = dstT.t[:, q4 * 4:(q4 + 1) * 4, col0:col0 + nrows]
            kb.copy("act" if q4 % 2 == 0 else "dve", dstT.v(dst), pb.v(src))

    def norm_transpose(src_rows, nrows, dstT, col0):
        kb.dma("sp", xs.v(xs.t[0:nrows, :]), src_rows)
        rms_rows(xs, nrows, gmix, xn, xn)
        transpose16(xn, nrows, dstT, col0)

    w_view = w_in.t.rearrange("(k p) n -> p k n", p=128)

    kb.phase_begin()
    W = kb.sb("W", [128, 16, 2560], BF16)
    for k in range(16):
        kb.dma("pool", W.v(W.t[:, k, 0:1536]), w_in.v(w_view[:, k, C_KC:C_KC + 1536]))
        kb.dma("pool", W.v(W.t[:, k, 1536:2560]), w_in.v(w_view[:, k, C_XR:C_XR + 1024]))
    kvf = kb.sb("kvf", [128, 3 * KVW], F32)
    kvb = kb.sb("kvb", [128, 3 * KVW], BF16)
    kTt = kb.sb("kTt", [128, 4, 128], BF16)
    rpar = kb.sb("rpar", [128, 8, 8], F32)
    kb.dma("sp", rpar[:], rnn_par_in[:])
    wa_bd = kb.sb("wa_bd_sb", [128, 8, 128], BF16)
    wx_bd = kb.sb("wx_bd_sb", [128, 8, 128], BF16)
    kb.dma("pool", wa_bd[:], wa_bd_in[:])
    kb.dma("pool", wx_bd[:], wx_bd_in[:])
    poolk = kb.sb("poolk_sb", [128, 4], BF16)
    poolv = kb.sb("poolv_sb", [128, 252], BF16)
    kb.dma("pool", poolk[:], poolk_in[:])
    kb.dma("pool", poolv[:], poolv_in[:])
    cdec = kb.sb("cdec", [128, 8], F32)
    kb.act(cdec[:], rpar.v(rpar.t[:, :, 7]), AF.Exp, scale=-1.0)
    kb.act(cdec[:], cdec[:], AF.Ln, bias=one_t[:], scale=1.0)
    kb.ts("dve", cdec[:], cdec[:], -8.0, None, ALU.mult)
    xp = [kb.sb("xp%d" % m, [128, 515], F32) for m in range(8)]
    hprev = kb.sb("hprev", [128, 8], F32)
    kb.memset("dve", hprev[:], 0.0)
    for m in range(8):
        kb.memset("pool", xp[m][:, 0:3], 0.0)
    r_xc = kb.sb("r_xc", [128, 512], F32)
    r_xcb = kb.sb("r_xcb", [128, 512], BF16)
    r_r = kb.sb("r_r", [128, 512], F32)
    r_i = kb.sb("r_i", [128, 512], F32)
    r_s = kb.sb("r_s", [128, 512], F32)
    r_u = kb.sb("r_u", [128, 512], F32)
    r_h = kb.sb("r_h", [128, 512], F32)
    kb.memset("dve", psb[7][:], 0.0)

    def kv_tile(xT, col0, nrows, out_rows, tile_idx):
        for blk in range(3):
            pb = psb[4 + (blk % 2)]
            for k in range(16):
                kb.mm(pb.v(pb.t[0:nrows, :]), xT.v(xT.t[:, k, col0:col0 + nrows]), W.v(W.t[:, k, blk * 512:(blk + 1) * 512]),
                      start=(k == 0), stop=(k == 15), signal=(k == 15))
            kb.copy("act" if blk % 2 == 0 else "dve", kvf.v(kvf.t[0:nrows, blk * 512:(blk + 1) * 512]), pb.v(pb.t[0:nrows, :]))
        kb.dma("sp", out_rows, kvf.v(kvf.t[0:nrows, :]))
        if tile_idx is None:
            return
        kb.copy("pool", kvb[:], kvf[:])
        for br in range(2):
            src = kvb.t[:, 512 * (br + 1):512 * (br + 2)].rearrange("p (g c d) -> p g c d", g=2, c=2)[:, :, 1, :]
            kb.dma("sp", scr_v.v(scr_v.t[br, tile_idx]), kvb.v(src))
        pb = psb[4]
        pbv = bview(pb)
        for br in range(2):
            for g in range(2):
                c = 512 * (br + 1) + g * 256
                kb.tr(pb.v(pbv[:, (br * 2 + g) * 128:(br * 2 + g + 1) * 128]), kvb.v(kvb.t[:, c:c + 128]), ident_b[:],
                      signal=(br == 1 and g == 1))
        kb.copy("act", kTt[:], pb.v(pbv[:, 0:512].rearrange("p (a b) -> p a b", a=4)))
        dst = scr_kT.t[:, :, :, tile_idx * 128:(tile_idx + 1) * 128].rearrange("b g d t -> d (b g) t")
        kb.dma("sp", scr_kT.v(dst), kTt[:])
        p7 = psb[7]
        for g in range(2):
            kb.mm(p7.v(p7.t[:, g * 128 + 4 * tile_idx: g * 128 + 4 * tile_idx + 4]), kvb.v(kvb.t[:, g * 256:g * 256 + 128]), poolk[:],
                  start=False, stop=False, signal=False)
        vsrc = kvb.t[:, 0:512].rearrange("p (g c d) -> p g c d", g=2, c=2)[:, :, 1, :]
        kb.mm(p7.v(p7.t[:, 256:512].rearrange("p (g d) -> p g d", g=2)), poolv.v(poolv.t[:, 124 - 4 * tile_idx:252 - 4 * tile_idx]), kvb.v(vsrc),
              start=False, stop=False, signal=True)

    def rnn_group(xT, n, hinit, xpl, sel_dst, c0=0):
        for m in range(8):
            pb = psb[6]
            for k in range(16):
                kb.mm(pb.v(pb.t[:, 0:n]), W.v(W.t[:, k, 1536 + m * 128:1536 + (m + 1) * 128]), xT.v(xT.t[:, k, c0:c0 + n]),
                      start=(k == 0), stop=(k == 15), signal=(k == 15))
            xpm = xpl[m]
            kb.copy("act", xpm.v(xpm.t[:, 3:3 + n]), pb.v(pb.t[:, 0:n]))
            kb.ts("dve", r_xc.v(r_xc.t[:, 0:n]), xpm.v(xpm.t[:, 0:n]), rpar.v(rpar.t[:, m, 0:1]), rpar.v(rpar.t[:, m, 4:5]), ALU.mult, ALU.add)
            for kk in range(1, 4):
                kb.stt("dve", r_xc.v(r_xc.t[:, 0:n]), xpm.v(xpm.t[:, kk:kk + n]), rpar.v(rpar.t[:, m, kk:kk + 1]), r_xc.v(r_xc.t[:, 0:n]), ALU.mult, ALU.add)
            kb.copy("pool", xpm.v(xpm.t[:, 0:3]), xpm.v(xpm.t[:, n:n + 3]))
            kb.copy("pool", r_xcb.v(r_xcb.t[:, 0:n]), r_xc.v(r_xc.t[:, 0:n]))
            pa = psb[4]
            kb.mm(pa.v(pa.t[:, 0:n]), wa_bd.v(wa_bd.t[:, m, :]), r_xcb.v(r_xcb.t[:, 0:n]))
            kb.act(r_r.v(r_r.t[:, 0:n]), pa.v(pa.t[:, 0:n]), AF.Sigmoid, bias=rpar.v(rpar.t[:, m, 5:6]), scale=1.0)
            px = psb[5]
            kb.mm(px.v(px.t[:, 0:n]), wx_bd.v(wx_bd.t[:, m, :]), r_xcb.v(r_xcb.t[:, 0:n]))
            kb.act(r_i.v(r_i.t[:, 0:n]), px.v(px.t[:, 0:n]), AF.Sigmoid, bias=rpar.v(rpar.t[:, m, 6:7]), scale=1.0)
            kb.act(r_r.v(r_r.t[:, 0:n]), r_r.v(r_r.t[:, 0:n]), AF.Exp, scale=cdec.v(cdec.t[:, m:m + 1]))
            kb.tt("pool", r_s.v(r_s.t[:, 0:n]), r_r.v(r_r.t[:, 0:n]), r_r.v(r_r.t[:, 0:n]), ALU.mult)
            kb.act(r_s.v(r_s.t[:, 0:n]), r_s.v(r_s.t[:, 0:n]), AF.Sqrt, bias=one_t[:], scale=-1.0)
            kb.tt("pool", r_u.v(r_u.t[:, 0:n]), r_i.v(r_i.t[:, 0:n]), r_xc.v(r_xc.t[:, 0:n]), ALU.mult)
            kb.tt("pool", r_u.v(r_u.t[:, 0:n]), r_u.v(r_u.t[:, 0:n]), r_s.v(r_s.t[:, 0:n]), ALU.mult)
            kb.op("dve", lambda e: e.tensor_tensor_scan(out=r_h.t[:, 0:n], data0=r_r.t[:, 0:n], data1=r_u.t[:, 0:n],
                                                         initial=hinit.t[:, m:m + 1], op0=ALU.mult, op1=ALU.add),
                  [r_r[:], r_u[:], hinit[:]], [r_h[:]])
            kb.copy("dve", hinit.v(hinit.t[:, m:m + 1]), r_h.v(r_h.t[:, n - 1:n]))
            sel_dst(m, r_h)

    def own_sel(G):
        def f(m, rh):
            dst = own_h.v(own_h.t[:, m, (G % 2) * 512:(G % 2) * 512 + 512])
            kb.stt("dve", dst, rh[:], onehot.v(onehot.t[:, G // 2:G // 2 + 1]), dst, ALU.mult, ALU.add)
        return f

    for G in range(8):
        for i in range(4):
            r0 = G * 512 + i * 128
            norm_transpose(x_full.v(x_full.t[r0:r0 + 128, :]), 128, xnT, i * 128)
        for i in range(4):
            r0 = G * 512 + i * 128
            kv_tile(xnT, i * 128, 128, o_kv_p.v(o_kv_p.t[r0:r0 + 128, :]), G * 4 + i)
        rnn_group(xnT, 512, hprev, xp, own_sel(G))
    kb.dma("sp", o_h_p[:], hprev[:])
    for m in range(8):
        kb.dma("sp", o_conv_p.v(o_conv_p.t[:, m, :]), xp[m].v(xp[m].t[:, 0:3]))
    kb.copy("act", kcmpT_sb[:], psb[7].v(psb[7].t[:, 0:256].rearrange("p (g b) -> p g b", g=2)))
    kb.copy("dve", vcmp_sb[:], psb[7].v(psb[7].t[:, 256:512].rearrange("p (g d) -> p g d", g=2)))
    norm_transpose(x_s[:, :], 32, xnT, 0)
    kv_tile(xnT, 0, 32, o_kv_s[:, :], None)
    kb.copy("pool", kvb.v(kvb.t[0:32, :]), kvf.v(kvf.t[0:32, :]))
    kb.dma("sp", scr_kvs[:], kvb.v(kvb.t[0:32, :]))
    for q in range(4):
        kb.dma("sp", o_win_s.v(o_win_s.t[q, 0:504, :]), cwin_in.v(cwin_in.t[q, 8:512, :]))
        kb.dma("sp", o_win_s.v(o_win_s.t[q, 504:512, :]), kvf.v(kvf.t[8 * q:8 * q + 8, 1024:1536]))
    hs = kb.sb("hs", [128, 4, 8], F32)
    kb.dma("sp", hs[:], st_h_in[:])
    xps = [[kb.sb("xps%d_%d" % (q, m), [128, 16], F32) for m in range(8)] for q in range(4)]
    stc = kb.sb("stc", [128, 4, 8, 3], F32)
    kb.dma("sp", stc[:], st_conv_in[:])
    for q in range(4):
        for m in range(8):
            kb.copy("pool", xps[q][m].v(xps[q][m].t[:, 0:3]), stc.v(stc.t[:, q, m, :]))
        hq = T(hs.t[:, q, :], "hsq")
        hq.buf = hs.buf

        def keep(m, rh, q=q):
            kb.copy("pool", h_s_all.v(h_s_all.t[:, m, 8 * q:8 * q + 8]), rh.v(rh.t[:, 0:8]))
        rnn_group(xnT, 8, hq, xps[q], keep, c0=8 * q)
        for m in range(8):
            kb.dma("sp", o_conv_s.v(o_conv_s.t[:, q, m, :]), xps[q][m].v(xps[q][m].t[:, 0:3]))
    kb.dma("sp", o_h_s[:], hs[:])
    kb.phase_end()
    stage_gate(2)

    kb.phase_begin()
    W2 = kb.sb("W2", [128, 16, 2072], BF16)
    for k in range(16):
        kb.dma("pool", W2.v(W2.t[:, k, 0:1024]), w_in.v(w_view[:, k, C_Q:C_Q + 1024]))
        kb.dma("pool", W2.v(W2.t[:, k, 1024:1048]), w_in.v(w_view[:, k, C_GT:C_GT + 24]))
        kb.dma("pool", W2.v(W2.t[:, k, 1048:2072]), w_in.v(w_view[:, k, C_XG:C_XG + 1024]))
    qTt = kb.sb("qTt", [128, 8, 512], BF16)
    g_x = kb.sb("g_x", [128, 512], F32)
    g_t = kb.sb("g_t", [128, 512], F32)
    g_o = kb.sb("g_o", [128, 512], BF16)
    hsb = kb.sb("hsb", [128, 8, 32], BF16)
    kb.copy("pool", hsb[:], h_s_all[:])
    for G in range(3):
        n = 512 if G < 2 else 32
        t0 = G * 512
        if G < 2:
            for i in range(4):
                norm_transpose(x_own.v(x_own.t[t0 + i * 128:t0 + (i + 1) * 128, :]), 128, xnT, i * 128)
        else:
            norm_transpose(x_s[:, :], 32, xnT, 0)
        for h in range(8):
            pb = psb[4 + (h % 2)]
            for k in range(16):
                kb.mm(pb.v(pb.t[:, 0:n]), W2.v(W2.t[:, k, h * 128:(h + 1) * 128]), xnT.v(xnT.t[:, k, 0:n]),
                      start=(k == 0), stop=(k == 15), signal=(k == 15))
            kb.act(qTt.v(qTt.t[:, h, 0:n]), pb.v(pb.t[:, 0:n]), AF.Copy, scale=float(128 ** -0.5))
        kb.dma("sp", scr_q.v(scr_q.t[:, :, t0:t0 + n]), qTt.v(qTt.t[:, :, 0:n]))
        for i in range((n + 127) // 128):
            rows = min(128, n - i * 128)
            pb = psb[6]
            for k in range(16):
                kb.mm(pb.v(pb.t[0:rows, 0:24]), xnT.v(xnT.t[:, k, i * 128:i * 128 + rows]), W2.v(W2.t[:, k, 1024:1048]),
                      start=(k == 0), stop=(k == 15), signal=(k == 15))
            kb.act(gates_sb.v(gates_sb.t[0:rows, G * 4 + i, :]), pb.v(pb.t[0:rows, 0:24]), AF.Sigmoid)
        for m in range(8):
            pb = psb[4 + (m % 2)]
            for k in range(16):
                kb.mm(pb.v(pb.t[:, 0:n]), W2.v(W2.t[:, k, 1048 + m * 128:1048 + (m + 1) * 128]), xnT.v(xnT.t[:, k, 0:n]),
                      start=(k == 0), stop=(k == 15), signal=(k == 15))
            gx, gt, go = g_x.v(g_x.t[:, 0:n]), g_t.v(g_t.t[:, 0:n]), g_o.v(g_o.t[:, 0:n])
            kb.copy("act", gx, pb.v(pb.t[:, 0:n]))
            kb.tt("pool", gt, gx, gx, ALU.mult)
            kb.ts("dve", gt, gt, 0.044715, 1.0, ALU.mult, ALU.add)
            kb.tt("pool", gt, gt, gx, ALU.mult)
            kb.act(gt, gt, AF.Sigmoid, scale=1.5957691216057308)
            kb.tt("pool", gt, gt, gx, ALU.mult)
            hsrc = own_h.v(own_h.t[:, m, t0:t0 + n]) if G < 2 else hsb.v(hsb.t[:, m, :])
            kb.tt("pool", go, gt, hsrc, ALU.mult)
            kb.dma("sp", scr_cat.v(scr_cat.t[:, 8 + m, t0:t0 + n]), go)
    kb.dma("sp", scr_g[:], gates_sb.v(gates_sb.t[0:32, 8, :]))
    kb.phase_end()
    kb.phase_end()
    stage_gate(3)

    kb.phase_begin()
    iota_p = kb.sb("iota_p", [128, 4096], F32)
    kb.dma("sp", iota_p[:], iota_in.v(iota_in.t[0:1, :].partition_broadcast(128)))
    iota_ce = kb.sb("iota_ce_sb", [128, 128], F32)
    kb.dma("sp", iota_ce[:], iotace_in.v(iotace_in.t[0:1, :].partition_broadcast(128)))
    iota_blk = kb.sb("iota_blk_sb", [128, 64], F32)
    kb.dma("sp", iota_blk[:], iotablk_in.v(iotablk_in.t[0:1, :].partition_broadcast(128)))
    tpos = kb.sb("tpos_sb", [128, 8], F32)
    kb.dma("sp", tpos[:], tpos_in[:])
    curb = kb.sb("curb_sb", [128, 8], F32)
    kb.dma("sp", curb[:], curb_in[:])
    curm1 = kb.sb("curm1", [128, 8], F32)
    kb.ts("dve", curm1[:], curb[:], -1.0, None, ALU.add)
    negdw = kb.sb("negdw_sb", [128, 640], F32)
    kb.dma("sp", negdw[:], negdw_in[:])
    bandw = kb.sb("bandw_sb", [128, 640], F32)
    kb.dma("sp", bandw[:], bandw_in[:])
    kselT = kb.sb("kselT", [128, 2, SEQ], BF16)
    vsel = kb.sb("vsel", [128, 32, 2, 129], BF16)
    kwin_loc = kb.sb("kwin_loc", [128, 2, 1536], BF16)
    vwin_loc = kb.sb("vwin_loc", [128, 12, 2, 129], BF16)
    kb.memset("pool", vsel[:], 1.0)
    kb.memset("pool", vwin_loc[:], 1.0)
    kb.memset("pool", vwin_loc.v(vwin_loc.t[:, :, :, 0:128]), 0.0)
    kb.memset("pool", kwin_loc[:], 0.0)
    for g in range(2):
        kb.dma("sp", kselT.v(kselT.t[:, g, :]), scr_kT.v(scr_kT.t[0, g]))
    for t in range(32):
        kb.dma("sp", vsel.v(vsel.t[:, t, :, 0:128]), scr_v.v(scr_v.t[0, t]))
    negd = kb.sb("negd", [128, 4096], F32)
    Sb = kb.sb("Sb", [128, 4096], F32)
    kwfull = T(Sb.t[:].bitcast(BF16), "kwfull")
    kwfull.buf = Sb.buf
    vwfull = T(negd.t[:].bitcast(BF16), "vwfull")
    vwfull.buf = negd.buf
    for g in range(2):
        kb.dma("sp", kwfull.v(kwfull.t[:, g * 4096:(g + 1) * 4096]), scr_kT.v(scr_kT.t[1, g]))
    for q in range(4):
        lo = 8 * q - 4
        a = max(lo, 0)
        for g in range(2):
            dst = kwin_loc.v(kwin_loc.t[:, g, (a - lo) * 128:1536])
            kb.stt("dve", dst, kwfull.v(kwfull.t[:, g * 4096 + a * 128:g * 4096 + (lo + 12) * 128]), onehot.v(onehot.t[:, q:q + 1]), dst, ALU.mult, ALU.add)
    vw3 = vwfull.t[:, 0:8192].rearrange("p (t g d) -> p t g d", t=32, g=2)
    for t in range(32):
        kb.dma("sp", vwfull.v(vw3[:, t]), scr_v.v(scr_v.t[1, t]))
    for q in range(4):
        lo = 8 * q - 4
        a = max(lo, 0)
        dst = vwin_loc.v(vwin_loc.t[:, a - lo:12, :, 0:128])
        kb.stt("dve", dst, vwfull.v(vw3[:, a:lo + 12]), onehot.v(onehot.t[:, q:q + 1]), dst, ALU.mult, ALU.add)
    maskg = kb.sb("maskg", [128, 4096], BF16)
    Eb = kb.sb("Eb", [128, 4096], BF16)
    PT = kb.sb("PT", [128, 8, 128], BF16)
    Sb_b = kb.sb("Sb_b", [128, 4096], F32)
    Eb_b = kb.sb("Eb_b", [128, 4096], BF16)
    PT_b = kb.sb("PT_b", [128, 8, 128], BF16)
    Sb2, Eb2, PT2 = [Sb, Sb_b], [Eb, Eb_b], [PT, PT_b]
    qs = kb.sb("qs", [128, 8, 128], BF16)
    negdc = kb.sb("negdc", [128, 128], F32)
    validc = kb.sb("validc", [128, 128], F32)
    Sc = kb.sb("Sc", [128, 4, 128], F32)
    pcb = kb.sb("pcb", [128, 4, 128], BF16)
    mc = kb.sb("mc", [128, 4], F32)
    sc = kb.sb("sc", [128, 4], F32)
    imp = kb.sb("imp", [128, 64], F32)
    imp2 = kb.sb("imp2", [128, 64], F32)
    fm = kb.sb("fm", [128, 64], F32)
    validb = kb.sb("validb", [128, 64], F32)
    selb = kb.sb("selb", [128, 64], BF16)
    mx8 = kb.sb("mx8", [128, 8], F32)
    thr = kb.sb("thr", [128, 1], F32)
    m1_2 = [kb.sb("m1_%d" % i, [128, 1], F32) for i in range(4)]
    coef_2 = [kb.sb("coef_%d" % i, [128, 1], F32) for i in range(4)]
    Sw2 = [kb.sb("Sw%d" % i, [128, 640], F32) for i in range(2)]
    Ew2 = [kb.sb("Ew%d" % i, [128, 640], BF16) for i in range(2)]
    PTw2 = [kb.sb("PTw%d" % i, [128, 8, 128], BF16) for i in range(2)]
    wm = kb.sb("wm", [128, 640], BF16)
    wtmp = kb.sb("wtmp", [128, 640], F32)
    attn_tm = kb.sb("attn_tm", [128, 1024], F32)
    attn_bf = kb.sb("attn_bf", [128, 1024], BF16)
    aT = kb.sb("aT", [128, 8, 128], BF16)

    def softmax_A(S_t, width, E_t, mask_v, m1):
        kb.op("dve", lambda e: e.tensor_reduce(out=m1.t[:], in_=S_t.t[:, 0:width], axis=AX.X, op=ALU.max), [S_t[:]], [m1[:]])
        kb.ts("dve", m1[:], m1[:], -1.0, None, ALU.mult)
        kb.act(E_t.v(E_t.t[:, 0:width]), S_t.v(S_t.t[:, 0:width]), AF.Exp, bias=m1[:], scale=1.0)
        kb.tt("pool", E_t.v(E_t.t[:, 0:width]), E_t.v(E_t.t[:, 0:width]), mask_v, ALU.mult)

    def softmax_B(width, E_t, ntiles, vsrc, po, h, gate_col, s, first, PT, coef, tb):
        for rr in range((ntiles + 7) // 8):
            cnt = min(8, ntiles - rr * 8)
            pt = psb[tb + rr % 2]
            ptv = bview(pt)
            for i in range(cnt):
                kt = rr * 8 + i
                kb.tr(pt.v(ptv[:, i * 128:(i + 1) * 128]), E_t.v(E_t.t[:, kt * 128:(kt + 1) * 128]), ident_b[:], signal=(i == cnt - 1))
            kb.copy("act", PT.v(PT.t[:, 0:cnt, :]), pt.v(ptv[:, 0:cnt * 128].rearrange("p (a b) -> p a b", a=cnt)))
            for i in range(cnt):
                kt = rr * 8 + i
                kb.mm(po.v(po.t[:, 0:129]), PT.v(PT.t[:, i, :]), vsrc(kt), start=(kt == 0), stop=(kt == ntiles - 1),
                      signal=(kt == ntiles - 1))
        kb.ts("dve", coef[:], po.v(po.t[:, 128:129]), 1e-30, None, ALU.max)
        kb.op("dve", lambda e: e.reciprocal(out=coef.t[:], in_=coef.t[:]), [coef[:]], [coef[:]])
        kb.tt("dve", coef[:], coef[:], gates_sb.v(gates_sb.t[:, s, gate_col:gate_col + 1]), ALU.mult)
        dst = attn_tm.v(attn_tm.t[:, h * 128:(h + 1) * 128])
        if first:
            kb.ts("dve", dst, po.v(po.t[:, 0:128]), coef.v(coef.t[:, 0:1]), None, ALU.mult)
        else:
            kb.stt("dve", dst, po.v(po.t[:, 0:128]), coef.v(coef.t[:, 0:1]), dst, ALU.mult, ALU.add)

    for s in range(8):
        tq = tpos.v(tpos.t[:, s:s + 1])
        kb.dma("sp", qs[:], scr_q.v(scr_q.t[:, :, s * 128:(s + 1) * 128]))
        kb.ts("dve", negd[:], iota_p[:], tq, 0.0, ALU.subtract, ALU.min)
        kb.ts("dve", negdc[:], iota_ce[:], tq, 0.0, ALU.subtract, ALU.min)
        kb.ts("dve", validc[:], iota_ce[:], tq, None, ALU.is_le)
        kb.ts("dve", wtmp[:], iota_p.v(iota_p.t[:, 0:640]), float(512 - 128 * s), onehot.v(onehot.t[:, 0:1]), ALU.is_lt, ALU.mult)
        kb.ts("dve", wtmp[:], wtmp[:], -1.0, 1.0, ALU.mult, ALU.add)
        kb.tt("pool", wm[:], bandw[:], wtmp[:], ALU.mult)
        for g in range(2):
            pc = psb[0]
            for r in range(4):
                kb.mm(pc.v(pc.t[:, r * 128:(r + 1) * 128]), qs.v(qs.t[:, 4 * g + r, :]), kcmpT_sb.v(kcmpT_sb.t[:, g, :]), signal=(r == 3))
            for r in range(4):
                kb.stt("dve", Sc.v(Sc.t[:, r, :]), negdc[:], SLOPES[4 * g + r], pc.v(pc.t[:, r * 128:(r + 1) * 128]), ALU.mult, ALU.add)
            kb.op("dve", lambda e: e.tensor_reduce(out=mc.t[:], in_=Sc.t[:], axis=AX.X, op=ALU.max), [Sc[:]], [mc[:]])
            kb.ts("dve", mc[:], mc[:], -1.0, None, ALU.mult)
            for r in range(4):
                kb.act(Sc.v(Sc.t[:, r, :]), Sc.v(Sc.t[:, r, :]), AF.Exp, bias=mc.v(mc.t[:, r:r + 1]), scale=1.0)
            kb.tt("pool", Sc[:], Sc[:], validc.v(validc.t[:, :].unsqueeze(1).to_broadcast([128, 4, 128])), ALU.mult)
            kb.op("dve", lambda e: e.tensor_reduce(out=sc.t[:], in_=Sc.t[:], axis=AX.X, op=ALU.add), [Sc[:]], [sc[:]])
            kb.ts("dve", sc[:], sc[:], 1e-30, None, ALU.max)
            kb.op("dve", lambda e: e.reciprocal(out=sc.t[:], in_=sc.t[:]), [sc[:]], [sc[:]])
            kb.tt("dve", Sc[:], Sc[:], sc.v(sc.t[:, :].unsqueeze(2).to_broadcast([128, 4, 128])), ALU.mult)
            kb.op("dve", lambda e: e.tensor_reduce(out=imp.t[:], in_=Sc.t[:].rearrange("p h (j two) -> p j h two", two=2),
                                                    axis=AX.XY, op=ALU.add), [Sc[:]], [imp[:]])
            cur = curb.v(curb.t[:, s:s + 1])
            kb.ts("dve", fm[:], iota_blk[:], cur, None, ALU.is_equal)
            kb.ts("dve", imp2[:], iota_blk[:], curm1.v(curm1.t[:, s:s + 1]), None, ALU.is_equal)
            kb.tt("dve", fm[:], fm[:], imp2[:], ALU.max)
            kb.ts("dve", imp2[:], iota_blk[:], 0.0, None, ALU.is_equal)
            kb.tt("dve", fm[:], fm[:], imp2[:], ALU.max)
            kb.stt("dve", imp[:], fm[:], 1e4, imp[:], ALU.mult, ALU.max)
            kb.ts("dve", validb[:], iota_blk[:], cur, None, ALU.is_le)
            kb.tt("dve", imp[:], imp[:], validb[:], ALU.mult)
            kb.stt("dve", imp[:], validb[:], -1.0, imp[:], ALU.add, ALU.add)
            kb.op("dve", lambda e: e.max(out=mx8.t[:], in_=imp.t[:]), [imp[:]], [mx8[:]])
            kb.op("dve", lambda e: e.match_replace(out=imp2.t[:], in_to_replace=mx8.t[:], in_values=imp.t[:], imm_value=-2.0),
                  [mx8[:], imp[:]], [imp2[:]])
            kb.op("dve", lambda e: e.max(out=mx8.t[:], in_=imp2.t[:]), [imp2[:]], [mx8[:]])
            kb.op("dve", lambda e: e.tensor_reduce(out=thr.t[:], in_=mx8.t[:], axis=AX.X, op=ALU.min), [mx8[:]], [thr[:]])
            kb.ts("dve", imp2[:], imp[:], thr.v(thr.t[:, 0:1]), None, ALU.is_ge)
            kb.tt("dve", selb[:], imp2[:], validb[:], ALU.mult)
            kb.stt("dve", maskg.v(maskg.t[:].rearrange("p (b k) -> p b k", k=64)), iota_p.v(iota_p.t[:].rearrange("p (b k) -> p b k", k=64)),
                   tq, selb.v(selb.t[:, :].unsqueeze(2).to_broadcast([128, 64, 64])), ALU.is_le, ALU.mult)
            kb.copy("pool", pcb[:], Sc[:])
            pt = psb[1]
            ptv = bview(pt)
            for r in range(4):
                kb.tr(pt.v(ptv[:, r * 128:(r + 1) * 128]), pcb.v(pcb.t[:, r, :]), ident_b[:], signal=(r == 3))
            kb.copy("act", PT.v(PT.t[:, 0:4, :]), pt.v(ptv[:, 0:512].rearrange("p (a b) -> p a b", a=4)))
            po = psb[0]
            for r in range(4):
                kb.mm(po.v(po.t[:, r * 128:(r + 1) * 128]), PT.v(PT.t[:, r, :]), vcmp_sb.v(vcmp_sb.t[:, g, :]), signal=(r == 3))
            for r in range(4):
                h = 4 * g + r
                kb.ts("dve", attn_tm.v(attn_tm.t[:, h * 128:(h + 1) * 128]), po.v(po.t[:, r * 128:(r + 1) * 128]),
                      gates_sb.v(gates_sb.t[:, s, 3 * h:3 * h + 1]), None, ALU.mult)
            def mk(r, g=g, s=s):
                h = 4 * g + r
                par = r % 2
                Sbx, Sw, Ew = Sb2[par], Sw2[par], Ew2[par]

                def A_sel():
                    for c4 in range(8):
                        ps = psb[2 + c4 % 2]
                        kb.mm(ps[:], qs.v(qs.t[:, h, :]), kselT.v(kselT.t[:, g, c4 * 512:(c4 + 1) * 512]))
                        kb.stt("dve", Sbx.v(Sbx.t[:, c4 * 512:(c4 + 1) * 512]), negd.v(negd.t[:, c4 * 512:(c4 + 1) * 512]), SLOPES[h], ps[:], ALU.mult, ALU.add)
                    softmax_A(Sbx, 4096, Eb2[par], maskg[:], m1_2[par])

                def B_sel():
                    softmax_B(4096, Eb2[par], 32, lambda kt: vsel.v(vsel.t[:, kt, g, :]), psb[6] if par == 0 else psb[0], h, 3 * h + 1, s, False,
                              PT2[par], coef_2[par], 4)

                def A_win():
                    kb.mm(psb[2][:], qs.v(qs.t[:, h, :]), kwin_loc.v(kwin_loc.t[:, g, s * 128:s * 128 + 512]))
                    kb.mm(psb[3].v(psb[3].t[:, 0:128]), qs.v(qs.t[:, h, :]), kwin_loc.v(kwin_loc.t[:, g, s * 128 + 512:s * 128 + 640]))
                    kb.stt("dve", Sw.v(Sw.t[:, 0:512]), negdw.v(negdw.t[:, 0:512]), SLOPES[h], psb[2][:], ALU.mult, ALU.add)
                    kb.stt("dve", Sw.v(Sw.t[:, 512:640]), negdw.v(negdw.t[:, 512:640]), SLOPES[h], psb[3].v(psb[3].t[:, 0:128]), ALU.mult, ALU.add)
                    softmax_A(Sw, 640, Ew, wm[:], m1_2[2 + par])

                def B_win():
                    softmax_B(640, Ew, 5, lambda kt: vwin_loc.v(vwin_loc.t[:, s + kt, g, :]), psb[7] if par == 0 else psb[1], h, 3 * h + 2, s, False,
                              PTw2[par], coef_2[2 + par], 4)
                return A_sel, B_sel, A_win, B_win
            jobs = [mk(r) for r in range(4)]
            jobs[0][0]()
            jobs[0][2]()
            for r in range(4):
                if r + 1 < 4:
                    jobs[r + 1][0]()
                jobs[r][1]()
                if r + 1 < 4:
                    jobs[r + 1][2]()
                jobs[r][3]()
        kb.copy("pool", attn_bf[:], attn_tm[:])
        pt = psb[1]
        ptv = bview(pt)
        for h in range(8):
            kb.tr(pt.v(ptv[:, h * 128:(h + 1) * 128]), attn_bf.v(attn_bf.t[:, h * 128:(h + 1) * 128]), ident_b[:], signal=(h == 7))
        kb.copy("act", aT[:], pt.v(ptv[:, 0:1024].rearrange("p (a b) -> p a b", a=8)))
        kb.dma("sp", scr_cat.v(scr_cat.t[:, 0:8, s * 128:(s + 1) * 128]), aT[:])
    kb.phase_end()
    stage_gate(35)

    kb.phase_begin()
    ptb = kb.sb("ptb", [128, 512], I32)
    kb.dma("sp", ptb[:], pt_in.v(pt_in.t[0:1, :].partition_broadcast(128)))
    pidx = kb.sb("pidx_sb", [128, 1], F32)
    kb.dma("sp", pidx[:], pidx_in[:])
    ptf = kb.sb("ptf", [128, 512], F32)
    kb.copy("dve", ptf[:], ptb[:])
    kb.ts("dve", ptf[:], ptf[:], 128.0, pidx.v(pidx.t[:, 0:1]), ALU.mult, ALU.add)
    pidx_i = kb.sb("pidx_i", [128, 512], I32)
    kb.copy("dve", pidx_i[:], ptf[:])
    cbs = kb.sb("cbs", [128, 2, 512], F32)
    kb.dma("sp", cbs[:], cbs_in[:])
    slrow = kb.sb("slrow", [128, 2], F32)
    kb.dma("sp", slrow[:], slrow_in[:])
    stok = kb.sb("stok_sb", [128, 2], F32)
    kb.dma("sp", stok[:], stok_in[:])
    kb.eng["pool"].wait(pidx_i.buf.w)
    wbs = kb.sb("wbs", [128, 2, 520], F32)
    kb.dma("sp", wbs[:], wbs_in[:])
    wms = kb.sb("wms", [128, 520], F32)
    kb.dma("sp", wms[:], wms_in[:])
    nbs = kb.sb("nbs", [128, 2, 8], F32)
    kb.dma("sp", nbs[:], nbs_in[:])
    nms = kb.sb("nms", [128, 8], F32)
    kb.dma("sp", nms[:], nms_in[:])
    rsum_f = kb.sb("rsum_f", [128, 128], F32)
    kb.dma("sp", rsum_f[:], rsum_in[:])
    rsum_b = kb.sb("rsum_b", [128, 128], BF16)
    kb.copy("pool", rsum_b[:], rsum_f[:])
    forced = kb.sb("forced", [128, 257], F32)
    kb.dma("sp", forced[:], forced_in.v(forced_in.t[0:1, :].partition_broadcast(128)))
    iota512 = kb.sb("iota512_sb", [128, 512], F32)
    kb.dma("sp", iota512[:], iota512_in.v(iota512_in.t[0:1, :].partition_broadcast(128)))
    poolk2 = kb.sb("poolk2", [128, 4], BF16)
    poolv2 = kb.sb("poolv2", [128, 252], BF16)
    kb.dma("pool", poolk2[:], poolk_in[:])
    kb.dma("pool", poolv2[:], poolv_in[:])
    qss = kb.sb("qss", [128, 2, 4, 32], BF16)
    for g in range(2):
        for b in range(4):
            kb.dma("sp", qss.v(qss.t[:, g, b, :].rearrange("p (r t) -> p r t", r=4)),
                   scr_q.v(scr_q.t[:, 4 * g:4 * g + 4, 1024 + 8 * b:1024 + 8 * b + 8]))
    grow = kb.sb("grow", [128, 2, 3], F32)
    for g in range(2):
        for b in range(4):
            for r in range(4):
                kb.dma("sp", grow.v(grow.t[32 * b + 8 * r:32 * b + 8 * r + 8, g, :]),
                       scr_g.v(scr_g.t[8 * b:8 * b + 8, (4 * g + r) * 3:(4 * g + r) * 3 + 3]))
    attn_s = kb.sb("attn_s", [128, 2, 128], F32)
    pg = [kb.sb("pg%d" % i, [128, 4, 512], F32) for i in range(6)]
    pgk2 = [kb.sb("pgk%d" % i, [128, 4, 2, 128], BF16) for i in range(2)]
    pgv2 = [kb.sb("pgv%d" % i, [128, 1, 2, 129], BF16) for i in range(2)]
    pgk = pgk2[0]
    for i in range(2):
        kb.memset("pool", pgv2[i][:], 1.0)
    vwS = kb.sb("vwS", [128, 4, 4, 2, 129], BF16)
    kb.memset("pool", vwS[:], 1.0)
    vsS2 = [kb.sb("vsS%d" % i, [128, 4, 4, 2, 129], BF16) for i in range(2)]
    for i in range(2):
        kb.memset("pool", vsS2[i][:], 1.0)
    kTs2 = [kb.sb("kTs%d" % i, [128, 8, 128], BF16) for i in range(2)]
    kTs = kTs2[0]
    kcmpT_s = kb.sb("kcmpT_s", [128, 4, 2, 512], BF16)
    vcmp_s = kb.sb("vcmp_s", [128, 4, 4, 2, 128], BF16)
    S_sb2 = [kb.sb("S_sb%d" % i, [128, 520], F32) for i in range(2)]
    E_sb2 = [kb.sb("E_sb%d" % i, [128, 520], BF16) for i in range(2)]
    PTs2 = [kb.sb("PTs%d" % i, [128, 5, 128], BF16) for i in range(2)]
    S_sb, E_sb, PTs = S_sb2[0], E_sb2[0], PTs2[0]
    mask_c2 = [kb.sb("mask_c%d" % i, [128, 512], BF16) for i in range(2)]
    p_hi = kb.sb("p_hi", [128, 512], BF16)
    p_lo = kb.sb("p_lo", [128, 512], BF16)
    imps = kb.sb("imps", [128, 264], F32)
    imps2 = kb.sb("imps2", [128, 264], F32)
    sels = [kb.sb("sels%d" % g, [128, 264], BF16) for g in range(2)]
    mrun = [kb.sb("mrun%d" % g, [128, 1], F32) for g in range(2)]
    oacc = [kb.sb("oacc%d" % g, [128, 129], F32) for g in range(2)]
    mc_ = kb.sb("mc_", [128, 1], F32)
    mn_ = kb.sb("mn_", [128, 1], F32)
    al_ = kb.sb("al_", [128, 1], F32)
    kc_ = kb.sb("kc_", [128, 1], F32)
    bi_ = kb.sb("bi_", [128, 1], F32)
    cf_ = kb.sb("cf_", [128, 1], F32)
    mx8s = kb.sb("mx8s", [128, 8], F32)
    thrs = kb.sb("thrs", [128, 1], F32)
    sm_ = kb.sb("sm_", [128, 1], F32)
    mask_c = kb.sb("mask_c", [128, 512], BF16)

    def gather_pages(buf, cache, lp):
        for b in range(4):
            col = b * 128 + lp
            kb.dma("pool", buf.v(buf.t[:, b, :]), cache[:, :],
                   fn=lambda e, b=b, col=col: e.indirect_dma_start(out=buf.t[:, b, :], out_offset=None, in_=cache.t[:, :],
                                                                    in_offset=bass.IndirectOffsetOnAxis(ap=pidx_i.t[:, col:col + 1], axis=0)))

    for b in range(4):
        for bank in (4, 5, 6, 7):
            kb.memset("dve", psb[bank][:], 0.0)
        for lp in range(128):
            buf = pg[lp % 6]
            col = b * 128 + lp
            kb.dma("pool", buf.v(buf.t[:, 0, :]), cache_c[:, :],
                   fn=lambda e, buf=buf, col=col: e.indirect_dma_start(out=buf.t[:, 0, :], out_offset=None, in_=cache_c.t[:, :],
                                                                        in_offset=bass.IndirectOffsetOnAxis(ap=pidx_i.t[:, col:col + 1], axis=0)))
            src4 = buf.t[:, 0, :].rearrange("p (g c d) -> p g c d", g=2, c=2)
            pgk, pgv = pgk2[lp % 2], pgv2[lp % 2]
            kb.copy("act", pgk.v(pgk.t[:, 0, :, :]), buf.v(src4[:, :, 0, :]))
            kb.copy("dve", pgv.v(pgv.t[:, 0, :, 0:128]), buf.v(src4[:, :, 1, :]))
            for g in range(2):
                pk = psb[4 + g]
                kb.mm(pk.v(pk.t[:, 4 * lp:4 * lp + 4]), pgk.v(pgk.t[:, 0, g, :]), poolk2[:], start=False, stop=False, signal=False)
            tl, off = lp // 32, lp % 32
            pv = psb[6 + tl // 2]
            kb.mm(pv.v(pv.t[:, (tl % 2) * 256:(tl % 2) * 256 + 256].rearrange("p (g d) -> p g d", g=2)),
                  poolv2.v(poolv2.t[:, 124 - 4 * off:252 - 4 * off]), pgv.v(pgv.t[:, 0, :, 0:128]), start=False, stop=False, signal=True)
        for g in range(2):
            kb.copy("act", kcmpT_s.v(kcmpT_s.t[:, b, g, :]), psb[4 + g][:])
        for tl in range(4):
            pv = psb[6 + tl // 2]
            kb.copy("dve", vcmp_s.v(vcmp_s.t[:, b, tl, :, :]), pv.v(pv.t[:, (tl % 2) * 256:(tl % 2) * 256 + 256].rearrange("p (g d) -> p g d", g=2)))

    def exact_softmax(S_ps_list, width, bias_v, mask_v):
        c0 = 0
        for (ps_v, w) in S_ps_list:
            kb.tt("dve", S_sb.v(S_sb.t[:, c0:c0 + w]), ps_v, V(bias_v.ap[:, c0:c0 + w], bias_v.buf), ALU.add)
            c0 += w
        kb.op("dve", lambda e: e.tensor_reduce(out=mc_.t[:], in_=S_sb.t[:, 0:width], axis=AX.X, op=ALU.max), [S_sb[:]], [mc_[:]])
        kb.ts("dve", mc_[:], mc_[:], -1.0, None, ALU.mult)
        kb.act(E_sb.v(E_sb.t[:, 0:width]), S_sb.v(S_sb.t[:, 0:width]), AF.Exp, bias=mc_[:], scale=1.0)
        if mask_v is not None:
            kb.tt("pool", E_sb.v(E_sb.t[:, 0:width]), E_sb.v(E_sb.t[:, 0:width]), mask_v, ALU.mult)

    def transposeE(ntiles, width, E_sb=E_sb, PTs=PTs):
        pt = psb[4]
        ptv = bview(pt)
        for i in range(ntiles):
            w = min(128, width - i * 128)
            kb.tr(pt.v(ptv[0:w, i * 128:(i + 1) * 128]), E_sb.v(E_sb.t[:, i * 128:i * 128 + w]), ident_b[:], signal=(i == ntiles - 1))
        kb.copy("act", PTs.v(PTs.t[:, 0:ntiles, :]), pt.v(ptv[:, 0:ntiles * 128].rearrange("p (a b) -> p a b", a=ntiles)))

    kvn = kb.sb("kvn", [8, 4, 3 * KVW], BF16)
    for b in range(4):
        kb.dma("sp", kvn.v(kvn.t[:, b, :]), scr_kvs.v(scr_kvs.t[8 * b:8 * b + 8, :]))
    vnew = kb.sb("vnew", [8, 2, 4, 2, 129], BF16)
    kb.memset("pool", vnew[:], 1.0)
    knT = kb.sb("knT", [128, 2, 4, 2, 8], BF16)
    for br in range(2):
        base = 512 * (br + 1)
        for b in range(4):
            src4 = kvn.t[:, b, base:base + 512].rearrange("p (g c d) -> p g c d", g=2, c=2)
            kb.copy("pool", vnew.v(vnew.t[:, br, b, :, 0:128]), kvn.v(src4[:, :, 1, :]))
            pt = psb[3]
            ptv = bview(pt)
            for g in range(2):
                kb.tr(pt.v(ptv[:, g * 8:g * 8 + 8]), kvn.v(kvn.t[:, b, base + g * 256:base + g * 256 + 128]), ident_b.v(ident_b.t[0:8, 0:8]), signal=(g == 1))
            kb.copy("act", knT.v(knT.t[:, br, b, :, :]), pt.v(ptv[:, 0:16].rearrange("p (g k) -> p g k", g=2)))

    for g in range(2):
        pc = psb[0]
        for b in range(4):
            kb.mm(pc.v(pc.t[32 * b:32 * b + 32, :]), qss.v(qss.t[:, g, b, :]), kcmpT_s.v(kcmpT_s.t[:, b, g, :]), signal=(b == 3))
        exact_softmax([(pc[:], 512)], 512, cbs.v(cbs.t[:, g, :]), None)
        kb.op("dve", lambda e: e.tensor_reduce(out=sm_.t[:], in_=E_sb.t[:, 0:512], axis=AX.X, op=ALU.add), [E_sb[:]], [sm_[:]])
        kb.op("dve", lambda e: e.reciprocal(out=sm_.t[:], in_=sm_.t[:]), [sm_[:]], [sm_[:]])
        kb.act(S_sb.v(S_sb.t[:, 0:512]), S_sb.v(S_sb.t[:, 0:512]), AF.Exp, bias=mc_[:], scale=1.0)
        kb.ts("dve", S_sb.v(S_sb.t[:, 0:512]), S_sb.v(S_sb.t[:, 0:512]), sm_.v(sm_.t[:, 0:1]), None, ALU.mult)
        kb.ts("dve", E_sb.v(E_sb.t[:, 0:512]), E_sb.v(E_sb.t[:, 0:512]), sm_.v(sm_.t[:, 0:1]), None, ALU.mult)
        kb.copy("pool", p_hi[:], S_sb.v(S_sb.t[:, 0:512]))
        kb.tt("pool", p_lo[:], S_sb.v(S_sb.t[:, 0:512]), p_hi[:], ALU.subtract)
        pi = psb[1]
        kb.mm(pi[:], rsum_b[:], p_hi[:], start=True, stop=False, signal=False)
        kb.mm(pi[:], rsum_b[:], p_lo[:], start=False, stop=True, signal=True)
        kb.memset("pool", imps[:], 0.0)
        kb.copy("act", S_sb.v(S_sb.t[:, 0:512]), pi[:])
        kb.tt("dve", imps.v(imps.t[:, 0:256]), S_sb.v(S_sb.t[:, 0:512].rearrange("p (j two) -> p j two", two=2)[:, :, 0]),
              S_sb.v(S_sb.t[:, 0:512].rearrange("p (j two) -> p j two", two=2)[:, :, 1]), ALU.add)
        kb.stt("dve", imps.v(imps.t[:, 0:257]), forced[:], 1e4, imps.v(imps.t[:, 0:257]), ALU.mult, ALU.max)
        kb.memset("pool", imps.v(imps.t[:, 257:264]), -1.0)
        kb.op("dve", lambda e: e.max(out=mx8s.t[:], in_=imps.t[:]), [imps[:]], [mx8s[:]])
        kb.op("dve", lambda e: e.match_replace(out=imps2.t[:], in_to_replace=mx8s.t[:], in_values=imps.t[:], imm_value=-2.0),
              [mx8s[:], imps[:]], [imps2[:]])
        kb.op("dve", lambda e: e.max(out=mx8s.t[:], in_=imps2.t[:]), [imps2[:]], [mx8s[:]])
        kb.op("dve", lambda e: e.tensor_reduce(out=thrs.t[:], in_=mx8s.t[:], axis=AX.X, op=ALU.min), [mx8s[:]], [thrs[:]])
        kb.ts("dve", sels[g][:], imps[:], thrs.v(thrs.t[:, 0:1]), None, ALU.is_ge)
        transposeE(4, 512)
        po = psb[5]
        for b in range(4):
            for tl in range(4):
                kb.mm(po.v(po.t[32 * b:32 * b + 32, 0:128]), PTs.v(PTs.t[:, tl, 32 * b:32 * b + 32]), vcmp_s.v(vcmp_s.t[:, b, tl, g, :]),
                      start=(tl == 0), stop=(tl == 3), signal=(b == 3 and tl == 3))
        kb.ts("dve", attn_s.v(attn_s.t[:, g, :]), po.v(po.t[:, 0:128]), grow.v(grow.t[:, g, 0:1]), None, ALU.mult)
        for tl in range(4):
            for b in range(4):
                kb.dma("sp", pg[0].v(pg[0].t[:, b, :]), cwin_in.v(cwin_in.t[b, tl * 128:(tl + 1) * 128, :]))
            src5 = pg[0].t[:].rearrange("p b (g c d) -> p b g c d", g=2, c=2)
            kb.copy("act", pgk[:], pg[0].v(src5[:, :, :, 0, :]))
            kb.copy("dve", vwS.v(vwS.t[:, tl, :, :, 0:128]), pg[0].v(src5[:, :, :, 1, :]))
            pt = psb[2]
            ptv = bview(pt)
            for b in range(4):
                kb.tr(pt.v(ptv[:, b * 128:(b + 1) * 128]), pgk.v(pgk.t[:, b, g, :]), ident_b[:], signal=(b == 3))
            kb.copy("act", kTs.v(kTs.t[:, 0:4, :]), pt.v(ptv[:, 0:512].rearrange("p (a b) -> p a b", a=4)))
            pw = psb[0]
            for b in range(4):
                kb.mm(pw.v(pw.t[32 * b:32 * b + 32, tl * 128:(tl + 1) * 128]), qss.v(qss.t[:, g, b, :]), kTs.v(kTs.t[:, b, :]), signal=(b == 3))
        pn = psb[1]
        for b in range(4):
            kb.mm(pn.v(pn.t[32 * b:32 * b + 32, 0:8]), qss.v(qss.t[:, g, b, :]), knT.v(knT.t[:, 1, b, g, :]), signal=(b == 3))
        exact_softmax([(psb[0][:], 512), (pn.v(pn.t[:, 0:8]), 8)], 520, wbs.v(wbs.t[:, g, :]), wms[:])
        transposeE(5, 520)
        po = psb[5]
        for b in range(4):
            for tl in range(5):
                if tl < 4:
                    rhs = vwS.v(vwS.t[:, tl, b, g, :])
                    lhs = PTs.v(PTs.t[:, tl, 32 * b:32 * b + 32])
                else:
                    rhs = vnew.v(vnew.t[:, 1, b, g, :])
                    lhs = PTs.v(PTs.t[0:8, 4, 32 * b:32 * b + 32])
                kb.mm(po.v(po.t[32 * b:32 * b + 32, 0:129]), lhs, rhs, start=(tl == 0), stop=(tl == 4), signal=(b == 3 and tl == 4))
        kb.ts("dve", cf_[:], po.v(po.t[:, 128:129]), 1e-30, None, ALU.max)
        kb.op("dve", lambda e: e.reciprocal(out=cf_.t[:], in_=cf_.t[:]), [cf_[:]], [cf_[:]])
        kb.tt("dve", cf_[:], cf_[:], grow.v(grow.t[:, g, 2:3]), ALU.mult)
        kb.stt("dve", attn_s.v(attn_s.t[:, g, :]), po.v(po.t[:, 0:128]), cf_.v(cf_.t[:, 0:1]), attn_s.v(attn_s.t[:, g, :]), ALU.mult, ALU.add)
        kb.memset("dve", mrun[g][:], -1e30)
        kb.memset("dve", oacc[g][:], 0.0)

    def online_update(g, ps_list, width, kc_val, mask_v, pv_fn, ntiles):
        S_sb, E_sb, PTs = S_sb2[g], E_sb2[g], PTs2[g]
        c0 = 0
        for (ps_v, w, add_v, is_iota) in ps_list:
            if is_iota:
                kb.stt("dve", S_sb.v(S_sb.t[:, c0:c0 + w]), V(add_v.ap[:, 0:w], add_v.buf), slrow.v(slrow.t[:, g:g + 1]), ps_v, ALU.mult, ALU.add)
            else:
                kb.tt("dve", S_sb.v(S_sb.t[:, c0:c0 + w]), ps_v, add_v, ALU.add)
            c0 += w
        kb.op("dve", lambda e: e.tensor_reduce(out=mc_.t[:], in_=S_sb.t[:, 0:width], axis=AX.X, op=ALU.max), [S_sb[:]], [mc_[:]])
        if kc_val is not None:
            kb.tt("dve", mc_[:], mc_[:], kc_val, ALU.add)
        kb.tt("dve", mn_[:], mc_[:], mrun[g][:], ALU.max)
        kb.tt("dve", al_[:], mrun[g][:], mn_[:], ALU.subtract)
        kb.act(al_[:], al_[:], AF.Exp)
        kb.copy("dve", mrun[g][:], mn_[:])
        if kc_val is not None:
            kb.tt("dve", bi_[:], kc_val, mn_[:], ALU.subtract)
        else:
            kb.ts("dve", bi_[:], mn_[:], -1.0, None, ALU.mult)
        kb.act(E_sb.v(E_sb.t[:, 0:width]), S_sb.v(S_sb.t[:, 0:width]), AF.Exp, bias=bi_[:], scale=1.0)
        kb.tt("pool", E_sb.v(E_sb.t[:, 0:width]), E_sb.v(E_sb.t[:, 0:width]), mask_v, ALU.mult)
        transposeE(ntiles, width, E_sb, PTs)
        po = psb[5]
        pv_fn(po, PTs)
        kb.stt("dve", oacc[g][:], oacc[g][:], al_.v(al_.t[:, 0:1]), po.v(po.t[:, 0:129]), ALU.mult, ALU.add)

    def prep_chunk(c):
        sb0 = 0 if c % 2 == 0 else 6
        for i in range(4):
            lp = 4 * c + i
            buf = pg[lp % 6]
            gather_pages(buf, cache_s, lp)
            src5 = buf.t[:].rearrange("p b (g c d) -> p b g c d", g=2, c=2)
            pgk, kTs, vsS = pgk2[lp % 2], kTs2[lp % 2], vsS2[c % 2]
            kb.copy("act", pgk[:], buf.v(src5[:, :, :, 0, :]))
            kb.copy("dve", vsS.v(vsS.t[:, i, :, :, 0:128]), buf.v(src5[:, :, :, 1, :]))
            for half in range(2):
                pt = psb[2 + half]
                ptv = bview(pt)
                for bb in range(2):
                    for g in range(2):
                        b = half * 2 + bb
                        kb.tr(pt.v(ptv[:, (bb * 2 + g) * 128:(bb * 2 + g + 1) * 128]), pgk.v(pgk.t[:, b, g, :]), ident_b[:], signal=(bb == 1 and g == 1))
                kb.copy("act" if half == 0 else "dve", kTs.v(kTs.t[:, half * 4:half * 4 + 4, :]), pt.v(ptv[:, 0:512].rearrange("p (a b) -> p a b", a=4)))
            for g in range(2):
                for b in range(4):
                    kb.mm(psb[sb0 + g].v(psb[sb0 + g].t[32 * b:32 * b + 32, i * 128:(i + 1) * 128]), qss.v(qss.t[:, g, b, :]), kTs.v(kTs.t[:, b * 2 + g, :]),
                          signal=(b == 3))

    def finish_chunk(c):
        sb0 = 0 if c % 2 == 0 else 6
        for g in range(2):
            mask_c = mask_c2[g]
            kb.ts("dve", kc_[:], slrow.v(slrow.t[:, g:g + 1]), float(512 * c - 16384), stok.v(stok.t[:, g:g + 1]), ALU.mult, ALU.add)
            kb.copy("dve", mask_c.v(mask_c.t[:].rearrange("p (b k) -> p b k", k=64)),
                    sels[g].v(sels[g].t[:, 8 * c:8 * c + 8].unsqueeze(2).to_broadcast([128, 8, 64])))

            def pv_fn(po, PTs, g=g, vsS=vsS2[c % 2]):
                for b in range(4):
                    for i in range(4):
                        kb.mm(po.v(po.t[32 * b:32 * b + 32, 0:129]), PTs.v(PTs.t[:, i, 32 * b:32 * b + 32]), vsS.v(vsS.t[:, i, b, g, :]),
                              start=(i == 0), stop=(i == 3), signal=(b == 3 and i == 3))
            online_update(g, [(psb[sb0 + g][:], 512, iota512[:], True)], 512, kc_[:], mask_c[:], pv_fn, 4)

    prep_chunk(0)
    for c in range(32):
        if c + 1 < 32:
            prep_chunk(c + 1)
        finish_chunk(c)
    for g in range(2):
        pn = psb[g]
        for b in range(4):
            kb.mm(pn.v(pn.t[32 * b:32 * b + 32, 0:8]), qss.v(qss.t[:, g, b, :]), knT.v(knT.t[:, 0, b, g, :]), signal=(b == 3))

        def pv_fn2(po, PTs, g=g):
            for b in range(4):
                kb.mm(po.v(po.t[32 * b:32 * b + 32, 0:129]), PTs.v(PTs.t[0:8, 0, 32 * b:32 * b + 32]), vnew.v(vnew.t[:, 0, b, g, :]),
                      start=True, stop=True, signal=(b == 3))
        online_update(g, [(pn.v(pn.t[:, 0:8]), 8, nbs.v(nbs.t[:, g, :]), False)], 8, None, nms[:], pv_fn2, 1)
        kb.ts("dve", cf_[:], oacc[g].v(oacc[g].t[:, 128:129]), 1e-30, None, ALU.max)
        kb.op("dve", lambda e: e.reciprocal(out=cf_.t[:], in_=cf_.t[:]), [cf_[:]], [cf_[:]])
        kb.tt("dve", cf_[:], cf_[:], grow.v(grow.t[:, g, 1:2]), ALU.mult)
        kb.stt("dve", attn_s.v(attn_s.t[:, g, :]), oacc[g].v(oacc[g].t[:, 0:128]), cf_.v(cf_.t[:, 0:1]), attn_s.v(attn_s.t[:, g, :]), ALU.mult, ALU.add)
    asb = kb.sb("asb", [128, 2, 128], BF16)
    kb.copy("pool", asb[:], attn_s[:])
    aTs = kb.sb("aTs", [128, 2, 128], BF16)
    pt = psb[2]
    ptv = bview(pt)
    for g in range(2):
        kb.tr(pt.v(ptv[:, g * 128:(g + 1) * 128]), asb.v(asb.t[:, g, :]), ident_b[:], signal=(g == 1))
    kb.copy("act", aTs[:], pt.v(ptv[:, 0:256].rearrange("p (a b) -> p a b", a=2)))
    for g in range(2):
        for b in range(4):
            for r in range(4):
                kb.dma("sp", scr_cat.v(scr_cat.t[:, 4 * g + r, 1024 + 8 * b:1024 + 8 * b + 8]),
                       aTs.v(aTs.t[:, g, 32 * b + 8 * r:32 * b + 8 * r + 8]))
    kb.phase_end()
    stage_gate(4)

    TILES = [(i * 128, 128) for i in range(8)] + [(1024, 32)]
    kb.phase_begin()
    acc = kb.sb("acc", [128, 9, D], F32)
    comb = kb.sb("comb", [128, 9, 16], F32)
    kb.phase_begin()
    catT = kb.sb("catT", [128, 16, NTOK], BF16)
    for k in range(16):
        kb.dma("sp", catT.v(catT.t[:, k, :]), scr_cat.v(scr_cat.t[:, k, :]))
    Wo = kb.sb("Wo", [128, 16, D], BF16)
    wo_view = w_out.t.rearrange("(k p) n -> p k n", p=128)
    for k in range(16):
        kb.dma("pool", Wo.v(Wo.t[:, k, :]), w_out.v(wo_view[:, k, :]))
    xs4 = kb.sb("xs4", [128, D], F32)
    for ti, (t0, rows) in enumerate(TILES):
        src = x_own.v(x_own.t[t0:t0 + rows, :]) if ti < 8 else x_s[:, :]
        kb.dma("sp", xs4.v(xs4.t[0:rows, :]), src)
        for cb in range(4):
            pb = psb[cb % 4]
            for k in range(16):
                kb.mm(pb.v(pb.t[0:rows, :]), catT.v(catT.t[:, k, t0:t0 + rows]), Wo.v(Wo.t[:, k, cb * 512:(cb + 1) * 512]),
                      start=(k == 0), stop=(k == 15), signal=(k == 15))
            kb.tt("dve", acc.v(acc.t[0:rows, ti, cb * 512:(cb + 1) * 512]), pb.v(pb.t[0:rows, :]), xs4.v(xs4.t[0:rows, cb * 512:(cb + 1) * 512]), ALU.add)
    kb.phase_end()
    stage_gate(5)
    kb.phase_begin()
    hnT = kb.sb("hnT", [128, 16, NTOK], BF16)
    kb.phase_begin()
    gffn = kb.sb("gffn", [128, D], F32)
    kb.dma("sp", gffn[:], norm_ffn.v(norm_ffn.t[0:1, :].partition_broadcast(128)))
    hn32 = kb.sb("hn32", [128, D], F32)
    junk = kb.sb("junk", [128, D], BF16)
    hn_hi = kb.sb("hn_hi", [128, D], BF16)
    hn_lo = kb.sb("hn_lo", [128, D], BF16)
    loT = kb.sb("loT", [128, 16, 128], BF16)
    wr = kb.sb("wr", [128, 16, 20], F32)
    wr_view = w_router.t.rearrange("(k p) n -> p k n", p=128)
    for k in range(16):
        kb.dma("sp", wr.v(wr.t[:, k, :]), w_router.v(wr_view[:, k, :]))
    whi = kb.sb("whi", [128, 16, 20], BF16)
    wlo = kb.sb("wlo", [128, 16, 20], BF16)
    kb.copy("pool", whi[:], wr[:])
    kb.tt("pool", wlo[:], wr[:], whi[:], ALU.subtract)
    br_t = kb.sb("br_t", [128, 20], F32)
    kb.dma("sp", br_t[:], b_router.v(b_router.t[0:1, :].partition_broadcast(128)))
    lg = kb.sb("lg", [128, 20], F32)
    gmax = kb.sb("gmax", [128, 1], F32)
    goh = kb.sb("goh", [128, 4], F32)
    gex = kb.sb("gex", [128, 4], F32)
    gsum = kb.sb("gsum", [128, 1], F32)
    el = kb.sb("el", [128, 4], F32)
    etmp = kb.sb("etmp", [128, 4, 4], F32)
    e1 = kb.sb("e1", [128, 1], F32)
    e2 = kb.sb("e2", [128, 1], F32)
    mk1 = kb.sb("mk1", [128, 4], F32)
    mk2 = kb.sb("mk2", [128, 4], F32)
    el2 = kb.sb("el2", [128, 4], F32)
    w1 = kb.sb("w1", [128, 1], F32)
    w2 = kb.sb("w2", [128, 1], F32)
    wv = kb.sb("wv", [128, 4], F32)
    for ti, (t0, rows) in enumerate(TILES):
        at = T(acc.t[:, ti, :], "acc_t")
        at.buf = acc.buf
        kb.act(junk.v(junk.t[0:rows, :]), at.v(at.t[0:rows, :]), AF.Square, accum=ssq.v(ssq.t[0:rows, :]))
        kb.act(rstd.v(rstd.t[0:rows, :]), ssq.v(ssq.t[0:rows, :]), AF.Sqrt, bias=eps_t.v(eps_t.t[0:rows, :]), scale=1.0 / D)
        kb.op("dve", lambda e: e.reciprocal(out=rstd.t[0:rows, :], in_=rstd.t[0:rows, :]), [rstd[:]], [rstd[:]])
        kb.stt("dve", hn32.v(hn32.t[0:rows, :]), at.v(at.t[0:rows, :]), rstd.v(rstd.t[0:rows, 0:1]), gffn.v(gffn.t[0:rows, :]), ALU.mult, ALU.mult)
        kb.copy("pool", hn_hi.v(hn_hi.t[0:rows, :]), hn32.v(hn32.t[0:rows, :]))
        kb.tt("pool", hn_lo.v(hn_lo.t[0:rows, :]), hn32.v(hn32.t[0:rows, :]), hn_hi.v(hn_hi.t[0:rows, :]), ALU.subtract)
        transpose16(hn_hi, rows, hnT, t0)
        transpose16(hn_lo, rows, loT, 0)
        pl = psb[4]
        for k in range(16):
            kb.mm(pl.v(pl.t[0:rows, 0:20]), hnT.v(hnT.t[:, k, t0:t0 + rows]), whi.v(whi.t[:, k, :]), start=(k == 0), stop=False, signal=False)
        for k in range(16):
            kb.mm(pl.v(pl.t[0:rows, 0:20]), hnT.v(hnT.t[:, k, t0:t0 + rows]), wlo.v(wlo.t[:, k, :]), start=False, stop=False, signal=False)
        for k in range(16):
            kb.mm(pl.v(pl.t[0:rows, 0:20]), loT.v(loT.t[:, k, 0:rows]), whi.v(whi.t[:, k, :]), start=False, stop=(k == 15), signal=(k == 15))
        R = slice(0, rows)
        kb.tt("dve", lg.v(lg.t[R, :]), pl.v(pl.t[R, 0:20]), br_t.v(br_t.t[R, :]), ALU.add)
        kb.op("dve", lambda e: e.tensor_reduce(out=gmax.t[R, :], in_=lg.t[R, 0:4], axis=AX.X, op=ALU.max), [lg[:]], [gmax[:]])
        kb.ts("dve", goh.v(goh.t[R, :]), lg.v(lg.t[R, 0:4]), gmax.v(gmax.t[R, 0:1]), None, ALU.is_ge)
        kb.ts("dve", gmax.v(gmax.t[R, :]), gmax.v(gmax.t[R, :]), -1.0, None, ALU.mult)
        kb.act(gex.v(gex.t[R, :]), lg.v(lg.t[R, 0:4]), AF.Exp, bias=gmax.v(gmax.t[R, 0:1]), scale=1.0)
        kb.op("dve", lambda e: e.tensor_reduce(out=gsum.t[R, :], in_=gex.t[R, :], axis=AX.X, op=ALU.add), [gex[:]], [gsum[:]])
        kb.op("dve", lambda e: e.reciprocal(out=gsum.t[R, :], in_=gsum.t[R, :]), [gsum[:]], [gsum[:]])
        kb.tt("dve", etmp.v(etmp.t[R]), lg.v(lg.t[R, 4:20].rearrange("p (g i) -> p g i", g=4)),
              goh.v(goh.t[R, :].unsqueeze(2).to_broadcast([rows, 4, 4])), ALU.mult)
        kb.op("dve", lambda e: e.tensor_reduce(out=el.t[R, :], in_=etmp.t[R].rearrange("p g i -> p i g"), axis=AX.X, op=ALU.add), [etmp[:]], [el[:]])
        kb.op("dve", lambda e: e.tensor_reduce(out=e1.t[R, :], in_=el.t[R, :], axis=AX.X, op=ALU.max), [el[:]], [e1[:]])
        kb.ts("dve", mk1.v(mk1.t[R, :]), el.v(el.t[R, :]), e1.v(e1.t[R, 0:1]), None, ALU.is_ge)
        kb.stt("dve", el2.v(el2.t[R, :]), mk1.v(mk1.t[R, :]), -1e30, el.v(el.t[R, :]), ALU.mult, ALU.add)
        kb.op("dve", lambda e: e.tensor_reduce(out=e2.t[R, :], in_=el2.t[R, :], axis=AX.X, op=ALU.max), [el2[:]], [e2[:]])
        kb.ts("dve", mk2.v(mk2.t[R, :]), el2.v(el2.t[R, :]), e2.v(e2.t[R, 0:1]), None, ALU.is_ge)
        kb.tt("dve", w2.v(w2.t[R, :]), e2.v(e2.t[R, :]), e1.v(e1.t[R, :]), ALU.subtract)
        kb.act(w2.v(w2.t[R, :]), w2.v(w2.t[R, :]), AF.Exp)
        kb.ts("dve", w1.v(w1.t[R, :]), w2.v(w2.t[R, :]), 1.0, None, ALU.add)
        kb.op("dve", lambda e: e.reciprocal(out=w1.t[R, :], in_=w1.t[R, :]), [w1[:]], [w1[:]])
        kb.tt("dve", w2.v(w2.t[R, :]), w2.v(w2.t[R, :]), w1.v(w1.t[R, :]), ALU.mult)
        kb.tt("dve", w1.v(w1.t[R, :]), w1.v(w1.t[R, :]), gsum.v(gsum.t[R, :]), ALU.mult)
        kb.tt("dve", w2.v(w2.t[R, :]), w2.v(w2.t[R, :]), gsum.v(gsum.t[R, :]), ALU.mult)
        kb.ts("dve", wv.v(wv.t[R, :]), mk1.v(mk1.t[R, :]), w1.v(w1.t[R, 0:1]), None, ALU.mult)
        kb.stt("dve", wv.v(wv.t[R, :]), mk2.v(mk2.t[R, :]), w2.v(w2.t[R, 0:1]), wv.v(wv.t[R, :]), ALU.mult, ALU.add)
        kb.tt("dve", comb.v(comb.t[R, ti, :].rearrange("p (g i) -> p g i", g=4)),
              goh.v(goh.t[R, :].unsqueeze(2).to_broadcast([rows, 4, 4])), wv.v(wv.t[R, :].unsqueeze(1).to_broadcast([rows, 4, 4])), ALU.mult)
    kb.phase_end()
    stage_gate(6)
    kb.phase_begin()
    wg2 = [kb.sb("wg%d" % i, [128, 16, 512], BF16) for i in range(2)]
    wu2 = [kb.sb("wu%d" % i, [128, 16, 512], BF16) for i in range(2)]
    wd = kb.sb("wd", [128, 4, D], BF16)
    hT = kb.sb("hT", [128, 4, NTOK], BF16)
    sgt = kb.sb("sgt", [128, 512], F32)
    for e_ in range(16):
        wg, wu = wg2[e_ % 2], wu2[e_ % 2]
        wgv = w_gate.t[e_].rearrange("(k p) n -> p k n", p=128)
        wuv = w_up.t[e_].rearrange("(k p) n -> p k n", p=128)
        wdv = w_down.t[e_].rearrange("(k p) n -> p k n", p=128)
        for k in range(16):
            kb.dma("pool", wg.v(wg.t[:, k, :]), w_gate.v(wgv[:, k, :]))
            kb.dma("pool", wu.v(wu.t[:, k, :]), w_up.v(wuv[:, k, :]))
        for k in range(4):
            kb.dma("pool", wd.v(wd.t[:, k, :]), w_down.v(wdv[:, k, :]))
        for fc in range(4):
            for (c0, n) in ((0, 512), (512, 512), (1024, 32)):
                pg, pu = psb[0 + (fc % 2) * 2], psb[1 + (fc % 2) * 2]
                for k in range(16):
                    kb.mm(pg.v(pg.t[:, 0:n]), wg.v(wg.t[:, k, fc * 128:(fc + 1) * 128]), hnT.v(hnT.t[:, k, c0:c0 + n]),
                          start=(k == 0), stop=(k == 15), signal=(k == 15))
                for k in range(16):
                    kb.mm(pu.v(pu.t[:, 0:n]), wu.v(wu.t[:, k, fc * 128:(fc + 1) * 128]), hnT.v(hnT.t[:, k, c0:c0 + n]),
                          start=(k == 0), stop=(k == 15), signal=(k == 15))
                kb.act(sgt.v(sgt.t[:, 0:n]), pg.v(pg.t[:, 0:n]), AF.Silu)
                kb.tt("dve", hT.v(hT.t[:, fc, c0:c0 + n]), sgt.v(sgt.t[:, 0:n]), pu.v(pu.t[:, 0:n]), ALU.mult)
        for ti, (t0, rows) in enumerate(TILES):
            for cb in range(4):
                po = psb[4 + cb % 4]
                for fc in range(4):
                    kb.mm(po.v(po.t[0:rows, :]), hT.v(hT.t[:, fc, t0:t0 + rows]), wd.v(wd.t[:, fc, cb * 512:(cb + 1) * 512]),
                          start=(fc == 0), stop=(fc == 3), signal=(fc == 3))
                dst = acc.v(acc.t[0:rows, ti, cb * 512:(cb + 1) * 512])
                kb.stt("dve", dst, po.v(po.t[0:rows, :]), comb.v(comb.t[0:rows, ti, e_:e_ + 1]), dst, ALU.mult, ALU.add)
    kb.phase_end()
    kb.phase_end()
    stage_gate(7)
    kb.phase_begin()
    gfin = kb.sb("gfin", [128, D], F32)
    kb.dma("sp", gfin[:], norm_final.v(norm_final.t[0:1, :].partition_broadcast(128)))
    yo = kb.sb("yo", [128, D], F32)
    junk2 = kb.sb("junk2", [128, D], BF16)
    for ti, (t0, rows) in enumerate(TILES):
        at = T(acc.t[:, ti, :], "acc_t2")
        at.buf = acc.buf
        kb.act(junk2.v(junk2.t[0:rows, :]), at.v(at.t[0:rows, :]), AF.Square, accum=ssq.v(ssq.t[0:rows, :]))
        kb.act(rstd.v(rstd.t[0:rows, :]), ssq.v(ssq.t[0:rows, :]), AF.Sqrt, bias=eps_t.v(eps_t.t[0:rows, :]), scale=1.0 / D)
        kb.op("dve", lambda e: e.reciprocal(out=rstd.t[0:rows, :], in_=rstd.t[0:rows, :]), [rstd[:]], [rstd[:]])
        kb.stt("dve", yo.v(yo.t[0:rows, :]), at.v(at.t[0:rows, :]), rstd.v(rstd.t[0:rows, 0:1]), gfin.v(gfin.t[0:rows, :]), ALU.mult, ALU.mult)
        kb.dma("sp", o_y.v(o_y.t[t0:t0 + rows, :]), yo.v(yo.t[0:rows, :]))
    kb.phase_end()
    kb.phase_end()


_CACHE = {}


def kernel(**inputs):
    g = lambda k: np.ascontiguousarray(inputs[k], dtype=np.float32)
    x_prompt = g("x_prompt")
    x_sample = g("x_sample")
    w_in = g("w_in")[0]
    if "nc" not in _CACHE:
        _CACHE["nc"] = build_program()
    nc = _CACHE["nc"]
    ident = np.eye(128, dtype=np.float32)
    chan = lambda v: v.reshape(8, 128).T
    rnn_par = np.zeros((128, 8, 8), np.float32)
    cw = g("conv_w")[0]
    for k in range(4):
        rnn_par[:, :, k] = chan(cw[k])
    rnn_par[:, :, 4] = chan(g("conv_b")[0])
    rnn_par[:, :, 5] = chan(g("lru_ba")[0])
    rnn_par[:, :, 6] = chan(g("lru_bx")[0])
    rnn_par[:, :, 7] = chan(g("lru_lambda")[0])

    def bd(w):
        o = np.zeros((128, 8, 128), np.float32)
        for m in range(8):
            o[0:64, m, 0:64] = w[2 * m]
            o[64:128, m, 64:128] = w[2 * m + 1]
        return o
    wa_bd, wx_bd = bd(g("lru_wa")[0]), bd(g("lru_wx")[0])
    wc = g("cmp_pool_w")[0]
    poolk = np.zeros((128, 4), np.float32)
    poolv = np.zeros((128, 252), np.float32)
    for t in range(128):
        poolk[t, t // 32] = wc[t % 32, 0]
        poolv[t, 124 + t // 32] = wc[t % 32, 1]
    ii = np.arange(128, dtype=np.float32)[:, None]
    cc = np.arange(640, dtype=np.float32)[None, :]
    distw = 512.0 + ii - cc
    negdw = np.minimum(-distw, 0.0).astype(np.float32)
    bandw = ((distw >= 0) & (distw <= 512)).astype(np.float32)
    w_router = np.ascontiguousarray(np.concatenate([g("w_router_group")[0], g("w_router_expert")[0]], axis=1))
    b_router = np.ascontiguousarray(np.concatenate([g("b_router_group")[0], g("b_router_expert")[0]], axis=0)[None, :])
    shared = {
        "w_in": w_in, "norm_mix": g("norm_mix"), "ident": ident,
        "rnn_par": rnn_par, "wa_bd": wa_bd, "wx_bd": wx_bd, "poolk": poolk, "poolv": poolv,
        "iota4096": np.arange(4096, dtype=np.float32)[None, :],
        "iota_ce": (np.arange(128, dtype=np.float32) * 32 + 31)[None, :],
        "iota_blk": np.arange(64, dtype=np.float32)[None, :],
        "negdw": negdw, "bandw": bandw,
        "w_out": g("w_out")[0], "norm_ffn": g("norm_ffn"), "norm_final": g("norm_final")[None, :],
        "w_router": w_router, "b_router": b_router,
        "w_gate": g("w_exp_gate")[0], "w_up": g("w_exp_up")[0], "w_down": g("w_exp_down")[0],
    }
    rows = np.arange(128)
    rb, rr, rt = rows // 32, (rows // 8) % 4, rows % 8
    sl = np.stack([2.0 ** (-(4 * gg + rr + 1)) for gg in range(2)], 1).astype(np.float64)
    jj = np.arange(512)
    cb_s = (-sl[:, :, None] * (16384 + rt[:, None, None] - (32 * jj[None, None, :] + 31))).astype(np.float32)
    cwi = np.arange(520)
    distw_s = np.where(cwi[None, :] < 512, 512 + rt[:, None] - cwi[None, :], rt[:, None] - (cwi[None, :] - 512))
    wm_s = ((distw_s >= 0) & (distw_s <= 512)).astype(np.float32)
    wb_s = (-sl[:, :, None] * np.maximum(distw_s, 0)[:, None, :]).astype(np.float32)
    nn = np.arange(8)
    nm_s = (nn[None, :] <= rt[:, None]).astype(np.float32)
    nb_s = (-sl[:, :, None] * np.maximum(rt[:, None] - nn[None, :], 0)[:, None, :]).astype(np.float32)
    rsum = ((rb[:, None] == rb[None, :]) & (rt[:, None] == rt[None, :])).astype(np.float32)
    forced_s = np.zeros((1, 257), np.float32)
    forced_s[0, [0, 255, 256]] = 1.0
    shared.update({
        "cache_c": np.asarray(inputs["cache_cmp_kv"], dtype=np.float32).reshape(5120 * 128, 512),
        "cache_s": np.asarray(inputs["cache_sel_kv"], dtype=np.float32).reshape(5120 * 128, 512),
        "pidx": np.arange(128, dtype=np.float32)[:, None],
        "cb_s": cb_s, "slope_rows": sl.astype(np.float32), "stok": (-sl * rt[:, None]).astype(np.float32),
        "wb_s": wb_s, "wm_s": wm_s, "nb_s": nb_s, "nm_s": nm_s, "rsum": rsum, "forced_s": forced_s,
        "iota512": np.arange(512, dtype=np.float32)[None, :],
    })
    page_table = np.asarray(inputs["page_table"], dtype=np.int32)
    in_maps = []
    for c in range(8):
        b, j = c // 4, c % 4
        tpos = (1024 * j + 128 * np.arange(8, dtype=np.float32)[None, :] + ii).astype(np.float32)
        m = dict(shared)
        m.update({
            "x_full": x_prompt[b],
            "x_own": np.ascontiguousarray(x_prompt[b, 1024 * j:1024 * (j + 1)]),
            "x_s": np.ascontiguousarray(x_sample[4 * c:4 * c + 4].reshape(32, D)),
            "onehot": np.tile(np.eye(4, dtype=np.float32)[j][None, :], (128, 1)),
            "st_h": np.ascontiguousarray(g("state_h")[0, 4 * c:4 * c + 4].reshape(4, 8, 128).transpose(2, 0, 1)),
            "st_conv": np.ascontiguousarray(g("state_conv")[0, 4 * c:4 * c + 4].reshape(4, 3, 8, 128).transpose(3, 0, 2, 1)),
            "cwin": np.ascontiguousarray(g("cache_win_kv")[0, 4 * c:4 * c + 4].reshape(4, 512, 512)),
            "tpos": tpos, "curb": np.floor(tpos / 64.0).astype(np.float32),
            "pt": np.ascontiguousarray(page_table[4 * c:4 * c + 4].reshape(1, 512)),
        })
        in_maps.append(m)
    res = run_bass_kernel_spmd(nc, in_maps, core_ids=list(range(8)))
    r = res.results
    kvp = np.stack([r[0]["o_kv_p"], r[4]["o_kv_p"]], 0)
    kvs = np.concatenate([r[c]["o_kv_s"] for c in range(8)], 0).reshape(32, 8, 1536)
    f = lambda a, i: np.ascontiguousarray(a[..., i * 512:(i + 1) * 512]).reshape(a.shape[:-1] + (2, 2, 128))[None]
    y_prompt = np.concatenate([r[c]["o_y"][0:1024] for c in range(8)], 0).reshape(2, 4096, 2048)
    y_sample = np.concatenate([r[c]["o_y"][1024:1056] for c in range(8)], 0).reshape(32, 8, 2048)
    new_cmp_p, new_sel_p = f(kvp, 0), f(kvp, 1)
    new_win_p = np.ascontiguousarray(f(kvp, 2)[:, :, -512:])
    new_cmp_s, new_sel_s = f(kvs, 0), f(kvs, 1)
    new_win_s = np.concatenate([r[c]["o_win_s"] for c in range(8)], 0).reshape(1, 32, 512, 2, 2, 128)
    new_conv_p = np.stack([r[4 * b]["o_conv_p"].transpose(2, 1, 0).reshape(3, 1024) for b in range(2)], 0)[None]
    new_conv_s = np.concatenate([r[c]["o_conv_s"].transpose(1, 3, 2, 0).reshape(4, 3, 1024) for c in range(8)], 0)[None]
    new_h_p = np.stack([r[4 * b]["o_h_p"].T.reshape(1024) for b in range(2)], 0)[None]
    new_h_s = np.concatenate([r[c]["o_h_s"].transpose(1, 2, 0).reshape(4, 1024) for c in range(8)], 0)[None]
    return (y_prompt, y_sample, new_cmp_p, new_cmp_s, new_sel_p, new_sel_s, new_win_p, new_win_s,
            new_conv_p, new_conv_s, new_h_p, new_h_s)
```

```python
import numpy as np
import concourse.bass as bass
import concourse.mybir as mybir
from concourse.bass_utils import run_bass_kernel_spmd
from contextlib import ExitStack

F32 = mybir.dt.float32
BF16 = mybir.dt.bfloat16
I32 = mybir.dt.int32
AF = mybir.ActivationFunctionType
ALU = mybir.AluOpType
AX = mybir.AxisListType

D = 2048
NQ = 1024
KVW = 512
D_IN = 4632
C_Q, C_KC, C_KS, C_KW, C_GT, C_XR, C_XG = 0, 1024, 1536, 2048, 2560, 2584, 3608
SEQ = 4096
EPS = 1e-6


class Buf:
    def __init__(self, name):
        self.name = name
        self.w = None
        self.r = {}


class V:
    def __init__(self, ap, buf):
        self.ap = ap
        self.buf = buf


class T:
    def __init__(self, t, name):
        self.t = t
        self.buf = Buf(name)

    def __getitem__(self, idx):
        return V(self.t[idx], self.buf)

    def v(self, ap):
        return V(ap, self.buf)


class Eng:
    def __init__(self, kb, name, e, sem):
        self.kb = kb
        self.name = name
        self.e = e
        self.sem = sem
        self.count = 0
        self.waited = {}
        self.pend_r = []
        self.pend_w = []

    def wait(self, tok):
        if tok is None:
            return
        sem, val, owner = tok
        if owner is self and self.name == "pe":
            return
        key = id(sem)
        if self.waited.get(key, 0) >= val:
            return
        self.e.wait_ge(sem, val)
        self.waited[key] = val


class KB:
    def __init__(self, nc, es):
        self.nc = nc
        self.es = es
        self.eng = {}
        for name, e in (("pe", nc.tensor), ("act", nc.scalar), ("dve", nc.vector), ("pool", nc.gpsimd), ("sp", nc.sync)):
            sem = es.enter_context(nc.semaphore("sem_" + name))
            self.eng[name] = Eng(self, name, e, sem)
        self.dma_sems = []
        for i in range(24):
            self.dma_sems.append([es.enter_context(nc.semaphore("dsem%d" % i)), 0, None])
        self.dma_rr = 0
        self.all_bufs = []
        self.stack = [es]
        self.ntile = 0

    def sb(self, name, shape, dt, glob=False):
        if glob or len(self.stack) == 1:
            t = self.stack[0].enter_context(self.nc.sbuf_tensor(name, shape, dt, side="right"))
        else:
            t = self.stack[-1].enter_context(self.nc.sbuf_tensor(name, shape, dt))
        T_ = T(t, name)
        self.all_bufs.append(T_.buf)
        return T_

    def phase_begin(self):
        es = ExitStack()
        es.__enter__()
        self.stack.append(es)

    def phase_end(self):
        self.barrier()
        es = self.stack.pop()
        es.__exit__(None, None, None)

    def barrier(self):
        toks = []
        for F in self.eng.values():
            if F.count > 0:
                toks.append((F.sem, F.count, F))
        for slot in self.dma_sems:
            if slot[2] is not None:
                toks.append(slot[2])
        for E in self.eng.values():
            for tok in toks:
                if tok[2] is E:
                    continue
                E.wait(tok)

    def ps(self, name, shape, dt=F32):
        t = self.es.enter_context(self.nc.psum_tensor(name, shape, dt))
        T_ = T(t, name)
        self.all_bufs.append(T_.buf)
        return T_

    def dram(self, name, shape, dt, kind="Internal"):
        t = self.nc.dram_tensor(name, shape, dt, kind=kind)
        T_ = T(t.ap(), name)
        self.all_bufs.append(T_.buf)
        return T_

    def _pre(self, E, reads, writes):
        for b in reads:
            E.wait(b.w)
        for b in writes:
            E.wait(b.w)
            for tok in b.r.values():
                E.wait(tok)

    def _post(self, tok, reads, writes):
        for b in reads:
            b.r[id(tok[0])] = tok
        for b in writes:
            b.w = tok
            b.r = {}

    def op(self, eng, fn, reads, writes, signal=True):
        E = self.eng[eng]
        rb = [v.buf for v in reads if v is not None]
        wb = [v.buf for v in writes if v is not None]
        self._pre(E, rb, wb)
        ins = fn(E.e)
        if signal:
            E.count += 1
            ins.then_inc(E.sem, 1)
            tok = (E.sem, E.count, E)
            self._post(tok, rb + E.pend_r, wb + E.pend_w)
            E.pend_r = []
            E.pend_w = []
        else:
            E.pend_r += rb
            E.pend_w += wb
        return ins

    def dma(self, q, out, in_, fn=None):
        E = self.eng[q]
        rb, wb = [in_.buf], [out.buf]
        self._pre(E, rb, wb)
        slot = self.dma_sems[self.dma_rr % len(self.dma_sems)]
        self.dma_rr += 1
        if slot[2] is not None:
            E.wait(slot[2])
        if fn is None:
            ins = E.e.dma_start(out=out.ap, in_=in_.ap)
        else:
            ins = fn(E.e)
        slot[1] += 16
        ins.then_inc(slot[0], 16)
        tok = (slot[0], slot[1], None)
        slot[2] = tok
        self._post(tok, rb, wb)
        return tok

    def finish(self):
        E = self.eng["sp"]
        for b in self.all_bufs:
            E.wait(b.w)
            for tok in b.r.values():
                E.wait(tok)
        for slot in self.dma_sems:
            if slot[2] is not None:
                E.wait(slot[2])

    def mm(self, out, lhsT, rhs, start=True, stop=True, signal=True):
        kw = {}
        try:
            out.ap.base_partition()
        except AssertionError:
            kw["tile_position"] = (0, 96)
        return self.op("pe", lambda e: e.matmul(out.ap, lhsT.ap, rhs.ap, start=start, stop=stop,
                                                 skip_group_check=True, **kw),
                       [lhsT, rhs], [out], signal=signal)

    def tr(self, out, in_, ident, signal=True):
        return self.op("pe", lambda e: e.transpose(out.ap, in_.ap, ident.ap), [in_, ident], [out], signal=signal)

    def act(self, out, in_, func, bias=None, scale=None, accum=None, eng="act"):
        kw = {}
        rd = [in_]
        if bias is not None:
            if isinstance(bias, V):
                kw["bias"] = bias.ap
                rd.append(bias)
            else:
                kw["bias"] = bias
        if scale is not None:
            if isinstance(scale, V):
                kw["scale"] = scale.ap
                rd.append(scale)
            else:
                kw["scale"] = scale
        wr = [out]
        if accum is not None:
            kw["accum_out"] = accum.ap
            wr.append(accum)
        return self.op(eng, lambda e: e.activation(out.ap, in_.ap, func, **kw), rd, wr)

    def copy(self, eng, out, in_):
        if eng == "act":
            return self.op("act", lambda e: e.copy(out.ap, in_.ap), [in_], [out])
        return self.op(eng, lambda e: e.tensor_copy(out=out.ap, in_=in_.ap), [in_], [out])

    def memset(self, eng, out, val):
        return self.op(eng, lambda e: e.memset(out.ap, val), [], [out])

    def tt(self, eng, out, a, b, op):
        return self.op(eng, lambda e: e.tensor_tensor(out=out.ap, in0=a.ap, in1=b.ap, op=op), [a, b], [out])

    def ts(self, eng, out, a, s1, s2, op0, op1=None, accum=None):
        rd = [a]
        x1 = s1.ap if isinstance(s1, V) else s1
        x2 = s2.ap if isinstance(s2, V) else s2
        if isinstance(s1, V):
            rd.append(s1)
        if isinstance(s2, V):
            rd.append(s2)
        kw = {}
        wr = [out]
        if op1 is not None:
            kw["op1"] = op1
        if accum is not None:
            kw["accum_out"] = accum.ap
            wr.append(accum)
        return self.op(eng, lambda e: e.tensor_scalar(out=out.ap, in0=a.ap, scalar1=x1, scalar2=x2, op0=op0, **kw),
                       rd, wr)

    def stt(self, eng, out, a, s, b, op0, op1):
        rd = [a, b]
        x = s.ap if isinstance(s, V) else s
        if isinstance(s, V):
            rd.append(s)
        return self.op(eng, lambda e: e.scalar_tensor_tensor(out=out.ap, in0=a.ap, scalar=x, in1=b.ap, op0=op0, op1=op1),
                       rd, [out])


def build_program():
    nc = bass.Bass("TRN2", target_bir_lowering=False)
    es = ExitStack()
    with es:
        kb = KB(nc, es)
        try:
            _build(nc, kb)
        except StopBuild:
            while len(kb.stack) > 1:
                kb.phase_end()
        kb.finish()
    return nc


class StopBuild(Exception):
    pass


import os
STAGE = int(os.environ.get("KSTAGE", "99"))


def stage_gate(n):
    if STAGE < n:
        raise StopBuild()


SLOPES = [2.0 ** (-(h + 1)) for h in range(8)]
NTOK = 1056


def _build(nc, kb):
    dram_in = lambda name, shape, dt=F32: kb.dram(name, shape, dt, kind="ExternalInput")
    dram_out = lambda name, shape, dt=F32: kb.dram(name, shape, dt, kind="ExternalOutput")
    x_full = dram_in("x_full", [SEQ, D])
    x_own = dram_in("x_own", [1024, D])
    x_s = dram_in("x_s", [32, D])
    w_in = dram_in("w_in", [D, D_IN])
    norm_mix = dram_in("norm_mix", [1, D])
    ident_in = dram_in("ident", [128, 128])
    rnn_par_in = dram_in("rnn_par", [128, 8, 8])
    wa_bd_in = dram_in("wa_bd", [128, 8, 128])
    wx_bd_in = dram_in("wx_bd", [128, 8, 128])
    onehot_in = dram_in("onehot", [128, 4])
    st_h_in = dram_in("st_h", [128, 4, 8])
    st_conv_in = dram_in("st_conv", [128, 4, 8, 3])
    cwin_in = dram_in("cwin", [4, 512, 512])
    poolk_in = dram_in("poolk", [128, 4])
    poolv_in = dram_in("poolv", [128, 252])
    tpos_in = dram_in("tpos", [128, 8])
    curb_in = dram_in("curb", [128, 8])
    iota_in = dram_in("iota4096", [1, 4096])
    iotace_in = dram_in("iota_ce", [1, 128])
    iotablk_in = dram_in("iota_blk", [1, 64])
    negdw_in = dram_in("negdw", [128, 640])
    bandw_in = dram_in("bandw", [128, 640])
    w_out = dram_in("w_out", [D, D])
    norm_ffn = dram_in("norm_ffn", [1, D])
    norm_final = dram_in("norm_final", [1, D])
    w_router = dram_in("w_router", [D, 20])
    b_router = dram_in("b_router", [1, 20])
    w_gate = dram_in("w_gate", [16, D, 512])
    w_up = dram_in("w_up", [16, D, 512])
    w_down = dram_in("w_down", [16, 512, D])
    cache_c = dram_in("cache_c", [5120 * 128, 512])
    cache_s = dram_in("cache_s", [5120 * 128, 512])
    pt_in = dram_in("pt", [1, 512], I32)
    pidx_in = dram_in("pidx", [128, 1])
    cbs_in = dram_in("cb_s", [128, 2, 512])
    slrow_in = dram_in("slope_rows", [128, 2])
    stok_in = dram_in("stok", [128, 2])
    wbs_in = dram_in("wb_s", [128, 2, 520])
    wms_in = dram_in("wm_s", [128, 520])
    nbs_in = dram_in("nb_s", [128, 2, 8])
    nms_in = dram_in("nm_s", [128, 8])
    rsum_in = dram_in("rsum", [128, 128])
    forced_in = dram_in("forced_s", [1, 257])
    iota512_in = dram_in("iota512", [1, 512])
    o_h_p = dram_out("o_h_p", [128, 8])
    o_h_s = dram_out("o_h_s", [128, 4, 8])
    o_conv_s = dram_out("o_conv_s", [128, 4, 8, 3])
    o_win_s = dram_out("o_win_s", [4, 512, 512])
    o_conv_p = dram_out("o_conv_p", [128, 8, 3])
    o_kv_p = dram_out("o_kv_p", [SEQ, 3 * KVW])
    o_kv_s = dram_out("o_kv_s", [32, 3 * KVW])
    o_y = dram_out("o_y", [NTOK, D])
    scr_kT = kb.dram("scr_kT", [2, 2, 128, SEQ], BF16)
    scr_v = kb.dram("scr_v", [2, 32, 128, 2, 128], BF16)
    scr_q = kb.dram("scr_q", [128, 8, NTOK], BF16)
    scr_cat = kb.dram("scr_cat", [128, 16, NTOK], BF16)
    scr_kvs = kb.dram("scr_kvs", [32, 3 * KVW], BF16)
    scr_g = kb.dram("scr_g", [32, 24], F32)

    ident_f = kb.sb("ident_f", [128, 128], F32)
    ident_b = kb.sb("ident_b", [128, 128], BF16)
    kb.dma("sp", ident_f[:], ident_in[:])
    kb.copy("dve", ident_b[:], ident_f[:])
    eps_t = kb.sb("eps_t", [128, 1], F32)
    kb.memset("dve", eps_t[:], EPS)
    one_t = kb.sb("one_t", [128, 1], F32)
    kb.memset("dve", one_t[:], 1.0)
    onehot = kb.sb("onehot_sb", [128, 4], F32)
    kb.dma("sp", onehot[:], onehot_in[:])
    gates_sb = kb.sb("gates_sb", [128, 9, 24], F32)
    h_s_all = kb.sb("h_s_all", [128, 8, 32], F32)
    kcmpT_sb = kb.sb("kcmpT_sb", [128, 2, 128], BF16)
    vcmp_sb = kb.sb("vcmp_sb", [128, 2, 128], BF16)
    ssq = kb.sb("ssq", [128, 1], F32)
    rstd = kb.sb("rstd", [128, 1], F32)
    psb = [kb.ps("psb%d" % i, [128, 512], F32) for i in range(8)]

    def bview(pb):
        return pb.t[:].bitcast(BF16)

    kb.phase_begin()
    gmix = kb.sb("gmix", [128, D], F32)
    kb.dma("sp", gmix[:], norm_mix.v(norm_mix.t[0:1, :].partition_broadcast(128)))
    xs = kb.sb("xs", [128, D], F32)
    xn = kb.sb("xn", [128, D], BF16)
    xnT = kb.sb("xnT", [128, 16, 512], BF16)
    xnT_b = kb.sb("xnT_b", [128, 16, 512], BF16)
    xnT2 = [xnT, xnT_b]
    own_h = kb.sb("own_h", [128, 8, 1024], BF16)
    kb.memset("pool", own_h[:], 0.0)

    def rms_rows(src_tile, nrows, gam, dst_bf, junk):
        kb.act(junk.v(junk.t[0:nrows, :]), src_tile.v(src_tile.t[0:nrows, :]), AF.Square, accum=ssq.v(ssq.t[0:nrows, :]))
        kb.act(rstd.v(rstd.t[0:nrows, :]), ssq.v(ssq.t[0:nrows, :]), AF.Sqrt, bias=eps_t.v(eps_t.t[0:nrows, :]), scale=1.0 / D)
        kb.op("dve", lambda e: e.reciprocal(out=rstd.t[0:nrows, :], in_=rstd.t[0:nrows, :]), [rstd[:]], [rstd[:]])
        kb.stt("dve", dst_bf.v(dst_bf.t[0:nrows, :]), src_tile.v(src_tile.t[0:nrows, :]), rstd.v(rstd.t[0:nrows, 0:1]),
               gam.v(gam.t[0:nrows, :]), ALU.mult, ALU.mult)

    def transpose16(src_bf, nrows, dstT, col0):
        for q4 in range(4):
            pb = psb[q4]
            pbv = bview(pb)
            for kk in range(4):
                k = q4 * 4 + kk
                kb.tr(pb.v(pbv[:, kk * 128: kk * 128 + nrows]), src_bf.v(src_bf.t[0:nrows, k * 128:(k + 1) * 128]),
                      ident_b.v(ident_b.t[0:nrows, 0:nrows]), signal=(kk == 3))
            src = pbv[:, 0:512].rearrange("p (a b) -> p a b", a=4)[:, :, 0:nrows]
            dst = dstT.t[:, q4 * 4:(q4 + 1) * 4, col0:col0 + nrows]
            kb.copy("act" if q4 % 2 == 0 else "dve", dstT.v(dst), pb.v(src))

    def norm_transpose(src_rows, nrows, dstT, col0):
        kb.dma("sp", xs.v(xs.t[0:nrows, :]), src_rows)
        rms_rows(xs, nrows, gmix, xn, xn)
        transpose16(xn, nrows, dstT, col0)

    w_view = w_in.t.rearrange("(k p) n -> p k n", p=128)

    kb.phase_begin()
    W = kb.sb("W", [128, 16, 2560], BF16)
    for k in range(16):
        kb.dma("pool", W.v(W.t[:, k, 0:1536]), w_in.v(w_view[:, k, C_KC:C_KC + 1536]))
        kb.dma("pool", W.v(W.t[:, k, 1536:2560]), w_in.v(w_view[:, k, C_XR:C_XR + 1024]))
    kvf = kb.sb("kvf", [128, 3 * KVW], F32)
    kvb = kb.sb("kvb", [128, 3 * KVW], BF16)
    kTt = kb.sb("kTt", [128, 4, 128], BF16)
    rpar = kb.sb("rpar", [128, 8, 8], F32)
    kb.dma("sp", rpar[:], rnn_par_in[:])
    wa_bd = kb.sb("wa_bd_sb", [128, 8, 128], BF16)
    wx_bd = kb.sb("wx_bd_sb", [128, 8, 128], BF16)
    kb.dma("pool", wa_bd[:], wa_bd_in[:])
    kb.dma("pool", wx_bd[:], wx_bd_in[:])
    poolk = kb.sb("poolk_sb", [128, 4], BF16)
    poolv = kb.sb("poolv_sb", [128, 252], BF16)
    kb.dma("pool", poolk[:], poolk_in[:])
    kb.dma("pool", poolv[:], poolv_in[:])
    cdec = kb.sb("cdec", [128, 8], F32)
    kb.act(cdec[:], rpar.v(rpar.t[:, :, 7]), AF.Exp, scale=-1.0)
    kb.act(cdec[:], cdec[:], AF.Ln, bias=one_t[:], scale=1.0)
    kb.ts("dve", cdec[:], cdec[:], -8.0, None, ALU.mult)
    xp = [kb.sb("xp%d" % m, [128, 515], F32) for m in range(8)]
    hprev = kb.sb("hprev", [128, 8], F32)
    kb.memset("dve", hprev[:], 0.0)
    for m in range(8):
        kb.memset("pool", xp[m][:, 0:3], 0.0)
    r_xc = kb.sb("r_xc", [128, 512], F32)
    r_xcb = kb.sb("r_xcb", [128, 512], BF16)
    r_r = kb.sb("r_r", [128, 512], F32)
    r_i = kb.sb("r_i", [128, 512], F32)
    r_s = kb.sb("r_s", [128, 512], F32)
    r_u = kb.sb("r_u", [128, 512], F32)
    r_h = kb.sb("r_h", [128, 512], F32)
    kb.memset("dve", psb[7][:], 0.0)

    def kv_tile(xT, col0, nrows, out_rows, tile_idx):
        for blk in range(3):
            pb = psb[4 + (blk % 2)]
            for k in range(16):
                kb.mm(pb.v(pb.t[0:nrows, :]), xT.v(xT.t[:, k, col0:col0 + nrows]), W.v(W.t[:, k, blk * 512:(blk + 1) * 512]),
                      start=(k == 0), stop=(k == 15), signal=(k == 15))
            kb.copy("act" if blk % 2 == 0 else "dve", kvf.v(kvf.t[0:nrows, blk * 512:(blk + 1) * 512]), pb.v(pb.t[0:nrows, :]))
        kb.dma("sp", out_rows, kvf.v(kvf.t[0:nrows, :]))
        if tile_idx is None:
            return
        kb.copy("pool", kvb[:], kvf[:])
        for br in range(2):
            src = kvb.t[:, 512 * (br + 1):512 * (br + 2)].rearrange("p (g c d) -> p g c d", g=2, c=2)[:, :, 1, :]
            kb.dma("sp", scr_v.v(scr_v.t[br, tile_idx]), kvb.v(src))
        pb = psb[4]
        pbv = bview(pb)
        for br in range(2):
            for g in range(2):
                c = 512 * (br + 1) + g * 256
                kb.tr(pb.v(pbv[:, (br * 2 + g) * 128:(br * 2 + g + 1) * 128]), kvb.v(kvb.t[:, c:c + 128]), ident_b[:],
                      signal=(br == 1 and g == 1))
        kb.copy("act", kTt[:], pb.v(pbv[:, 0:512].rearrange("p (a b) -> p a b", a=4)))
        dst = scr_kT.t[:, :, :, tile_idx * 128:(tile_idx + 1) * 128].rearrange("b g d t -> d (b g) t")
        kb.dma("sp", scr_kT.v(dst), kTt[:])
        p7 = psb[7]
        for g in range(2):
            kb.mm(p7.v(p7.t[:, g * 128 + 4 * tile_idx: g * 128 + 4 * tile_idx + 4]), kvb.v(kvb.t[:, g * 256:g * 256 + 128]), poolk[:],
                  start=False, stop=False, signal=False)
        vsrc = kvb.t[:, 0:512].rearrange("p (g c d) -> p g c d", g=2, c=2)[:, :, 1, :]
        kb.mm(p7.v(p7.t[:, 256:512].rearrange("p (g d) -> p g d", g=2)), poolv.v(poolv.t[:, 124 - 4 * tile_idx:252 - 4 * tile_idx]), kvb.v(vsrc),
              start=False, stop=False, signal=True)

    def rnn_group(xT, n, hinit, xpl, sel_dst, c0=0):
        for m in range(8):
            pb = psb[6]
            for k in range(16):
                kb.mm(pb.v(pb.t[:, 0:n]), W.v(W.t[:, k, 1536 + m * 128:1536 + (m + 1) * 128]), xT.v(xT.t[:, k, c0:c0 + n]),
                      start=(k == 0), stop=(k == 15), signal=(k == 15))
            xpm = xpl[m]
            kb.copy("act", xpm.v(xpm.t[:, 3:3 + n]), pb.v(pb.t[:, 0:n]))
            kb.ts("dve", r_xc.v(r_xc.t[:, 0:n]), xpm.v(xpm.t[:, 0:n]), rpar.v(rpar.t[:, m, 0:1]), rpar.v(rpar.t[:, m, 4:5]), ALU.mult, ALU.add)
            for kk in range(1, 4):
                kb.stt("dve", r_xc.v(r_xc.t[:, 0:n]), xpm.v(xpm.t[:, kk:kk + n]), rpar.v(rpar.t[:, m, kk:kk + 1]), r_xc.v(r_xc.t[:, 0:n]), ALU.mult, ALU.add)
            kb.copy("pool", xpm.v(xpm.t[:, 0:3]), xpm.v(xpm.t[:, n:n + 3]))
            kb.copy("pool", r_xcb.v(r_xcb.t[:, 0:n]), r_xc.v(r_xc.t[:, 0:n]))
            pa = psb[4]
            kb.mm(pa.v(pa.t[:, 0:n]), wa_bd.v(wa_bd.t[:, m, :]), r_xcb.v(r_xcb.t[:, 0:n]))
            kb.act(r_r.v(r_r.t[:, 0:n]), pa.v(pa.t[:, 0:n]), AF.Sigmoid, bias=rpar.v(rpar.t[:, m, 5:6]), scale=1.0)
            px = psb[5]
            kb.mm(px.v(px.t[:, 0:n]), wx_bd.v(wx_bd.t[:, m, :]), r_xcb.v(r_xcb.t[:, 0:n]))
            kb.act(r_i.v(r_i.t[:, 0:n]), px.v(px.t[:, 0:n]), AF.Sigmoid, bias=rpar.v(rpar.t[:, m, 6:7]), scale=1.0)
            kb.act(r_r.v(r_r.t[:, 0:n]), r_r.v(r_r.t[:, 0:n]), AF.Exp, scale=cdec.v(cdec.t[:, m:m + 1]))
            kb.tt("pool", r_s.v(r_s.t[:, 0:n]), r_r.v(r_r.t[:, 0:n]), r_r.v(r_r.t[:, 0:n]), ALU.mult)
            kb.act(r_s.v(r_s.t[:, 0:n]), r_s.v(r_s.t[:, 0:n]), AF.Sqrt, bias=one_t[:], scale=-1.0)
            kb.tt("pool", r_u.v(r_u.t[:, 0:n]), r_i.v(r_i.t[:, 0:n]), r_xc.v(r_xc.t[:, 0:n]), ALU.mult)
            kb.tt("pool", r_u.v(r_u.t[:, 0:n]), r_u.v(r_u.t[:, 0:n]), r_s.v(r_s.t[:, 0:n]), ALU.mult)
            kb.op("dve", lambda e: e.tensor_tensor_scan(out=r_h.t[:, 0:n], data0=r_r.t[:, 0:n], data1=r_u.t[:, 0:n],
                                                         initial=hinit.t[:, m:m + 1], op0=ALU.mult, op1=ALU.add),
                  [r_r[:], r_u[:], hinit[:]], [r_h[:]])
            kb.copy("dve", hinit.v(hinit.t[:, m:m + 1]), r_h.v(r_h.t[:, n - 1:n]))
            sel_dst(m, r_h)

    def own_sel(G):
        def f(m, rh):
            dst = own_h.v(own_h.t[:, m, (G % 2) * 512:(G % 2) * 512 + 512])
            kb.stt("dve", dst, rh[:], onehot.v(onehot.t[:, G // 2:G // 2 + 1]), dst, ALU.mult, ALU.add)
        return f

    def front(G):
        xt = xnT2[G % 2]
        for i in range(4):
            r0 = G * 512 + i * 128
            norm_transpose(x_full.v(x_full.t[r0:r0 + 128, :]), 128, xt, i * 128)
        for i in range(4):
            r0 = G * 512 + i * 128
            kv_tile(xt, i * 128, 128, o_kv_p.v(o_kv_p.t[r0:r0 + 128, :]), G * 4 + i)

    front(0)
    for G in range(8):
        if G + 1 < 8:
            front(G + 1)
        rnn_group(xnT2[G % 2], 512, hprev, xp, own_sel(G))
    kb.dma("sp", o_h_p[:], hprev[:])
    for m in range(8):
        kb.dma("sp", o_conv_p.v(o_conv_p.t[:, m, :]), xp[m].v(xp[m].t[:, 0:3]))
    kb.copy("act", kcmpT_sb[:], psb[7].v(psb[7].t[:, 0:256].rearrange("p (g b) -> p g b", g=2)))
    kb.copy("dve", vcmp_sb[:], psb[7].v(psb[7].t[:, 256:512].rearrange("p (g d) -> p g d", g=2)))
    norm_transpose(x_s[:, :], 32, xnT, 0)
    kv_tile(xnT, 0, 32, o_kv_s[:, :], None)
    kb.copy("pool", kvb.v(kvb.t[0:32, :]), kvf.v(kvf.t[0:32, :]))
    kb.dma("sp", scr_kvs[:], kvb.v(kvb.t[0:32, :]))
    for q in range(4):
        kb.dma("sp", o_win_s.v(o_win_s.t[q, 0:504, :]), cwin_in.v(cwin_in.t[q, 8:512, :]))
        kb.dma("sp", o_win_s.v(o_win_s.t[q, 504:512, :]), kvf.v(kvf.t[8 * q:8 * q + 8, 1024:1536]))
    hs = kb.sb("hs", [128, 4, 8], F32)
    kb.dma("sp", hs[:], st_h_in[:])
    xps = [[kb.sb("xps%d_%d" % (q, m), [128, 16], F32) for m in range(8)] for q in range(4)]
    stc = kb.sb("stc", [128, 4, 8, 3], F32)
    kb.dma("sp", stc[:], st_conv_in[:])
    for q in range(4):
        for m in range(8):
            kb.copy("pool", xps[q][m].v(xps[q][m].t[:, 0:3]), stc.v(stc.t[:, q, m, :]))
        hq = T(hs.t[:, q, :], "hsq")
        hq.buf = hs.buf

        def keep(m, rh, q=q):
            kb.copy("pool", h_s_all.v(h_s_all.t[:, m, 8 * q:8 * q + 8]), rh.v(rh.t[:, 0:8]))
        rnn_group(xnT, 8, hq, xps[q], keep, c0=8 * q)
        for m in range(8):
            kb.dma("sp", o_conv_s.v(o_conv_s.t[:, q, m, :]), xps[q][m].v(xps[q][m].t[:, 0:3]))
    kb.dma("sp", o_h_s[:], hs[:])
    kb.phase_end()
    stage_gate(2)

    kb.phase_begin()
    W2 = kb.sb("W2", [128, 16, 2072], BF16)
    for k in range(16):
        kb.dma("pool", W2.v(W2.t[:, k, 0:1024]), w_in.v(w_view[:, k, C_Q:C_Q + 1024]))
        kb.dma("pool", W2.v(W2.t[:, k, 1024:1048]), w_in.v(w_view[:, k, C_GT:C_GT + 24]))
        kb.dma("pool", W2.v(W2.t[:, k, 1048:2072]), w_in.v(w_view[:, k, C_XG:C_XG + 1024]))
    qTt = kb.sb("qTt", [128, 8, 512], BF16)
    g_x = kb.sb("g_x", [128, 512], F32)
    g_t = kb.sb("g_t", [128, 512], F32)
    g_o = kb.sb("g_o", [128, 512], BF16)
    hsb = kb.sb("hsb", [128, 8, 32], BF16)
    kb.copy("pool", hsb[:], h_s_all[:])
    for G in range(3):
        n = 512 if G < 2 else 32
        t0 = G * 512
        if G < 2:
            for i in range(4):
                norm_transpose(x_own.v(x_own.t[t0 + i * 128:t0 + (i + 1) * 128, :]), 128, xnT, i * 128)
        else:
            norm_transpose(x_s[:, :], 32, xnT, 0)
        for h in range(8):
            pb = psb[4 + (h % 2)]
            for k in range(16):
                kb.mm(pb.v(pb.t[:, 0:n]), W2.v(W2.t[:, k, h * 128:(h + 1) * 128]), xnT.v(xnT.t[:, k, 0:n]),
                      start=(k == 0), stop=(k == 15), signal=(k == 15))
            kb.act(qTt.v(qTt.t[:, h, 0:n]), pb.v(pb.t[:, 0:n]), AF.Copy, scale=float(128 ** -0.5))
        kb.dma("sp", scr_q.v(scr_q.t[:, :, t0:t0 + n]), qTt.v(qTt.t[:, :, 0:n]))
        for i in range((n + 127) // 128):
            rows = min(128, n - i * 128)
            pb = psb[6]
            for k in range(16):
                kb.mm(pb.v(pb.t[0:rows, 0:24]), xnT.v(xnT.t[:, k, i * 128:i * 128 + rows]), W2.v(W2.t[:, k, 1024:1048]),
                      start=(k == 0), stop=(k == 15), signal=(k == 15))
            kb.act(gates_sb.v(gates_sb.t[0:rows, G * 4 + i, :]), pb.v(pb.t[0:rows, 0:24]), AF.Sigmoid)
        for m in range(8):
            pb = psb[4 + (m % 2)]
            for k in range(16):
                kb.mm(pb.v(pb.t[:, 0:n]), W2.v(W2.t[:, k, 1048 + m * 128:1048 + (m + 1) * 128]), xnT.v(xnT.t[:, k, 0:n]),
                      start=(k == 0), stop=(k == 15), signal=(k == 15))
            gx, gt, go = g_x.v(g_x.t[:, 0:n]), g_t.v(g_t.t[:, 0:n]), g_o.v(g_o.t[:, 0:n])
            kb.copy("act", gx, pb.v(pb.t[:, 0:n]))
            kb.tt("pool", gt, gx, gx, ALU.mult)
            kb.ts("dve", gt, gt, 0.044715, 1.0, ALU.mult, ALU.add)
            kb.tt("pool", gt, gt, gx, ALU.mult)
            kb.act(gt, gt, AF.Sigmoid, scale=1.5957691216057308)
            kb.tt("pool", gt, gt, gx, ALU.mult)
            hsrc = own_h.v(own_h.t[:, m, t0:t0 + n]) if G < 2 else hsb.v(hsb.t[:, m, :])
            kb.tt("pool", go, gt, hsrc, ALU.mult)
            kb.dma("sp", scr_cat.v(scr_cat.t[:, 8 + m, t0:t0 + n]), go)
    kb.dma("sp", scr_g[:], gates_sb.v(gates_sb.t[0:32, 8, :]))
    kb.phase_end()
    kb.phase_end()
    stage_gate(3)

    kb.phase_begin()
    iota_p = kb.sb("iota_p", [128, 4096], F32)
    kb.dma("sp", iota_p[:], iota_in.v(iota_in.t[0:1, :].partition_broadcast(128)))
    iota_ce = kb.sb("iota_ce_sb", [128, 128], F32)
    kb.dma("sp", iota_ce[:], iotace_in.v(iotace_in.t[0:1, :].partition_broadcast(128)))
    iota_blk = kb.sb("iota_blk_sb", [128, 64], F32)
    kb.dma("sp", iota_blk[:], iotablk_in.v(iotablk_in.t[0:1, :].partition_broadcast(128)))
    tpos = kb.sb("tpos_sb", [128, 8], F32)
    kb.dma("sp", tpos[:], tpos_in[:])
    curb = kb.sb("curb_sb", [128, 8], F32)
    kb.dma("sp", curb[:], curb_in[:])
    curm1 = kb.sb("curm1", [128, 8], F32)
    kb.ts("dve", curm1[:], curb[:], -1.0, None, ALU.add)
    negdw = kb.sb("negdw_sb", [128, 640], F32)
    kb.dma("sp", negdw[:], negdw_in[:])
    bandw = kb.sb("bandw_sb", [128, 640], F32)
    kb.dma("sp", bandw[:], bandw_in[:])
    kselT = kb.sb("kselT", [128, 2, SEQ], BF16)
    vsel = kb.sb("vsel", [128, 32, 2, 129], BF16)
    kwin_loc = kb.sb("kwin_loc", [128, 2, 1536], BF16)
    vwin_loc = kb.sb("vwin_loc", [128, 12, 2, 129], BF16)
    kb.memset("pool", vsel[:], 1.0)
    kb.memset("pool", vwin_loc[:], 1.0)
    kb.memset("pool", vwin_loc.v(vwin_loc.t[:, :, :, 0:128]), 0.0)
    kb.memset("pool", kwin_loc[:], 0.0)
    for g in range(2):
        kb.dma("sp", kselT.v(kselT.t[:, g, :]), scr_kT.v(scr_kT.t[0, g]))
    for t in range(32):
        kb.dma("sp", vsel.v(vsel.t[:, t, :, 0:128]), scr_v.v(scr_v.t[0, t]))
    negd = kb.sb("negd", [128, 4096], F32)
    Sb = kb.sb("Sb", [128, 4096], F32)
    kwfull = T(Sb.t[:].bitcast(BF16), "kwfull")
    kwfull.buf = Sb.buf
    vwfull = T(negd.t[:].bitcast(BF16), "vwfull")
    vwfull.buf = negd.buf
    for g in range(2):
        kb.dma("sp", kwfull.v(kwfull.t[:, g * 4096:(g + 1) * 4096]), scr_kT.v(scr_kT.t[1, g]))
    for q in range(4):
        lo = 8 * q - 4
        a = max(lo, 0)
        for g in range(2):
            dst = kwin_loc.v(kwin_loc.t[:, g, (a - lo) * 128:1536])
            kb.stt("dve", dst, kwfull.v(kwfull.t[:, g * 4096 + a * 128:g * 4096 + (lo + 12) * 128]), onehot.v(onehot.t[:, q:q + 1]), dst, ALU.mult, ALU.add)
    vw3 = vwfull.t[:, 0:8192].rearrange("p (t g d) -> p t g d", t=32, g=2)
    for t in range(32):
        kb.dma("sp", vwfull.v(vw3[:, t]), scr_v.v(scr_v.t[1, t]))
    for q in range(4):
        lo = 8 * q - 4
        a = max(lo, 0)
        dst = vwin_loc.v(vwin_loc.t[:, a - lo:12, :, 0:128])
        kb.stt("dve", dst, vwfull.v(vw3[:, a:lo + 12]), onehot.v(onehot.t[:, q:q + 1]), dst, ALU.mult, ALU.add)
    maskg = kb.sb("maskg", [128, 4096], BF16)
    Eb = kb.sb("Eb", [128, 4096], BF16)
    PT = kb.sb("PT", [128, 8, 128], BF16)
    Sb_b = kb.sb("Sb_b", [128, 4096], F32)
    Eb_b = kb.sb("Eb_b", [128, 4096], BF16)
    PT_b = kb.sb("PT_b", [128, 8, 128], BF16)
    Sb2, Eb2, PT2 = [Sb, Sb_b], [Eb, Eb_b], [PT, PT_b]
    qs = kb.sb("qs", [128, 8, 128], BF16)
    negdc = kb.sb("negdc", [128, 128], F32)
    validc = kb.sb("validc", [128, 128], F32)
    Sc = kb.sb("Sc", [128, 4, 128], F32)
    pcb = kb.sb("pcb", [128, 4, 128], BF16)
    mc = kb.sb("mc", [128, 4], F32)
    sc = kb.sb("sc", [128, 4], F32)
    imp = kb.sb("imp", [128, 64], F32)
    imp2 = kb.sb("imp2", [128, 64], F32)
    fm = kb.sb("fm", [128, 64], F32)
    validb = kb.sb("validb", [128, 64], F32)
    selb = kb.sb("selb", [128, 64], BF16)
    mx8 = kb.sb("mx8", [128, 8], F32)
    thr = kb.sb("thr", [128, 1], F32)
    m1_2 = [kb.sb("m1_%d" % i, [128, 1], F32) for i in range(4)]
    coef_2 = [kb.sb("coef_%d" % i, [128, 1], F32) for i in range(4)]
    Sw2 = [kb.sb("Sw%d" % i, [128, 640], F32) for i in range(2)]
    Ew2 = [kb.sb("Ew%d" % i, [128, 640], BF16) for i in range(2)]
    PTw2 = [kb.sb("PTw%d" % i, [128, 8, 128], BF16) for i in range(2)]
    wm = kb.sb("wm", [128, 640], BF16)
    wtmp = kb.sb("wtmp", [128, 640], F32)
    attn_tm = kb.sb("attn_tm", [128, 1024], F32)
    attn_bf = kb.sb("attn_bf", [128, 1024], BF16)
    aT = kb.sb("aT", [128, 8, 128], BF16)

    def softmax_A(S_t, width, E_t, mask_v, m1):
        kb.op("dve", lambda e: e.tensor_reduce(out=m1.t[:], in_=S_t.t[:, 0:width], axis=AX.X, op=ALU.max), [S_t[:]], [m1[:]])
        kb.ts("dve", m1[:], m1[:], -1.0, None, ALU.mult)
        kb.act(E_t.v(E_t.t[:, 0:width]), S_t.v(S_t.t[:, 0:width]), AF.Exp, bias=m1[:], scale=1.0)
        kb.tt("pool", E_t.v(E_t.t[:, 0:width]), E_t.v(E_t.t[:, 0:width]), mask_v, ALU.mult)

    def softmax_B(width, E_t, ntiles, vsrc, po, h, gate_col, s, first, PT, coef, tb):
        for rr in range((ntiles + 7) // 8):
            cnt = min(8, ntiles - rr * 8)
            pt = psb[tb + rr % 2]
            ptv = bview(pt)
            for i in range(cnt):
                kt = rr * 8 + i
                kb.tr(pt.v(ptv[:, i * 128:(i + 1) * 128]), E_t.v(E_t.t[:, kt * 128:(kt + 1) * 128]), ident_b[:], signal=(i == cnt - 1))
            kb.copy("act", PT.v(PT.t[:, 0:cnt, :]), pt.v(ptv[:, 0:cnt * 128].rearrange("p (a b) -> p a b", a=cnt)))
            for i in range(cnt):
                kt = rr * 8 + i
                kb.mm(po.v(po.t[:, 0:129]), PT.v(PT.t[:, i, :]), vsrc(kt), start=(kt == 0), stop=(kt == ntiles - 1),
                      signal=(kt == ntiles - 1))
        kb.ts("dve", coef[:], po.v(po.t[:, 128:129]), 1e-30, None, ALU.max)
        kb.op("dve", lambda e: e.reciprocal(out=coef.t[:], in_=coef.t[:]), [coef[:]], [coef[:]])
        kb.tt("dve", coef[:], coef[:], gates_sb.v(gates_sb.t[:, s, gate_col:gate_col + 1]), ALU.mult)
        dst = attn_tm.v(attn_tm.t[:, h * 128:(h + 1) * 128])
        if first:
            kb.ts("dve", dst, po.v(po.t[:, 0:128]), coef.v(coef.t[:, 0:1]), None, ALU.mult)
        else:
            kb.stt("dve", dst, po.v(po.t[:, 0:128]), coef.v(coef.t[:, 0:1]), dst, ALU.mult, ALU.add)

    for s in range(8):
        tq = tpos.v(tpos.t[:, s:s + 1])
        kb.dma("sp", qs[:], scr_q.v(scr_q.t[:, :, s * 128:(s + 1) * 128]))
        kb.ts("dve", negd[:], iota_p[:], tq, 0.0, ALU.subtract, ALU.min)
        kb.ts("dve", negdc[:], iota_ce[:], tq, 0.0, ALU.subtract, ALU.min)
        kb.ts("dve", validc[:], iota_ce[:], tq, None, ALU.is_le)
        kb.ts("dve", wtmp[:], iota_p.v(iota_p.t[:, 0:640]), float(512 - 128 * s), onehot.v(onehot.t[:, 0:1]), ALU.is_lt, ALU.mult)
        kb.ts("dve", wtmp[:], wtmp[:], -1.0, 1.0, ALU.mult, ALU.add)
        kb.tt("pool", wm[:], bandw[:], wtmp[:], ALU.mult)
        for g in range(2):
            pc = psb[0]
            for r in range(4):
                kb.mm(pc.v(pc.t[:, r * 128:(r + 1) * 128]), qs.v(qs.t[:, 4 * g + r, :]), kcmpT_sb.v(kcmpT_sb.t[:, g, :]), signal=(r == 3))
            for r in range(4):
                kb.stt("dve", Sc.v(Sc.t[:, r, :]), negdc[:], SLOPES[4 * g + r], pc.v(pc.t[:, r * 128:(r + 1) * 128]), ALU.mult, ALU.add)
            kb.op("dve", lambda e: e.tensor_reduce(out=mc.t[:], in_=Sc.t[:], axis=AX.X, op=ALU.max), [Sc[:]], [mc[:]])
            kb.ts("dve", mc[:], mc[:], -1.0, None, ALU.mult)
            for r in range(4):
                kb.act(Sc.v(Sc.t[:, r, :]), Sc.v(Sc.t[:, r, :]), AF.Exp, bias=mc.v(mc.t[:, r:r + 1]), scale=1.0)
            kb.tt("pool", Sc[:], Sc[:], validc.v(validc.t[:, :].unsqueeze(1).to_broadcast([128, 4, 128])), ALU.mult)
            kb.op("dve", lambda e: e.tensor_reduce(out=sc.t[:], in_=Sc.t[:], axis=AX.X, op=ALU.add), [Sc[:]], [sc[:]])
            kb.ts("dve", sc[:], sc[:], 1e-30, None, ALU.max)
            kb.op("dve", lambda e: e.reciprocal(out=sc.t[:], in_=sc.t[:]), [sc[:]], [sc[:]])
            kb.tt("dve", Sc[:], Sc[:], sc.v(sc.t[:, :].unsqueeze(2).to_broadcast([128, 4, 128])), ALU.mult)
            kb.op("dve", lambda e: e.tensor_reduce(out=imp.t[:], in_=Sc.t[:].rearrange("p h (j two) -> p j h two", two=2),
                                                    axis=AX.XY, op=ALU.add), [Sc[:]], [imp[:]])
            cur = curb.v(curb.t[:, s:s + 1])
            kb.ts("dve", fm[:], iota_blk[:], cur, None, ALU.is_equal)
            kb.ts("dve", imp2[:], iota_blk[:], curm1.v(curm1.t[:, s:s + 1]), None, ALU.is_equal)
            kb.tt("dve", fm[:], fm[:], imp2[:], ALU.max)
            kb.ts("dve", imp2[:], iota_blk[:], 0.0, None, ALU.is_equal)
            kb.tt("dve", fm[:], fm[:], imp2[:], ALU.max)
            kb.stt("dve", imp[:], fm[:], 1e4, imp[:], ALU.mult, ALU.max)
            kb.ts("dve", validb[:], iota_blk[:], cur, None, ALU.is_le)
            kb.tt("dve", imp[:], imp[:], validb[:], ALU.mult)
            kb.stt("dve", imp[:], validb[:], -1.0, imp[:], ALU.add, ALU.add)
            kb.op("dve", lambda e: e.max(out=mx8.t[:], in_=imp.t[:]), [imp[:]], [mx8[:]])
            kb.op("dve", lambda e: e.match_replace(out=imp2.t[:], in_to_replace=mx8.t[:], in_values=imp.t[:], imm_value=-2.0),
                  [mx8[:], imp[:]], [imp2[:]])
            kb.op("dve", lambda e: e.max(out=mx8.t[:], in_=imp2.t[:]), [imp2[:]], [mx8[:]])
            kb.op("dve", lambda e: e.tensor_reduce(out=thr.t[:], in_=mx8.t[:], axis=AX.X, op=ALU.min), [mx8[:]], [thr[:]])
            kb.ts("dve", imp2[:], imp[:], thr.v(thr.t[:, 0:1]), None, ALU.is_ge)
            kb.tt("dve", selb[:], imp2[:], validb[:], ALU.mult)
            kb.stt("dve", maskg.v(maskg.t[:].rearrange("p (b k) -> p b k", k=64)), iota_p.v(iota_p.t[:].rearrange("p (b k) -> p b k", k=64)),
                   tq, selb.v(selb.t[:, :].unsqueeze(2).to_broadcast([128, 64, 64])), ALU.is_le, ALU.mult)
            kb.copy("pool", pcb[:], Sc[:])
            pt = psb[1]
            ptv = bview(pt)
            for r in range(4):
                kb.tr(pt.v(ptv[:, r * 128:(r + 1) * 128]), pcb.v(pcb.t[:, r, :]), ident_b[:], signal=(r == 3))
            kb.copy("act", PT.v(PT.t[:, 0:4, :]), pt.v(ptv[:, 0:512].rearrange("p (a b) -> p a b", a=4)))
            po = psb[0]
            for r in range(4):
                kb.mm(po.v(po.t[:, r * 128:(r + 1) * 128]), PT.v(PT.t[:, r, :]), vcmp_sb.v(vcmp_sb.t[:, g, :]), signal=(r == 3))
            for r in range(4):
                h = 4 * g + r
                kb.ts("dve", attn_tm.v(attn_tm.t[:, h * 128:(h + 1) * 128]), po.v(po.t[:, r * 128:(r + 1) * 128]),
                      gates_sb.v(gates_sb.t[:, s, 3 * h:3 * h + 1]), None, ALU.mult)
            def mk(r, g=g, s=s):
                h = 4 * g + r
                par = r % 2
                Sbx, Sw, Ew = Sb2[par], Sw2[par], Ew2[par]

                def A_sel():
                    for c4 in range(8):
                        ps = psb[2 + c4 % 2]
                        kb.mm(ps[:], qs.v(qs.t[:, h, :]), kselT.v(kselT.t[:, g, c4 * 512:(c4 + 1) * 512]))
                        kb.stt("dve", Sbx.v(Sbx.t[:, c4 * 512:(c4 + 1) * 512]), negd.v(negd.t[:, c4 * 512:(c4 + 1) * 512]), SLOPES[h], ps[:], ALU.mult, ALU.add)
                    softmax_A(Sbx, 4096, Eb2[par], maskg[:], m1_2[par])

                def B_sel():
                    softmax_B(4096, Eb2[par], 32, lambda kt: vsel.v(vsel.t[:, kt, g, :]), psb[6] if par == 0 else psb[0], h, 3 * h + 1, s, False,
                              PT2[par], coef_2[par], 4)

                def A_win():
                    kb.mm(psb[2][:], qs.v(qs.t[:, h, :]), kwin_loc.v(kwin_loc.t[:, g, s * 128:s * 128 + 512]))
                    kb.mm(psb[3].v(psb[3].t[:, 0:128]), qs.v(qs.t[:, h, :]), kwin_loc.v(kwin_loc.t[:, g, s * 128 + 512:s * 128 + 640]))
                    kb.stt("dve", Sw.v(Sw.t[:, 0:512]), negdw.v(negdw.t[:, 0:512]), SLOPES[h], psb[2][:], ALU.mult, ALU.add)
                    kb.stt("dve", Sw.v(Sw.t[:, 512:640]), negdw.v(negdw.t[:, 512:640]), SLOPES[h], psb[3].v(psb[3].t[:, 0:128]), ALU.mult, ALU.add)
                    softmax_A(Sw, 640, Ew, wm[:], m1_2[2 + par])

                def B_win():
                    softmax_B(640, Ew, 5, lambda kt: vwin_loc.v(vwin_loc.t[:, s + kt, g, :]), psb[7] if par == 0 else psb[1], h, 3 * h + 2, s, False,
                              PTw2[par], coef_2[2 + par], 4)
                return A_sel, B_sel, A_win, B_win
            jobs = [mk(r) for r in range(4)]
            jobs[0][0]()
            jobs[0][2]()
            for r in range(4):
                if r + 1 < 4:
                    jobs[r + 1][0]()
                jobs[r][1]()
                if r + 1 < 4:
                    jobs[r + 1][2]()
                jobs[r][3]()
        kb.copy("pool", attn_bf[:], attn_tm[:])
        pt = psb[1]
        ptv = bview(pt)
        for h in range(8):
            kb.tr(pt.v(ptv[:, h * 128:(h + 1) * 128]), attn_bf.v(attn_bf.t[:, h * 128:(h + 1) * 128]), ident_b[:], signal=(h == 7))
        kb.copy("act", aT[:], pt.v(ptv[:, 0:1024].rearrange("p (a b) -> p a b", a=8)))
        kb.dma("sp", scr_cat.v(scr_cat.t[:, 0:8, s * 128:(s + 1) * 128]), aT[:])
    kb.phase_end()
    stage_gate(35)

    kb.phase_begin()
    ptb = kb.sb("ptb", [128, 512], I32)
    kb.dma("sp", ptb[:], pt_in.v(pt_in.t[0:1, :].partition_broadcast(128)))
    pidx = kb.sb("pidx_sb", [128, 1], F32)
    kb.dma("sp", pidx[:], pidx_in[:])
    ptf = kb.sb("ptf", [128, 512], F32)
    kb.copy("dve", ptf[:], ptb[:])
    kb.ts("dve", ptf[:], ptf[:], 128.0, pidx.v(pidx.t[:, 0:1]), ALU.mult, ALU.add)
    pidx_i = kb.sb("pidx_i", [128, 512], I32)
    kb.copy("dve", pidx_i[:], ptf[:])
    cbs = kb.sb("cbs", [128, 2, 512], F32)
    kb.dma("sp", cbs[:], cbs_in[:])
    slrow = kb.sb("slrow", [128, 2], F32)
    kb.dma("sp", slrow[:], slrow_in[:])
    stok = kb.sb("stok_sb", [128, 2], F32)
    kb.dma("sp", stok[:], stok_in[:])
    kb.eng["pool"].wait(pidx_i.buf.w)
    wbs = kb.sb("wbs", [128, 2, 520], F32)
    kb.dma("sp", wbs[:], wbs_in[:])
    wms = kb.sb("wms", [128, 520], F32)
    kb.dma("sp", wms[:], wms_in[:])
    nbs = kb.sb("nbs", [128, 2, 8], F32)
    kb.dma("sp", nbs[:], nbs_in[:])
    nms = kb.sb("nms", [128, 8], F32)
    kb.dma("sp", nms[:], nms_in[:])
    rsum_f = kb.sb("rsum_f", [128, 128], F32)
    kb.dma("sp", rsum_f[:], rsum_in[:])
    rsum_b = kb.sb("rsum_b", [128, 128], BF16)
    kb.copy("pool", rsum_b[:], rsum_f[:])
    forced = kb.sb("forced", [128, 257], F32)
    kb.dma("sp", forced[:], forced_in.v(forced_in.t[0:1, :].partition_broadcast(128)))
    iota512 = kb.sb("iota512_sb", [128, 512], F32)
    kb.dma("sp", iota512[:], iota512_in.v(iota512_in.t[0:1, :].partition_broadcast(128)))
    poolk2 = kb.sb("poolk2", [128, 4], BF16)
    poolv2 = kb.sb("poolv2", [128, 252], BF16)
    kb.dma("pool", poolk2[:], poolk_in[:])
    kb.dma("pool", poolv2[:], poolv_in[:])
    qss = kb.sb("qss", [128, 2, 4, 32], BF16)
    for g in range(2):
        for b in range(4):
            kb.dma("sp", qss.v(qss.t[:, g, b, :].rearrange("p (r t) -> p r t", r=4)),
                   scr_q.v(scr_q.t[:, 4 * g:4 * g + 4, 1024 + 8 * b:1024 + 8 * b + 8]))
    grow = kb.sb("grow", [128, 2, 3], F32)
    for g in range(2):
        for b in range(4):
            for r in range(4):
                kb.dma("sp", grow.v(grow.t[32 * b + 8 * r:32 * b + 8 * r + 8, g, :]),
                       scr_g.v(scr_g.t[8 * b:8 * b + 8, (4 * g + r) * 3:(4 * g + r) * 3 + 3]))
    attn_s = kb.sb("attn_s", [128, 2, 128], F32)
    pg = [kb.sb("pg%d" % i, [128, 4, 512], F32) for i in range(6)]
    pgk2 = [kb.sb("pgk%d" % i, [128, 4, 2, 128], BF16) for i in range(2)]
    pgv2 = [kb.sb("pgv%d" % i, [128, 1, 2, 129], BF16) for i in range(2)]
    pgk = pgk2[0]
    for i in range(2):
        kb.memset("pool", pgv2[i][:], 1.0)
    vwS = kb.sb("vwS", [128, 4, 4, 2, 129], BF16)
    kb.memset("pool", vwS[:], 1.0)
    vsS2 = [kb.sb("vsS%d" % i, [128, 4, 4, 2, 129], BF16) for i in range(2)]
    for i in range(2):
        kb.memset("pool", vsS2[i][:], 1.0)
    kTs2 = [kb.sb("kTs%d" % i, [128, 8, 128], BF16) for i in range(2)]
    kTs = kTs2[0]
    kcmpT_s = kb.sb("kcmpT_s", [128, 4, 2, 512], BF16)
    vcmp_s = kb.sb("vcmp_s", [128, 4, 4, 2, 128], BF16)
    S_sb2 = [kb.sb("S_sb%d" % i, [128, 520], F32) for i in range(2)]
    E_sb2 = [kb.sb("E_sb%d" % i, [128, 520], BF16) for i in range(2)]
    PTs2 = [kb.sb("PTs%d" % i, [128, 5, 128], BF16) for i in range(2)]
    S_sb, E_sb, PTs = S_sb2[0], E_sb2[0], PTs2[0]
    mask_c2 = [kb.sb("mask_c%d" % i, [128, 512], BF16) for i in range(2)]
    p_hi = kb.sb("p_hi", [128, 512], BF16)
    p_lo = kb.sb("p_lo", [128, 512], BF16)
    imps = kb.sb("imps", [128, 264], F32)
    imps2 = kb.sb("imps2", [128, 264], F32)
    sels = [kb.sb("sels%d" % g, [128, 264], BF16) for g in range(2)]
    mrun = [kb.sb("mrun%d" % g, [128, 1], F32) for g in range(2)]
    oacc = [kb.sb("oacc%d" % g, [128, 129], F32) for g in range(2)]
    mc_ = kb.sb("mc_", [128, 1], F32)
    mn_ = kb.sb("mn_", [128, 1], F32)
    al_ = kb.sb("al_", [128, 1], F32)
    kc_ = kb.sb("kc_", [128, 1], F32)
    bi_ = kb.sb("bi_", [128, 1], F32)
    cf_ = kb.sb("cf_", [128, 1], F32)
    mx8s = kb.sb("mx8s", [128, 8], F32)
    thrs = kb.sb("thrs", [128, 1], F32)
    sm_ = kb.sb("sm_", [128, 1], F32)
    mask_c = kb.sb("mask_c", [128, 512], BF16)

    def gather_pages(buf, cache, lp):
        for b in range(4):
            col = b * 128 + lp
            kb.dma("pool", buf.v(buf.t[:, b, :]), cache[:, :],
                   fn=lambda e, b=b, col=col: e.indirect_dma_start(out=buf.t[:, b, :], out_offset=None, in_=cache.t[:, :],
                                                                    in_offset=bass.IndirectOffsetOnAxis(ap=pidx_i.t[:, col:col + 1], axis=0)))

    for b in range(4):
        for bank in (4, 5, 6, 7):
            kb.memset("dve", psb[bank][:], 0.0)
        for lp in range(128):
            buf = pg[lp % 6]
            col = b * 128 + lp
            kb.dma("pool", buf.v(buf.t[:, 0, :]), cache_c[:, :],
                   fn=lambda e, buf=buf, col=col: e.indirect_dma_start(out=buf.t[:, 0, :], out_offset=None, in_=cache_c.t[:, :],
                                                                        in_offset=bass.IndirectOffsetOnAxis(ap=pidx_i.t[:, col:col + 1], axis=0)))
            src4 = buf.t[:, 0, :].rearrange("p (g c d) -> p g c d", g=2, c=2)
            pgk, pgv = pgk2[lp % 2], pgv2[lp % 2]
            kb.copy("act", pgk.v(pgk.t[:, 0, :, :]), buf.v(src4[:, :, 0, :]))
            kb.copy("dve", pgv.v(pgv.t[:, 0, :, 0:128]), buf.v(src4[:, :, 1, :]))
            for g in range(2):
                pk = psb[4 + g]
                kb.mm(pk.v(pk.t[:, 4 * lp:4 * lp + 4]), pgk.v(pgk.t[:, 0, g, :]), poolk2[:], start=False, stop=False, signal=False)
            tl, off = lp // 32, lp % 32
            pv = psb[6 + tl // 2]
            kb.mm(pv.v(pv.t[:, (tl % 2) * 256:(tl % 2) * 256 + 256].rearrange("p (g d) -> p g d", g=2)),
                  poolv2.v(poolv2.t[:, 124 - 4 * off:252 - 4 * off]), pgv.v(pgv.t[:, 0, :, 0:128]), start=False, stop=False, signal=True)
        for g in range(2):
            kb.copy("act", kcmpT_s.v(kcmpT_s.t[:, b, g, :]), psb[4 + g][:])
        for tl in range(4):
            pv = psb[6 + tl // 2]
            kb.copy("dve", vcmp_s.v(vcmp_s.t[:, b, tl, :, :]), pv.v(pv.t[:, (tl % 2) * 256:(tl % 2) * 256 + 256].rearrange("p (g d) -> p g d", g=2)))

    def exact_softmax(S_ps_list, width, bias_v, mask_v):
        c0 = 0
        for (ps_v, w) in S_ps_list:
            kb.tt("dve", S_sb.v(S_sb.t[:, c0:c0 + w]), ps_v, V(bias_v.ap[:, c0:c0 + w], bias_v.buf), ALU.add)
            c0 += w
        kb.op("dve", lambda e: e.tensor_reduce(out=mc_.t[:], in_=S_sb.t[:, 0:width], axis=AX.X, op=ALU.max), [S_sb[:]], [mc_[:]])
        kb.ts("dve", mc_[:], mc_[:], -1.0, None, ALU.mult)
        kb.act(E_sb.v(E_sb.t[:, 0:width]), S_sb.v(S_sb.t[:, 0:width]), AF.Exp, bias=mc_[:], scale=1.0)
        if mask_v is not None:
            kb.tt("dve", E_sb.v(E_sb.t[:, 0:width]), E_sb.v(E_sb.t[:, 0:width]), mask_v, ALU.mult)

    def transposeE(ntiles, width, E_sb=E_sb, PTs=PTs):
        pt = psb[4]
        ptv = bview(pt)
        for i in range(ntiles):
            w = min(128, width - i * 128)
            kb.tr(pt.v(ptv[0:w, i * 128:(i + 1) * 128]), E_sb.v(E_sb.t[:, i * 128:i * 128 + w]), ident_b[:], signal=(i == ntiles - 1))
        kb.copy("act", PTs.v(PTs.t[:, 0:ntiles, :]), pt.v(ptv[:, 0:ntiles * 128].rearrange("p (a b) -> p a b", a=ntiles)))

    kvn = kb.sb("kvn", [8, 4, 3 * KVW], BF16)
    for b in range(4):
        kb.dma("sp", kvn.v(kvn.t[:, b, :]), scr_kvs.v(scr_kvs.t[8 * b:8 * b + 8, :]))
    vnew = kb.sb("vnew", [8, 2, 4, 2, 129], BF16)
    kb.memset("pool", vnew[:], 1.0)
    knT = kb.sb("knT", [128, 2, 4, 2, 8], BF16)
    for br in range(2):
        base = 512 * (br + 1)
        for b in range(4):
            src4 = kvn.t[:, b, base:base + 512].rearrange("p (g c d) -> p g c d", g=2, c=2)
            kb.copy("pool", vnew.v(vnew.t[:, br, b, :, 0:128]), kvn.v(src4[:, :, 1, :]))
            pt = psb[3]
            ptv = bview(pt)
            for g in range(2):
                kb.tr(pt.v(ptv[:, g * 8:g * 8 + 8]), kvn.v(kvn.t[:, b, base + g * 256:base + g * 256 + 128]), ident_b.v(ident_b.t[0:8, 0:8]), signal=(g == 1))
            kb.copy("act", knT.v(knT.t[:, br, b, :, :]), pt.v(ptv[:, 0:16].rearrange("p (g k) -> p g k", g=2)))

    for g in range(2):
        pc = psb[0]
        for b in range(4):
            kb.mm(pc.v(pc.t[32 * b:32 * b + 32, :]), qss.v(qss.t[:, g, b, :]), kcmpT_s.v(kcmpT_s.t[:, b, g, :]), signal=(b == 3))
        exact_softmax([(pc[:], 512)], 512, cbs.v(cbs.t[:, g, :]), None)
        kb.op("dve", lambda e: e.tensor_reduce(out=sm_.t[:], in_=E_sb.t[:, 0:512], axis=AX.X, op=ALU.add), [E_sb[:]], [sm_[:]])
        kb.op("dve", lambda e: e.reciprocal(out=sm_.t[:], in_=sm_.t[:]), [sm_[:]], [sm_[:]])
        kb.act(S_sb.v(S_sb.t[:, 0:512]), S_sb.v(S_sb.t[:, 0:512]), AF.Exp, bias=mc_[:], scale=1.0)
        kb.ts("dve", S_sb.v(S_sb.t[:, 0:512]), S_sb.v(S_sb.t[:, 0:512]), sm_.v(sm_.t[:, 0:1]), None, ALU.mult)
        kb.ts("dve", E_sb.v(E_sb.t[:, 0:512]), E_sb.v(E_sb.t[:, 0:512]), sm_.v(sm_.t[:, 0:1]), None, ALU.mult)
        kb.copy("pool", p_hi[:], S_sb.v(S_sb.t[:, 0:512]))
        kb.tt("pool", p_lo[:], S_sb.v(S_sb.t[:, 0:512]), p_hi[:], ALU.subtract)
        pi = psb[1]
        kb.mm(pi[:], rsum_b[:], p_hi[:], start=True, stop=False, signal=False)
        kb.mm(pi[:], rsum_b[:], p_lo[:], start=False, stop=True, signal=True)
        kb.memset("pool", imps[:], 0.0)
        kb.copy("act", S_sb.v(S_sb.t[:, 0:512]), pi[:])
        kb.tt("dve", imps.v(imps.t[:, 0:256]), S_sb.v(S_sb.t[:, 0:512].rearrange("p (j two) -> p j two", two=2)[:, :, 0]),
              S_sb.v(S_sb.t[:, 0:512].rearrange("p (j two) -> p j two", two=2)[:, :, 1]), ALU.add)
        kb.stt("dve", imps.v(imps.t[:, 0:257]), forced[:], 1e4, imps.v(imps.t[:, 0:257]), ALU.mult, ALU.max)
        kb.memset("pool", imps.v(imps.t[:, 257:264]), -1.0)
        kb.op("dve", lambda e: e.max(out=mx8s.t[:], in_=imps.t[:]), [imps[:]], [mx8s[:]])
        kb.op("dve", lambda e: e.match_replace(out=imps2.t[:], in_to_replace=mx8s.t[:], in_values=imps.t[:], imm_value=-2.0),
              [mx8s[:], imps[:]], [imps2[:]])
        kb.op("dve", lambda e: e.max(out=mx8s.t[:], in_=imps2.t[:]), [imps2[:]], [mx8s[:]])
        kb.op("dve", lambda e: e.tensor_reduce(out=thrs.t[:], in_=mx8s.t[:], axis=AX.X, op=ALU.min), [mx8s[:]], [thrs[:]])
        kb.ts("dve", sels[g][:], imps[:], thrs.v(thrs.t[:, 0:1]), None, ALU.is_ge)
        transposeE(4, 512)
        po = psb[5]
        for b in range(4):
            for tl in range(4):
                kb.mm(po.v(po.t[32 * b:32 * b + 32, 0:128]), PTs.v(PTs.t[:, tl, 32 * b:32 * b + 32]), vcmp_s.v(vcmp_s.t[:, b, tl, g, :]),
                      start=(tl == 0), stop=(tl == 3), signal=(b == 3 and tl == 3))
        kb.ts("dve", attn_s.v(attn_s.t[:, g, :]), po.v(po.t[:, 0:128]), grow.v(grow.t[:, g, 0:1]), None, ALU.mult)
        for tl in range(4):
            for b in range(4):
                kb.dma("sp", pg[0].v(pg[0].t[:, b, :]), cwin_in.v(cwin_in.t[b, tl * 128:(tl + 1) * 128, :]))
            src5 = pg[0].t[:].rearrange("p b (g c d) -> p b g c d", g=2, c=2)
            kb.copy("act", pgk[:], pg[0].v(src5[:, :, :, 0, :]))
            kb.copy("dve", vwS.v(vwS.t[:, tl, :, :, 0:128]), pg[0].v(src5[:, :, :, 1, :]))
            pt = psb[2]
            ptv = bview(pt)
            for b in range(4):
                kb.tr(pt.v(ptv[:, b * 128:(b + 1) * 128]), pgk.v(pgk.t[:, b, g, :]), ident_b[:], signal=(b == 3))
            kb.copy("act", kTs.v(kTs.t[:, 0:4, :]), pt.v(ptv[:, 0:512].rearrange("p (a b) -> p a b", a=4)))
            pw = psb[0]
            for b in range(4):
                kb.mm(pw.v(pw.t[32 * b:32 * b + 32, tl * 128:(tl + 1) * 128]), qss.v(qss.t[:, g, b, :]), kTs.v(kTs.t[:, b, :]), signal=(b == 3))
        pn = psb[1]
        for b in range(4):
            kb.mm(pn.v(pn.t[32 * b:32 * b + 32, 0:8]), qss.v(qss.t[:, g, b, :]), knT.v(knT.t[:, 1, b, g, :]), signal=(b == 3))
        exact_softmax([(psb[0][:], 512), (pn.v(pn.t[:, 0:8]), 8)], 520, wbs.v(wbs.t[:, g, :]), wms[:])
        transposeE(5, 520)
        po = psb[5]
        for b in range(4):
            for tl in range(5):
                if tl < 4:
                    rhs = vwS.v(vwS.t[:, tl, b, g, :])
                    lhs = PTs.v(PTs.t[:, tl, 32 * b:32 * b + 32])
                else:
                    rhs = vnew.v(vnew.t[:, 1, b, g, :])
                    lhs = PTs.v(PTs.t[0:8, 4, 32 * b:32 * b + 32])
                kb.mm(po.v(po.t[32 * b:32 * b + 32, 0:129]), lhs, rhs, start=(tl == 0), stop=(tl == 4), signal=(b == 3 and tl == 4))
        kb.ts("dve", cf_[:], po.v(po.t[:, 128:129]), 1e-30, None, ALU.max)
        kb.op("dve", lambda e: e.reciprocal(out=cf_.t[:], in_=cf_.t[:]), [cf_[:]], [cf_[:]])
        kb.tt("dve", cf_[:], cf_[:], grow.v(grow.t[:, g, 2:3]), ALU.mult)
        kb.stt("dve", attn_s.v(attn_s.t[:, g, :]), po.v(po.t[:, 0:128]), cf_.v(cf_.t[:, 0:1]), attn_s.v(attn_s.t[:, g, :]), ALU.mult, ALU.add)
        kb.memset("dve", mrun[g][:], -1e30)
        kb.memset("dve", oacc[g][:], 0.0)

    def online_update(g, ps_list, width, kc_val, mask_v, pv_fn, ntiles):
        S_sb, E_sb, PTs = S_sb2[g], E_sb2[g], PTs2[g]
        c0 = 0
        for (ps_v, w, add_v, is_iota) in ps_list:
            if is_iota:
                kb.stt("dve", S_sb.v(S_sb.t[:, c0:c0 + w]), V(add_v.ap[:, 0:w], add_v.buf), slrow.v(slrow.t[:, g:g + 1]), ps_v, ALU.mult, ALU.add)
            else:
                kb.tt("dve", S_sb.v(S_sb.t[:, c0:c0 + w]), ps_v, add_v, ALU.add)
            c0 += w
        kb.op("dve", lambda e: e.tensor_reduce(out=mc_.t[:], in_=S_sb.t[:, 0:width], axis=AX.X, op=ALU.max), [S_sb[:]], [mc_[:]])
        if kc_val is not None:
            kb.tt("dve", mc_[:], mc_[:], kc_val, ALU.add)
        kb.tt("dve", mn_[:], mc_[:], mrun[g][:], ALU.max)
        kb.tt("dve", al_[:], mrun[g][:], mn_[:], ALU.subtract)
        kb.act(al_[:], al_[:], AF.Exp)
        kb.copy("dve", mrun[g][:], mn_[:])
        if kc_val is not None:
            kb.tt("dve", bi_[:], kc_val, mn_[:], ALU.subtract)
        else:
            kb.ts("dve", bi_[:], mn_[:], -1.0, None, ALU.mult)
        kb.act(E_sb.v(E_sb.t[:, 0:width]), S_sb.v(S_sb.t[:, 0:width]), AF.Exp, bias=bi_[:], scale=1.0)
        kb.tt("dve", E_sb.v(E_sb.t[:, 0:width]), E_sb.v(E_sb.t[:, 0:width]), mask_v, ALU.mult)
        transposeE(ntiles, width, E_sb, PTs)
        po = psb[5]
        pv_fn(po, PTs)
        kb.stt("dve", oacc[g][:], oacc[g][:], al_.v(al_.t[:, 0:1]), po.v(po.t[:, 0:129]), ALU.mult, ALU.add)

    def prep_chunk(c):
        sb0 = 0 if c % 2 == 0 else 6
        for i in range(4):
            lp = 4 * c + i
            buf = pg[lp % 6]
            gather_pages(buf, cache_s, lp)
            src5 = buf.t[:].rearrange("p b (g c d) -> p b g c d", g=2, c=2)
            pgk, kTs, vsS = pgk2[lp % 2], kTs2[lp % 2], vsS2[c % 2]
            kb.copy("act", pgk[:], buf.v(src5[:, :, :, 0, :]))
            kb.copy("dve", vsS.v(vsS.t[:, i, :, :, 0:128]), buf.v(src5[:, :, :, 1, :]))
            for half in range(2):
                pt = psb[2 + half]
                ptv = bview(pt)
                for bb in range(2):
                    for g in range(2):
                        b = half * 2 + bb
                        kb.tr(pt.v(ptv[:, (bb * 2 + g) * 128:(bb * 2 + g + 1) * 128]), pgk.v(pgk.t[:, b, g, :]), ident_b[:], signal=(bb == 1 and g == 1))
                kb.copy("act" if half == 0 else "dve", kTs.v(kTs.t[:, half * 4:half * 4 + 4, :]), pt.v(ptv[:, 0:512].rearrange("p (a b) -> p a b", a=4)))
            for g in range(2):
                for b in range(4):
                    kb.mm(psb[sb0 + g].v(psb[sb0 + g].t[32 * b:32 * b + 32, i * 128:(i + 1) * 128]), qss.v(qss.t[:, g, b, :]), kTs.v(kTs.t[:, b * 2 + g, :]),
                          signal=(b == 3))

    def finish_chunk(c):
        sb0 = 0 if c % 2 == 0 else 6
        for g in range(2):
            mask_c = mask_c2[g]
            kb.ts("dve", kc_[:], slrow.v(slrow.t[:, g:g + 1]), float(512 * c - 16384), stok.v(stok.t[:, g:g + 1]), ALU.mult, ALU.add)
            kb.copy("dve", mask_c.v(mask_c.t[:].rearrange("p (b k) -> p b k", k=64)),
                    sels[g].v(sels[g].t[:, 8 * c:8 * c + 8].unsqueeze(2).to_broadcast([128, 8, 64])))

            def pv_fn(po, PTs, g=g, vsS=vsS2[c % 2]):
                for b in range(4):
                    for i in range(4):
                        kb.mm(po.v(po.t[32 * b:32 * b + 32, 0:129]), PTs.v(PTs.t[:, i, 32 * b:32 * b + 32]), vsS.v(vsS.t[:, i, b, g, :]),
                              start=(i == 0), stop=(i == 3), signal=(b == 3 and i == 3))
            online_update(g, [(psb[sb0 + g][:], 512, iota512[:], True)], 512, kc_[:], mask_c[:], pv_fn, 4)

    prep_chunk(0)
    for c in range(32):
        if c + 1 < 32:
            prep_chunk(c + 1)
        finish_chunk(c)
    for g in range(2):
        pn = psb[g]
        for b in range(4):
            kb.mm(pn.v(pn.t[32 * b:32 * b + 32, 0:8]), qss.v(qss.t[:, g, b, :]), knT.v(knT.t[:, 0, b, g, :]), signal=(b == 3))

        def pv_fn2(po, PTs, g=g):
            for b in range(4):
                kb.mm(po.v(po.t[32 * b:32 * b + 32, 0:129]), PTs.v(PTs.t[0:8, 0, 32 * b:32 * b + 32]), vnew.v(vnew.t[:, 0, b, g, :]),
                      start=True, stop=True, signal=(b == 3))
        online_update(g, [(pn.v(pn.t[:, 0:8]), 8, nbs.v(nbs.t[:, g, :]), False)], 8, None, nms[:], pv_fn2, 1)
        kb.ts("dve", cf_[:], oacc[g].v(oacc[g].t[:, 128:129]), 1e-30, None, ALU.max)
        kb.op("dve", lambda e: e.reciprocal(out=cf_.t[:], in_=cf_.t[:]), [cf_[:]], [cf_[:]])
        kb.tt("dve", cf_[:], cf_[:], grow.v(grow.t[:, g, 1:2]), ALU.mult)
        kb.stt("dve", attn_s.v(attn_s.t[:, g, :]), oacc[g].v(oacc[g].t[:, 0:128]), cf_.v(cf_.t[:, 0:1]), attn_s.v(attn_s.t[:, g, :]), ALU.mult, ALU.add)
    asb = kb.sb("asb", [128, 2, 128], BF16)
    kb.copy("pool", asb[:], attn_s[:])
    aTs = kb.sb("aTs", [128, 2, 128], BF16)
    pt = psb[2]
    ptv = bview(pt)
    for g in range(2):
        kb.tr(pt.v(ptv[:, g * 128:(g + 1) * 128]), asb.v(asb.t[:, g, :]), ident_b[:], signal=(g == 1))
    kb.copy("act", aTs[:], pt.v(ptv[:, 0:256].rearrange("p (a b) -> p a b", a=2)))
    for g in range(2):
        for b in range(4):
            for r in range(4):
                kb.dma("sp", scr_cat.v(scr_cat.t[:, 4 * g + r, 1024 + 8 * b:1024 + 8 * b + 8]),
                       aTs.v(aTs.t[:, g, 32 * b + 8 * r:32 * b + 8 * r + 8]))
    kb.phase_end()
    stage_gate(4)

    TILES = [(i * 128, 128) for i in range(8)] + [(1024, 32)]
    kb.phase_begin()
    acc = kb.sb("acc", [128, 9, D], F32)
    comb = kb.sb("comb", [128, 9, 16], F32)
    kb.phase_begin()
    catT = kb.sb("catT", [128, 16, NTOK], BF16)
    for k in range(16):
        kb.dma("sp", catT.v(catT.t[:, k, :]), scr_cat.v(scr_cat.t[:, k, :]))
    Wo = kb.sb("Wo", [128, 16, D], BF16)
    wo_view = w_out.t.rearrange("(k p) n -> p k n", p=128)
    for k in range(16):
        kb.dma("pool", Wo.v(Wo.t[:, k, :]), w_out.v(wo_view[:, k, :]))
    xs4 = kb.sb("xs4", [128, D], F32)
    for ti, (t0, rows) in enumerate(TILES):
        src = x_own.v(x_own.t[t0:t0 + rows, :]) if ti < 8 else x_s[:, :]
        kb.dma("sp", xs4.v(xs4.t[0:rows, :]), src)
        for cb in range(4):
            pb = psb[cb % 4]
            for k in range(16):
                kb.mm(pb.v(pb.t[0:rows, :]), catT.v(catT.t[:, k, t0:t0 + rows]), Wo.v(Wo.t[:, k, cb * 512:(cb + 1) * 512]),
                      start=(k == 0), stop=(k == 15), signal=(k == 15))
            kb.tt("dve", acc.v(acc.t[0:rows, ti, cb * 512:(cb + 1) * 512]), pb.v(pb.t[0:rows, :]), xs4.v(xs4.t[0:rows, cb * 512:(cb + 1) * 512]), ALU.add)
    kb.phase_end()
    stage_gate(5)
    kb.phase_begin()
    hnT = kb.sb("hnT", [128, 16, NTOK], BF16)
    kb.phase_begin()
    gffn = kb.sb("gffn", [128, D], F32)
    kb.dma("sp", gffn[:], norm_ffn.v(norm_ffn.t[0:1, :].partition_broadcast(128)))
    hn32 = kb.sb("hn32", [128, D], F32)
    junk = kb.sb("junk", [128, D], BF16)
    hn_hi = kb.sb("hn_hi", [128, D], BF16)
    hn_lo = kb.sb("hn_lo", [128, D], BF16)
    loT = kb.sb("loT", [128, 16, 128], BF16)
    wr = kb.sb("wr", [128, 16, 20], F32)
    wr_view = w_router.t.rearrange("(k p) n -> p k n", p=128)
    for k in range(16):
        kb.dma("sp", wr.v(wr.t[:, k, :]), w_router.v(wr_view[:, k, :]))
    whi = kb.sb("whi", [128, 16, 20], BF16)
    wlo = kb.sb("wlo", [128, 16, 20], BF16)
    kb.copy("pool", whi[:], wr[:])
    kb.tt("pool", wlo[:], wr[:], whi[:], ALU.subtract)
    br_t = kb.sb("br_t", [128, 20], F32)
    kb.dma("sp", br_t[:], b_router.v(b_router.t[0:1, :].partition_broadcast(128)))
    lg = kb.sb("lg", [128, 20], F32)
    gmax = kb.sb("gmax", [128, 1], F32)
    goh = kb.sb("goh", [128, 4], F32)
    gex = kb.sb("gex", [128, 4], F32)
    gsum = kb.sb("gsum", [128, 1], F32)
    el = kb.sb("el", [128, 4], F32)
    etmp = kb.sb("etmp", [128, 4, 4], F32)
    e1 = kb.sb("e1", [128, 1], F32)
    e2 = kb.sb("e2", [128, 1], F32)
    mk1 = kb.sb("mk1", [128, 4], F32)
    mk2 = kb.sb("mk2", [128, 4], F32)
    el2 = kb.sb("el2", [128, 4], F32)
    w1 = kb.sb("w1", [128, 1], F32)
    w2 = kb.sb("w2", [128, 1], F32)
    wv = kb.sb("wv", [128, 4], F32)
    for ti, (t0, rows) in enumerate(TILES):
        at = T(acc.t[:, ti, :], "acc_t")
        at.buf = acc.buf
        kb.act(junk.v(junk.t[0:rows, :]), at.v(at.t[0:rows, :]), AF.Square, accum=ssq.v(ssq.t[0:rows, :]))
        kb.act(rstd.v(rstd.t[0:rows, :]), ssq.v(ssq.t[0:rows, :]), AF.Sqrt, bias=eps_t.v(eps_t.t[0:rows, :]), scale=1.0 / D)
        kb.op("dve", lambda e: e.reciprocal(out=rstd.t[0:rows, :], in_=rstd.t[0:rows, :]), [rstd[:]], [rstd[:]])
        kb.stt("dve", hn32.v(hn32.t[0:rows, :]), at.v(at.t[0:rows, :]), rstd.v(rstd.t[0:rows, 0:1]), gffn.v(gffn.t[0:rows, :]), ALU.mult, ALU.mult)
        kb.copy("pool", hn_hi.v(hn_hi.t[0:rows, :]), hn32.v(hn32.t[0:rows, :]))
        kb.tt("pool", hn_lo.v(hn_lo.t[0:rows, :]), hn32.v(hn32.t[0:rows, :]), hn_hi.v(hn_hi.t[0:rows, :]), ALU.subtract)
        transpose16(hn_hi, rows, hnT, t0)
        transpose16(hn_lo, rows, loT, 0)
        pl = psb[4]
        for k in range(16):
            kb.mm(pl.v(pl.t[0:rows, 0:20]), hnT.v(hnT.t[:, k, t0:t0 + rows]), whi.v(whi.t[:, k, :]), start=(k == 0), stop=False, signal=False)
        for k in range(16):
            kb.mm(pl.v(pl.t[0:rows, 0:20]), hnT.v(hnT.t[:, k, t0:t0 + rows]), wlo.v(wlo.t[:, k, :]), start=False, stop=False, signal=False)
        for k in range(16):
            kb.mm(pl.v(pl.t[0:rows, 0:20]), loT.v(loT.t[:, k, 0:rows]), whi.v(whi.t[:, k, :]), start=False, stop=(k == 15), signal=(k == 15))
        R = slice(0, rows)
        kb.tt("dve", lg.v(lg.t[R, :]), pl.v(pl.t[R, 0:20]), br_t.v(br_t.t[R, :]), ALU.add)
        kb.op("dve", lambda e: e.tensor_reduce(out=gmax.t[R, :], in_=lg.t[R, 0:4], axis=AX.X, op=ALU.max), [lg[:]], [gmax[:]])
        kb.ts("dve", goh.v(goh.t[R, :]), lg.v(lg.t[R, 0:4]), gmax.v(gmax.t[R, 0:1]), None, ALU.is_ge)
        kb.ts("dve", gmax.v(gmax.t[R, :]), gmax.v(gmax.t[R, :]), -1.0, None, ALU.mult)
        kb.act(gex.v(gex.t[R, :]), lg.v(lg.t[R, 0:4]), AF.Exp, bias=gmax.v(gmax.t[R, 0:1]), scale=1.0)
        kb.op("dve", lambda e: e.tensor_reduce(out=gsum.t[R, :], in_=gex.t[R, :], axis=AX.X, op=ALU.add), [gex[:]], [gsum[:]])
        kb.op("dve", lambda e: e.reciprocal(out=gsum.t[R, :], in_=gsum.t[R, :]), [gsum[:]], [gsum[:]])
        kb.tt("dve", etmp.v(etmp.t[R]), lg.v(lg.t[R, 4:20].rearrange("p (g i) -> p g i", g=4)),
              goh.v(goh.t[R, :].unsqueeze(2).to_broadcast([rows, 4, 4])), ALU.mult)
        kb.op("dve", lambda e: e.tensor_reduce(out=el.t[R, :], in_=etmp.t[R].rearrange("p g i -> p i g"), axis=AX.X, op=ALU.add), [etmp[:]], [el[:]])
        kb.op("dve", lambda e: e.tensor_reduce(out=e1.t[R, :], in_=el.t[R, :], axis=AX.X, op=ALU.max), [el[:]], [e1[:]])
        kb.ts("dve", mk1.v(mk1.t[R, :]), el.v(el.t[R, :]), e1.v(e1.t[R, 0:1]), None, ALU.is_ge)
        kb.stt("dve", el2.v(el2.t[R, :]), mk1.v(mk1.t[R, :]), -1e30, el.v(el.t[R, :]), ALU.mult, ALU.add)
        kb.op("dve", lambda e: e.tensor_reduce(out=e2.t[R, :], in_=el2.t[R, :], axis=AX.X, op=ALU.max), [el2[:]], [e2[:]])
        kb.ts("dve", mk2.v(mk2.t[R, :]), el2.v(el2.t[R, :]), e2.v(e2.t[R, 0:1]), None, ALU.is_ge)
        kb.tt("dve", w2.v(w2.t[R, :]), e2.v(e2.t[R, :]), e1.v(e1.t[R, :]), ALU.subtract)
        kb.act(w2.v(w2.t[R, :]), w2.v(w2.t[R, :]), AF.Exp)
        kb.ts("dve", w1.v(w1.t[R, :]), w2.v(w2.t[R, :]), 1.0, None, ALU.add)
        kb.op("dve", lambda e: e.reciprocal(out=w1.t[R, :], in_=w1.t[R, :]), [w1[:]], [w1[:]])
        kb.tt("dve", w2.v(w2.t[R, :]), w2.v(w2.t[R, :]), w1.v(w1.t[R, :]), ALU.mult)
        kb.tt("dve", w1.v(w1.t[R, :]), w1.v(w1.t[R, :]), gsum.v(gsum.t[R, :]), ALU.mult)
        kb.tt("dve", w2.v(w2.t[R, :]), w2.v(w2.t[R, :]), gsum.v(gsum.t[R, :]), ALU.mult)
        kb.ts("dve", wv.v(wv.t[R, :]), mk1.v(mk1.t[R, :]), w1.v(w1.t[R, 0:1]), None, ALU.mult)
        kb.stt("dve", wv.v(wv.t[R, :]), mk2.v(mk2.t[R, :]), w2.v(w2.t[R, 0:1]), wv.v(wv.t[R, :]), ALU.mult, ALU.add)
        kb.tt("dve", comb.v(comb.t[R, ti, :].rearrange("p (g i) -> p g i", g=4)),
              goh.v(goh.t[R, :].unsqueeze(2).to_broadcast([rows, 4, 4])), wv.v(wv.t[R, :].unsqueeze(1).to_broadcast([rows, 4, 4])), ALU.mult)
    kb.phase_end()
    stage_gate(6)
    kb.phase_begin()
    wg2 = [kb.sb("wg%d" % i, [128, 16, 512], BF16) for i in range(2)]
    wu2 = [kb.sb("wu%d" % i, [128, 16, 512], BF16) for i in range(2)]
    wd = kb.sb("wd", [128, 4, D], BF16)
    hT = kb.sb("hT", [128, 4, NTOK], BF16)
    sgt = kb.sb("sgt", [128, 512], F32)
    for e_ in range(16):
        wg, wu = wg2[e_ % 2], wu2[e_ % 2]
        wgv = w_gate.t[e_].rearrange("(k p) n -> p k n", p=128)
        wuv = w_up.t[e_].rearrange("(k p) n -> p k n", p=128)
        wdv = w_down.t[e_].rearrange("(k p) n -> p k n", p=128)
        for k in range(16):
            kb.dma("pool", wg.v(wg.t[:, k, :]), w_gate.v(wgv[:, k, :]))
            kb.dma("pool", wu.v(wu.t[:, k, :]), w_up.v(wuv[:, k, :]))
        for k in range(4):
            kb.dma("pool", wd.v(wd.t[:, k, :]), w_down.v(wdv[:, k, :]))
        for fc in range(4):
            for (c0, n) in ((0, 512), (512, 512), (1024, 32)):
                pg, pu = psb[0 + (fc % 2) * 2], psb[1 + (fc % 2) * 2]
                for k in range(16):
                    kb.mm(pg.v(pg.t[:, 0:n]), wg.v(wg.t[:, k, fc * 128:(fc + 1) * 128]), hnT.v(hnT.t[:, k, c0:c0 + n]),
                          start=(k == 0), stop=(k == 15), signal=(k == 15))
                for k in range(16):
                    kb.mm(pu.v(pu.t[:, 0:n]), wu.v(wu.t[:, k, fc * 128:(fc + 1) * 128]), hnT.v(hnT.t[:, k, c0:c0 + n]),
                          start=(k == 0), stop=(k == 15), signal=(k == 15))
                kb.act(sgt.v(sgt.t[:, 0:n]), pg.v(pg.t[:, 0:n]), AF.Silu)
                kb.tt("dve", hT.v(hT.t[:, fc, c0:c0 + n]), sgt.v(sgt.t[:, 0:n]), pu.v(pu.t[:, 0:n]), ALU.mult)
        for ti, (t0, rows) in enumerate(TILES):
            for cb in range(4):
                po = psb[4 + cb % 4]
                for fc in range(4):
                    kb.mm(po.v(po.t[0:rows, :]), hT.v(hT.t[:, fc, t0:t0 + rows]), wd.v(wd.t[:, fc, cb * 512:(cb + 1) * 512]),
                          start=(fc == 0), stop=(fc == 3), signal=(fc == 3))
                dst = acc.v(acc.t[0:rows, ti, cb * 512:(cb + 1) * 512])
                kb.stt("dve", dst, po.v(po.t[0:rows, :]), comb.v(comb.t[0:rows, ti, e_:e_ + 1]), dst, ALU.mult, ALU.add)
    kb.phase_end()
    kb.phase_end()
    stage_gate(7)
    kb.phase_begin()
    gfin = kb.sb("gfin", [128, D], F32)
    kb.dma("sp", gfin[:], norm_final.v(norm_final.t[0:1, :].partition_broadcast(128)))
    yo = kb.sb("yo", [128, D], F32)
    junk2 = kb.sb("junk2", [128, D], BF16)
    for ti, (t0, rows) in enumerate(TILES):
        at = T(acc.t[:, ti, :], "acc_t2")
        at.buf = acc.buf
        kb.act(junk2.v(junk2.t[0:rows, :]), at.v(at.t[0:rows, :]), AF.Square, accum=ssq.v(ssq.t[0:rows, :]))
        kb.act(rstd.v(rstd.t[0:rows, :]), ssq.v(ssq.t[0:rows, :]), AF.Sqrt, bias=eps_t.v(eps_t.t[0:rows, :]), scale=1.0 / D)
        kb.op("dve", lambda e: e.reciprocal(out=rstd.t[0:rows, :], in_=rstd.t[0:rows, :]), [rstd[:]], [rstd[:]])
        kb.stt("dve", yo.v(yo.t[0:rows, :]), at.v(at.t[0:rows, :]), rstd.v(rstd.t[0:rows, 0:1]), gfin.v(gfin.t[0:rows, :]), ALU.mult, ALU.mult)
        kb.dma("sp", o_y.v(o_y.t[t0:t0 + rows, :]), yo.v(yo.t[0:rows, :]))
    kb.phase_end()
    kb.phase_end()


_CACHE = {}


def kernel(**inputs):
    g = lambda k: np.ascontiguousarray(inputs[k], dtype=np.float32)
    x_prompt = g("x_prompt")
    x_sample = g("x_sample")
    w_in = g("w_in")[0]
    if "nc" not in _CACHE:
        _CACHE["nc"] = build_program()
    nc = _CACHE["nc"]
    ident = np.eye(128, dtype=np.float32)
    chan = lambda v: v.reshape(8, 128).T
    rnn_par = np.zeros((128, 8, 8), np.float32)
    cw = g("conv_w")[0]
    for k in range(4):
        rnn_par[:, :, k] = chan(cw[k])
    rnn_par[:, :, 4] = chan(g("conv_b")[0])
    rnn_par[:, :, 5] = chan(g("lru_ba")[0])
    rnn_par[:, :, 6] = chan(g("lru_bx")[0])
    rnn_par[:, :, 7] = chan(g("lru_lambda")[0])

    def bd(w):
        o = np.zeros((128, 8, 128), np.float32)
        for m in range(8):
            o[0:64, m, 0:64] = w[2 * m]
            o[64:128, m, 64:128] = w[2 * m + 1]
        return o
    wa_bd, wx_bd = bd(g("lru_wa")[0]), bd(g("lru_wx")[0])
    wc = g("cmp_pool_w")[0]
    poolk = np.zeros((128, 4), np.float32)
    poolv = np.zeros((128, 252), np.float32)
    for t in range(128):
        poolk[t, t // 32] = wc[t % 32, 0]
        poolv[t, 124 + t // 32] = wc[t % 32, 1]
    ii = np.arange(128, dtype=np.float32)[:, None]
    cc = np.arange(640, dtype=np.float32)[None, :]
    distw = 512.0 + ii - cc
    negdw = np.minimum(-distw, 0.0).astype(np.float32)
    bandw = ((distw >= 0) & (distw <= 512)).astype(np.float32)
    w_router = np.ascontiguousarray(np.concatenate([g("w_router_group")[0], g("w_router_expert")[0]], axis=1))
    b_router = np.ascontiguousarray(np.concatenate([g("b_router_group")[0], g("b_router_expert")[0]], axis=0)[None, :])
    shared = {
        "w_in": w_in, "norm_mix": g("norm_mix"), "ident": ident,
        "rnn_par": rnn_par, "wa_bd": wa_bd, "wx_bd": wx_bd, "poolk": poolk, "poolv": poolv,
        "iota4096": np.arange(4096, dtype=np.float32)[None, :],
        "iota_ce": (np.arange(128, dtype=np.float32) * 32 + 31)[None, :],
        "iota_blk": np.arange(64, dtype=np.float32)[None, :],
        "negdw": negdw, "bandw": bandw,
        "w_out": g("w_out")[0], "norm_ffn": g("norm_ffn"), "norm_final": g("norm_final")[None, :],
        "w_router": w_router, "b_router": b_router,
        "w_gate": g("w_exp_gate")[0], "w_up": g("w_exp_up")[0], "w_down": g("w_exp_down")[0],
    }
    rows = np.arange(128)
    rb, rr, rt = rows // 32, (rows // 8) % 4, rows % 8
    sl = np.stack([2.0 ** (-(4 * gg + rr + 1)) for gg in range(2)], 1).astype(np.float64)
    jj = np.arange(512)
    cb_s = (-sl[:, :, None] * (16384 + rt[:, None, None] - (32 * jj[None, None, :] + 31))).astype(np.float32)
    cwi = np.arange(520)
    distw_s = np.where(cwi[None, :] < 512, 512 + rt[:, None] - cwi[None, :], rt[:, None] - (cwi[None, :] - 512))
    wm_s = ((distw_s >= 0) & (distw_s <= 512)).astype(np.float32)
    wb_s = (-sl[:, :, None] * np.maximum(distw_s, 0)[:, None, :]).astype(np.float32)
    nn = np.arange(8)
    nm_s = (nn[None, :] <= rt[:, None]).astype(np.float32)
    nb_s = (-sl[:, :, None] * np.maximum(rt[:, None] - nn[None, :], 0)[:, None, :]).astype(np.float32)
    rsum = ((rb[:, None] == rb[None, :]) & (rt[:, None] == rt[None, :])).astype(np.float32)
    forced_s = np.zeros((1, 257), np.float32)
    forced_s[0, [0, 255, 256]] = 1.0
    shared.update({
        "cache_c": np.asarray(inputs["cache_cmp_kv"], dtype=np.float32).reshape(5120 * 128, 512),
        "cache_s": np.asarray(inputs["cache_sel_kv"], dtype=np.float32).reshape(5120 * 128, 512),
        "pidx": np.arange(128, dtype=np.float32)[:, None],
        "cb_s": cb_s, "slope_rows": sl.astype(np.float32), "stok": (-sl * rt[:, None]).astype(np.float32),
        "wb_s": wb_s, "wm_s": wm_s, "nb_s": nb_s, "nm_s": nm_s, "rsum": rsum, "forced_s": forced_s,
        "iota512": np.arange(512, dtype=np.float32)[None, :],
    })
    page_table = np.asarray(inputs["page_table"], dtype=np.int32)
    in_maps = []
    for c in range(8):
        b, j = c // 4, c % 4
        tpos = (1024 * j + 128 * np.arange(8, dtype=np.float32)[None, :] + ii).astype(np.float32)
        m = dict(shared)
        m.update({
            "x_full": x_prompt[b],
            "x_own": np.ascontiguousarray(x_prompt[b, 1024 * j:1024 * (j + 1)]),
            "x_s": np.ascontiguousarray(x_sample[4 * c:4 * c + 4].reshape(32, D)),
            "onehot": np.tile(np.eye(4, dtype=np.float32)[j][None, :], (128, 1)),
            "st_h": np.ascontiguousarray(g("state_h")[0, 4 * c:4 * c + 4].reshape(4, 8, 128).transpose(2, 0, 1)),
            "st_conv": np.ascontiguousarray(g("state_conv")[0, 4 * c:4 * c + 4].reshape(4, 3, 8, 128).transpose(3, 0, 2, 1)),
            "cwin": np.ascontiguousarray(g("cache_win_kv")[0, 4 * c:4 * c + 4].reshape(4, 512, 512)),
            "tpos": tpos, "curb": np.floor(tpos / 64.0).astype(np.float32),
            "pt": np.ascontiguousarray(page_table[4 * c:4 * c + 4].reshape(1, 512)),
        })
        in_maps.append(m)
    res = run_bass_kernel_spmd(nc, in_maps, core_ids=list(range(8)))
    r = res.results
    kvp = np.stack([r[0]["o_kv_p"], r[4]["o_kv_p"]], 0)
    kvs = np.concatenate([r[c]["o_kv_s"] for c in range(8)], 0).reshape(32, 8, 1536)
    f = lambda a, i: np.ascontiguousarray(a[..., i * 512:(i + 1) * 512]).reshape(a.shape[:-1] + (2, 2, 128))[None]
    y_prompt = np.concatenate([r[c]["o_y"][0:1024] for c in range(8)], 0).reshape(2, 4096, 2048)
    y_sample = np.concatenate([r[c]["o_y"][1024:1056] for c in range(8)], 0).reshape(32, 8, 2048)
    new_cmp_p, new_sel_p = f(kvp, 0), f(kvp, 1)
    new_win_p = np.ascontiguousarray(f(kvp, 2)[:, :, -512:])
    new_cmp_s, new_sel_s = f(kvs, 0), f(kvs, 1)
    new_win_s = np.concatenate([r[c]["o_win_s"] for c in range(8)], 0).reshape(1, 32, 512, 2, 2, 128)
    new_conv_p = np.stack([r[4 * b]["o_conv_p"].transpose(2, 1, 0).reshape(3, 1024) for b in range(2)], 0)[None]
    new_conv_s = np.concatenate([r[c]["o_conv_s"].transpose(1, 3, 2, 0).reshape(4, 3, 1024) for c in range(8)], 0)[None]
    new_h_p = np.stack([r[4 * b]["o_h_p"].T.reshape(1024) for b in range(2)], 0)[None]
    new_h_s = np.concatenate([r[c]["o_h_s"].transpose(1, 2, 0).reshape(4, 1024) for c in range(8)], 0)[None]
    return (y_prompt, y_sample, new_cmp_p, new_cmp_s, new_sel_p, new_sel_s, new_win_p, new_win_s,
            new_conv_p, new_conv_s, new_h_p, new_h_s)
```

```python
import numpy as np
import concourse.bass as bass
import concourse.mybir as mybir
from concourse.bass_utils import run_bass_kernel_spmd
from contextlib import ExitStack

F32 = mybir.dt.float32
BF16 = mybir.dt.bfloat16
I32 = mybir.dt.int32
AF = mybir.ActivationFunctionType
ALU = mybir.AluOpType
AX = mybir.AxisListType

D = 2048
NQ = 1024
KVW = 512
D_IN = 4632
C_Q, C_KC, C_KS, C_KW, C_GT, C_XR, C_XG = 0, 1024, 1536, 2048, 2560, 2584, 3608
SEQ = 4096
EPS = 1e-6


class Buf:
    def __init__(self, name):
        self.name = name
        self.w = None
        self.r = {}


class V:
    def __init__(self, ap, buf):
        self.ap = ap
        self.buf = buf


class T:
    def __init__(self, t, name):
        self.t = t
        self.buf = Buf(name)

    def __getitem__(self, idx):
        return V(self.t[idx], self.buf)

    def v(self, ap):
        return V(ap, self.buf)


class Eng:
    def __init__(self, kb, name, e, sem):
        self.kb = kb
        self.name = name
        self.e = e
        self.sem = sem
        self.count = 0
        self.waited = {}
        self.pend_r = []
        self.pend_w = []

    def wait(self, tok):
        if tok is None:
            return
        sem, val, owner = tok
        if owner is self and self.name == "pe":
            return
        key = id(sem)
        if self.waited.get(key, 0) >= val:
            return
        self.e.wait_ge(sem, val)
        self.waited[key] = val


class KB:
    def __init__(self, nc, es):
        self.nc = nc
        self.es = es
        self.eng = {}
        for name, e in (("pe", nc.tensor), ("act", nc.scalar), ("dve", nc.vector), ("pool", nc.gpsimd), ("sp", nc.sync)):
            sem = es.enter_context(nc.semaphore("sem_" + name))
            self.eng[name] = Eng(self, name, e, sem)
        self.dma_sems = []
        for i in range(24):
            self.dma_sems.append([es.enter_context(nc.semaphore("dsem%d" % i)), 0, None])
        self.dma_rr = 0
        self.all_bufs = []
        self.stack = [es]
        self.ntile = 0

    def sb(self, name, shape, dt, glob=False):
        if glob or len(self.stack) == 1:
            t = self.stack[0].enter_context(self.nc.sbuf_tensor(name, shape, dt, side="right"))
        else:
            t = self.stack[-1].enter_context(self.nc.sbuf_tensor(name, shape, dt))
        T_ = T(t, name)
        self.all_bufs.append(T_.buf)
        return T_

    def phase_begin(self):
        es = ExitStack()
        es.__enter__()
        self.stack.append(es)

    def phase_end(self):
        self.barrier()
        es = self.stack.pop()
        es.__exit__(None, None, None)

    def barrier(self):
        toks = []
        for F in self.eng.values():
            if F.count > 0:
                toks.append((F.sem, F.count, F))
        for slot in self.dma_sems:
            if slot[2] is not None:
                toks.append(slot[2])
        for E in self.eng.values():
            for tok in toks:
                if tok[2] is E:
                    continue
                E.wait(tok)

    def ps(self, name, shape, dt=F32):
        t = self.es.enter_context(self.nc.psum_tensor(name, shape, dt))
        T_ = T(t, name)
        self.all_bufs.append(T_.buf)
        return T_

    def dram(self, name, shape, dt, kind="Internal"):
        t = self.nc.dram_tensor(name, shape, dt, kind=kind)
        T_ = T(t.ap(), name)
        self.all_bufs.append(T_.buf)
        return T_

    def _pre(self, E, reads, writes):
        for b in reads:
            E.wait(b.w)
        for b in writes:
            E.wait(b.w)
            for tok in b.r.values():
                E.wait(tok)

    def _post(self, tok, reads, writes):
        for b in reads:
            b.r[id(tok[0])] = tok
        for b in writes:
            b.w = tok
            b.r = {}

    def op(self, eng, fn, reads, writes, signal=True):
        E = self.eng[eng]
        rb = [v.buf for v in reads if v is not None]
        wb = [v.buf for v in writes if v is not None]
        self._pre(E, rb, wb)
        ins = fn(E.e)
        if signal:
            E.count += 1
            ins.then_inc(E.sem, 1)
            tok = (E.sem, E.count, E)
            self._post(tok, rb + E.pend_r, wb + E.pend_w)
            E.pend_r = []
            E.pend_w = []
        else:
            E.pend_r += rb
            E.pend_w += wb
        return ins

    def dma(self, q, out, in_, fn=None):
        E = self.eng[q]
        rb, wb = [in_.buf], [out.buf]
        self._pre(E, rb, wb)
        slot = self.dma_sems[self.dma_rr % len(self.dma_sems)]
        self.dma_rr += 1
        if slot[2] is not None:
            E.wait(slot[2])
        if fn is None:
            ins = E.e.dma_start(out=out.ap, in_=in_.ap)
        else:
            ins = fn(E.e)
        slot[1] += 16
        ins.then_inc(slot[0], 16)
        tok = (slot[0], slot[1], None)
        slot[2] = tok
        self._post(tok, rb, wb)
        return tok

    def finish(self):
        E = self.eng["sp"]
        for b in self.all_bufs:
            E.wait(b.w)
            for tok in b.r.values():
                E.wait(tok)
        for slot in self.dma_sems:
            if slot[2] is not None:
                E.wait(slot[2])

    def mm(self, out, lhsT, rhs, start=True, stop=True, signal=True):
        kw = {}
        try:
            out.ap.base_partition()
        except AssertionError:
            kw["tile_position"] = (0, 96)
        return self.op("pe", lambda e: e.matmul(out.ap, lhsT.ap, rhs.ap, start=start, stop=stop,
                                                 skip_group_check=True, **kw),
                       [lhsT, rhs], [out], signal=signal)

    def tr(self, out, in_, ident, signal=True):
        return self.op("pe", lambda e: e.transpose(out.ap, in_.ap, ident.ap), [in_, ident], [out], signal=signal)

    def act(self, out, in_, func, bias=None, scale=None, accum=None, eng="act"):
        kw = {}
        rd = [in_]
        if bias is not None:
            if isinstance(bias, V):
                kw["bias"] = bias.ap
                rd.append(bias)
            else:
                kw["bias"] = bias
        if scale is not None:
            if isinstance(scale, V):
                kw["scale"] = scale.ap
                rd.append(scale)
            else:
                kw["scale"] = scale
        wr = [out]
        if accum is not None:
            kw["accum_out"] = accum.ap
            wr.append(accum)
        return self.op(eng, lambda e: e.activation(out.ap, in_.ap, func, **kw), rd, wr)

    def copy(self, eng, out, in_):
        if eng == "act":
            return self.op("act", lambda e: e.copy(out.ap, in_.ap), [in_], [out])
        return self.op(eng, lambda e: e.tensor_copy(out=out.ap, in_=in_.ap), [in_], [out])

    def memset(self, eng, out, val):
        return self.op(eng, lambda e: e.memset(out.ap, val), [], [out])

    def tt(self, eng, out, a, b, op):
        return self.op(eng, lambda e: e.tensor_tensor(out=out.ap, in0=a.ap, in1=b.ap, op=op), [a, b], [out])

    def ts(self, eng, out, a, s1, s2, op0, op1=None, accum=None):
        rd = [a]
        x1 = s1.ap if isinstance(s1, V) else s1
        x2 = s2.ap if isinstance(s2, V) else s2
        if isinstance(s1, V):
            rd.append(s1)
        if isinstance(s2, V):
            rd.append(s2)
        kw = {}
        wr = [out]
        if op1 is not None:
            kw["op1"] = op1
        if accum is not None:
            kw["accum_out"] = accum.ap
            wr.append(accum)
        return self.op(eng, lambda e: e.tensor_scalar(out=out.ap, in0=a.ap, scalar1=x1, scalar2=x2, op0=op0, **kw),
                       rd, wr)

    def stt(self, eng, out, a, s, b, op0, op1):
        rd = [a, b]
        x = s.ap if isinstance(s, V) else s
        if isinstance(s, V):
            rd.append(s)
        return self.op(eng, lambda e: e.scalar_tensor_tensor(out=out.ap, in0=a.ap, scalar=x, in1=b.ap, op0=op0, op1=op1),
                       rd, [out])


def build_program():
    nc = bass.Bass("TRN2", target_bir_lowering=False)
    es = ExitStack()
    with es:
        kb = KB(nc, es)
        try:
            _build(nc, kb)
        except StopBuild:
            while len(kb.stack) > 1:
                kb.phase_end()
        kb.finish()
    return nc


class StopBuild(Exception):
    pass


import os
STAGE = int(os.environ.get("KSTAGE", "99"))


def stage_gate(n):
    if STAGE < n:
        raise StopBuild()


SLOPES = [2.0 ** (-(h + 1)) for h in range(8)]
NTOK = 1056


def _build(nc, kb):
    dram_in = lambda name, shape, dt=F32: kb.dram(name, shape, dt, kind="ExternalInput")
    dram_out = lambda name, shape, dt=F32: kb.dram(name, shape, dt, kind="ExternalOutput")
    x_full = dram_in("x_full", [SEQ, D])
    x_own = dram_in("x_own", [1024, D])
    x_s = dram_in("x_s", [32, D])
    w_in = dram_in("w_in", [D, D_IN])
    norm_mix = dram_in("norm_mix", [1, D])
    ident_in = dram_in("ident", [128, 128])
    rnn_par_in = dram_in("rnn_par", [128, 8, 8])
    wa_bd_in = dram_in("wa_bd", [128, 8, 128])
    wx_bd_in = dram_in("wx_bd", [128, 8, 128])
    onehot_in = dram_in("onehot", [128, 4])
    st_h_in = dram_in("st_h", [128, 4, 8])
    st_conv_in = dram_in("st_conv", [128, 4, 8, 3])
    cwin_in = dram_in("cwin", [4, 512, 512])
    poolk_in = dram_in("poolk", [128, 4])
    poolv_in = dram_in("poolv", [128, 252])
    tpos_in = dram_in("tpos", [128, 8])
    curb_in = dram_in("curb", [128, 8])
    iota_in = dram_in("iota4096", [1, 4096])
    iotace_in = dram_in("iota_ce", [1, 128])
    iotablk_in = dram_in("iota_blk", [1, 64])
    negdw_in = dram_in("negdw", [128, 640])
    bandw_in = dram_in("bandw", [128, 640])
    w_out = dram_in("w_out", [D, D])
    norm_ffn = dram_in("norm_ffn", [1, D])
    norm_final = dram_in("norm_final", [1, D])
    w_router = dram_in("w_router", [D, 20])
    b_router = dram_in("b_router", [1, 20])
    w_gate = dram_in("w_gate", [16, D, 512])
    w_up = dram_in("w_up", [16, D, 512])
    w_down = dram_in("w_down", [16, 512, D])
    cache_c = dram_in("cache_c", [5120 * 128, 512])
    cache_s = dram_in("cache_s", [5120 * 128, 512])
    pt_in = dram_in("pt", [1, 512], I32)
    pidx_in = dram_in("pidx", [128, 1])
    cbs_in = dram_in("cb_s", [128, 2, 512])
    slrow_in = dram_in("slope_rows", [128, 2])
    stok_in = dram_in("stok", [128, 2])
    wbs_in = dram_in("wb_s", [128, 2, 520])
    wms_in = dram_in("wm_s", [128, 520])
    nbs_in = dram_in("nb_s", [128, 2, 8])
    nms_in = dram_in("nm_s", [128, 8])
    rsum_in = dram_in("rsum", [128, 128])
    forced_in = dram_in("forced_s", [1, 257])
    iota512_in = dram_in("iota512", [1, 512])
    o_h_p = dram_out("o_h_p", [128, 8])
    o_h_s = dram_out("o_h_s", [128, 4, 8])
    o_conv_s = dram_out("o_conv_s", [128, 4, 8, 3])
    o_win_s = dram_out("o_win_s", [4, 512, 512])
    o_conv_p = dram_out("o_conv_p", [128, 8, 3])
    o_kv_p = dram_out("o_kv_p", [SEQ, 3 * KVW])
    o_kv_s = dram_out("o_kv_s", [32, 3 * KVW])
    o_y = dram_out("o_y", [NTOK, D])
    scr_kT = kb.dram("scr_kT", [2, 2, 128, SEQ], BF16)
    scr_v = kb.dram("scr_v", [2, 32, 128, 2, 128], BF16)
    scr_q = kb.dram("scr_q", [128, 8, NTOK], BF16)
    scr_cat = kb.dram("scr_cat", [128, 16, NTOK], BF16)
    scr_kvs = kb.dram("scr_kvs", [32, 3 * KVW], BF16)
    scr_g = kb.dram("scr_g", [32, 24], F32)

    ident_f = kb.sb("ident_f", [128, 128], F32)
    ident_b = kb.sb("ident_b", [128, 128], BF16)
    kb.dma("sp", ident_f[:], ident_in[:])
    kb.copy("dve", ident_b[:], ident_f[:])
    eps_t = kb.sb("eps_t", [128, 1], F32)
    kb.memset("dve", eps_t[:], EPS)
    one_t = kb.sb("one_t", [128, 1], F32)
    kb.memset("dve", one_t[:], 1.0)
    onehot = kb.sb("onehot_sb", [128, 4], F32)
    kb.dma("sp", onehot[:], onehot_in[:])
    gates_sb = kb.sb("gates_sb", [128, 9, 24], F32)
    h_s_all = kb.sb("h_s_all", [128, 8, 32], F32)
    kcmpT_sb = kb.sb("kcmpT_sb", [128, 2, 128], BF16)
    vcmp_sb = kb.sb("vcmp_sb", [128, 2, 128], BF16)
    ssq = kb.sb("ssq", [128, 1], F32)
    rstd = kb.sb("rstd", [128, 1], F32)
    psb = [kb.ps("psb%d" % i, [128, 512], F32) for i in range(8)]

    def bview(pb):
        return pb.t[:].bitcast(BF16)

    kb.phase_begin()
    gmix = kb.sb("gmix", [128, D], F32)
    kb.dma("sp", gmix[:], norm_mix.v(norm_mix.t[0:1, :].partition_broadcast(128)))
    xs = kb.sb("xs", [128, D], F32)
    xn = kb.sb("xn", [128, D], BF16)
    xnT = kb.sb("xnT", [128, 16, 512], BF16)
    xnT_b = kb.sb("xnT_b", [128, 16, 512], BF16)
    xnT2 = [xnT, xnT_b]
    own_h = kb.sb("own_h", [128, 8, 1024], BF16)
    kb.memset("pool", own_h[:], 0.0)

    def rms_rows(src_tile, nrows, gam, dst_bf, junk):
        kb.act(junk.v(junk.t[0:nrows, :]), src_tile.v(src_tile.t[0:nrows, :]), AF.Square, accum=ssq.v(ssq.t[0:nrows, :]))
        kb.act(rstd.v(rstd.t[0:nrows, :]), ssq.v(ssq.t[0:nrows, :]), AF.Sqrt, bias=eps_t.v(eps_t.t[0:nrows, :]), scale=1.0 / D)
        kb.op("dve", lambda e: e.reciprocal(out=rstd.t[0:nrows, :], in_=rstd.t[0:nrows, :]), [rstd[:]], [rstd[:]])
        kb.stt("dve", dst_bf.v(dst_bf.t[0:nrows, :]), src_tile.v(src_tile.t[0:nrows, :]), rstd.v(rstd.t[0:nrows, 0:1]),
               gam.v(gam.t[0:nrows, :]), ALU.mult, ALU.mult)

    def transpose16(src_bf, nrows, dstT, col0):
        for q4 in range(4):
            pb = psb[q4]
            pbv = bview(pb)
            for kk in range(4):
                k = q4 * 4 + kk
                kb.tr(pb.v(pbv[:, kk * 128: kk * 128 + nrows]), src_bf.v(src_bf.t[0:nrows, k * 128:(k + 1) * 128]),
                      ident_b.v(ident_b.t[0:nrows, 0:nrows]), signal=(kk == 3))
            src = pbv[:, 0:512].rearrange("p (a b) -> p a b", a=4)[:, :, 0:nrows]
            dst = dstT.t[:, q4 * 4:(q4 + 1) * 4, col0:col0 + nrows]
            kb.copy("act" if q4 % 2 == 0 else "dve", dstT.v(dst), pb.v(src))

    def norm_transpose(src_rows, nrows, dstT, col0):
        kb.dma("sp", xs.v(xs.t[0:nrows, :]), src_rows)
        rms_rows(xs, nrows, gmix, xn, xn)
        transpose16(xn, nrows, dstT, col0)

    w_view = w_in.t.rearrange("(k p) n -> p k n", p=128)

    kb.phase_begin()
    W = kb.sb("W", [128, 16, 2560], BF16)
    for k in range(16):
        kb.dma("pool", W.v(W.t[:, k, 0:1536]), w_in.v(w_view[:, k, C_KC:C_KC + 1536]))
        kb.dma("pool", W.v(W.t[:, k, 1536:2560]), w_in.v(w_view[:, k, C_XR:C_XR + 1024]))
    kvf = kb.sb("kvf", [128, 3 * KVW], F32)
    kvb = kb.sb("kvb", [128, 3 * KVW], BF16)
    kTt = kb.sb("kTt", [128, 4, 128], BF16)
    rpar = kb.sb("rpar", [128, 8, 8], F32)
    kb.dma("sp", rpar[:], rnn_par_in[:])
    wa_bd = kb.sb("wa_bd_sb", [128, 8, 128], BF16)
    wx_bd = kb.sb("wx_bd_sb", [128, 8, 128], BF16)
    kb.dma("pool", wa_bd[:], wa_bd_in[:])
    kb.dma("pool", wx_bd[:], wx_bd_in[:])
    poolk = kb.sb("poolk_sb", [128, 4], BF16)
    poolv = kb.sb("poolv_sb", [128, 252], BF16)
    kb.dma("pool", poolk[:], poolk_in[:])
    kb.dma("pool", poolv[:], poolv_in[:])
    cdec = kb.sb("cdec", [128, 8], F32)
    kb.act(cdec[:], rpar.v(rpar.t[:, :, 7]), AF.Exp, scale=-1.0)
    kb.act(cdec[:], cdec[:], AF.Ln, bias=one_t[:], scale=1.0)
    kb.ts("dve", cdec[:], cdec[:], -8.0, None, ALU.mult)
    xp = [kb.sb("xp%d" % m, [128, 515], F32) for m in range(8)]
    hprev = kb.sb("hprev", [128, 8], F32)
    kb.memset("dve", hprev[:], 0.0)
    for m in range(8):
        kb.memset("pool", xp[m][:, 0:3], 0.0)
    r_xc = kb.sb("r_xc", [128, 512], F32)
    r_xcb = kb.sb("r_xcb", [128, 512], BF16)
    r_r = kb.sb("r_r", [128, 512], F32)
    r_i = kb.sb("r_i", [128, 512], F32)
    r_s = kb.sb("r_s", [128, 512], F32)
    r_u = kb.sb("r_u", [128, 512], F32)
    r_h = kb.sb("r_h", [128, 512], F32)
    kb.memset("dve", psb[7][:], 0.0)

    def kv_tile(xT, col0, nrows, out_rows, tile_idx):
        for blk in range(3):
            pb = psb[4 + (blk % 2)]
            for k in range(16):
                kb.mm(pb.v(pb.t[0:nrows, :]), xT.v(xT.t[:, k, col0:col0 + nrows]), W.v(W.t[:, k, blk * 512:(blk + 1) * 512]),
                      start=(k == 0), stop=(k == 15), signal=(k == 15))
            kb.copy("act" if blk % 2 == 0 else "dve", kvf.v(kvf.t[0:nrows, blk * 512:(blk + 1) * 512]), pb.v(pb.t[0:nrows, :]))
        kb.dma("sp", out_rows, kvf.v(kvf.t[0:nrows, :]))
        if tile_idx is None:
            return
        kb.copy("pool", kvb[:], kvf[:])
        for br in range(2):
            src = kvb.t[:, 512 * (br + 1):512 * (br + 2)].rearrange("p (g c d) -> p g c d", g=2, c=2)[:, :, 1, :]
            kb.dma("sp", scr_v.v(scr_v.t[br, tile_idx]), kvb.v(src))
        pb = psb[4]
        pbv = bview(pb)
        for br in range(2):
            for g in range(2):
                c = 512 * (br + 1) + g * 256
                kb.tr(pb.v(pbv[:, (br * 2 + g) * 128:(br * 2 + g + 1) * 128]), kvb.v(kvb.t[:, c:c + 128]), ident_b[:],
                      signal=(br == 1 and g == 1))
        kb.copy("act", kTt[:], pb.v(pbv[:, 0:512].rearrange("p (a b) -> p a b", a=4)))
        dst = scr_kT.t[:, :, :, tile_idx * 128:(tile_idx + 1) * 128].rearrange("b g d t -> d (b g) t")
        kb.dma("sp", scr_kT.v(dst), kTt[:])
        p7 = psb[7]
        for g in range(2):
            kb.mm(p7.v(p7.t[:, g * 128 + 4 * tile_idx: g * 128 + 4 * tile_idx + 4]), kvb.v(kvb.t[:, g * 256:g * 256 + 128]), poolk[:],
                  start=False, stop=False, signal=False)
        vsrc = kvb.t[:, 0:512].rearrange("p (g c d) -> p g c d", g=2, c=2)[:, :, 1, :]
        kb.mm(p7.v(p7.t[:, 256:512].rearrange("p (g d) -> p g d", g=2)), poolv.v(poolv.t[:, 124 - 4 * tile_idx:252 - 4 * tile_idx]), kvb.v(vsrc),
              start=False, stop=False, signal=True)

    def rnn_group(xT, n, hinit, xpl, sel_dst, c0=0, ms=range(8)):
        for m in ms:
            pb = psb[6]
            for k in range(16):
                kb.mm(pb.v(pb.t[:, 0:n]), W.v(W.t[:, k, 1536 + m * 128:1536 + (m + 1) * 128]), xT.v(xT.t[:, k, c0:c0 + n]),
                      start=(k == 0), stop=(k == 15), signal=(k == 15))
            xpm = xpl[m]
            kb.copy("act", xpm.v(xpm.t[:, 3:3 + n]), pb.v(pb.t[:, 0:n]))
            kb.ts("dve", r_xc.v(r_xc.t[:, 0:n]), xpm.v(xpm.t[:, 0:n]), rpar.v(rpar.t[:, m, 0:1]), rpar.v(rpar.t[:, m, 4:5]), ALU.mult, ALU.add)
            for kk in range(1, 4):
                kb.stt("dve", r_xc.v(r_xc.t[:, 0:n]), xpm.v(xpm.t[:, kk:kk + n]), rpar.v(rpar.t[:, m, kk:kk + 1]), r_xc.v(r_xc.t[:, 0:n]), ALU.mult, ALU.add)
            kb.copy("pool", xpm.v(xpm.t[:, 0:3]), xpm.v(xpm.t[:, n:n + 3]))
            kb.copy("pool", r_xcb.v(r_xcb.t[:, 0:n]), r_xc.v(r_xc.t[:, 0:n]))
            pa = psb[4]
            kb.mm(pa.v(pa.t[:, 0:n]), wa_bd.v(wa_bd.t[:, m, :]), r_xcb.v(r_xcb.t[:, 0:n]))
            kb.act(r_r.v(r_r.t[:, 0:n]), pa.v(pa.t[:, 0:n]), AF.Sigmoid, bias=rpar.v(rpar.t[:, m, 5:6]), scale=1.0)
            px = psb[5]
            kb.mm(px.v(px.t[:, 0:n]), wx_bd.v(wx_bd.t[:, m, :]), r_xcb.v(r_xcb.t[:, 0:n]))
            kb.act(r_i.v(r_i.t[:, 0:n]), px.v(px.t[:, 0:n]), AF.Sigmoid, bias=rpar.v(rpar.t[:, m, 6:7]), scale=1.0)
            kb.act(r_r.v(r_r.t[:, 0:n]), r_r.v(r_r.t[:, 0:n]), AF.Exp, scale=cdec.v(cdec.t[:, m:m + 1]))
            kb.tt("pool", r_s.v(r_s.t[:, 0:n]), r_r.v(r_r.t[:, 0:n]), r_r.v(r_r.t[:, 0:n]), ALU.mult)
            kb.act(r_s.v(r_s.t[:, 0:n]), r_s.v(r_s.t[:, 0:n]), AF.Sqrt, bias=one_t[:], scale=-1.0)
            kb.tt("pool", r_u.v(r_u.t[:, 0:n]), r_i.v(r_i.t[:, 0:n]), r_xc.v(r_xc.t[:, 0:n]), ALU.mult)
            kb.tt("pool", r_u.v(r_u.t[:, 0:n]), r_u.v(r_u.t[:, 0:n]), r_s.v(r_s.t[:, 0:n]), ALU.mult)
            kb.op("dve", lambda e: e.tensor_tensor_scan(out=r_h.t[:, 0:n], data0=r_r.t[:, 0:n], data1=r_u.t[:, 0:n],
                                                         initial=hinit.t[:, m:m + 1], op0=ALU.mult, op1=ALU.add),
                  [r_r[:], r_u[:], hinit[:]], [r_h[:]])
            kb.copy("dve", hinit.v(hinit.t[:, m:m + 1]), r_h.v(r_h.t[:, n - 1:n]))
            sel_dst(m, r_h)

    def own_sel(G):
        def f(m, rh):
            dst = own_h.v(own_h.t[:, m, (G % 2) * 512:(G % 2) * 512 + 512])
            kb.stt("dve", dst, rh[:], onehot.v(onehot.t[:, G // 2:G // 2 + 1]), dst, ALU.mult, ALU.add)
        return f

    def front_unit(G, u):
        xt = xnT2[G % 2]
        i = u % 4
        r0 = G * 512 + i * 128
        if u < 4:
            norm_transpose(x_full.v(x_full.t[r0:r0 + 128, :]), 128, xt, i * 128)
        else:
            kv_tile(xt, i * 128, 128, o_kv_p.v(o_kv_p.t[r0:r0 + 128, :]), G * 4 + i)

    for u in range(8):
        front_unit(0, u)
    for G in range(8):
        for u in range(8):
            if G + 1 < 8:
                front_unit(G + 1, u)
            rnn_group(xnT2[G % 2], 512, hprev, xp, own_sel(G), ms=[u])
    kb.dma("sp", o_h_p[:], hprev[:])
    for m in range(8):
        kb.dma("sp", o_conv_p.v(o_conv_p.t[:, m, :]), xp[m].v(xp[m].t[:, 0:3]))
    kb.copy("act", kcmpT_sb[:], psb[7].v(psb[7].t[:, 0:256].rearrange("p (g b) -> p g b", g=2)))
    kb.copy("dve", vcmp_sb[:], psb[7].v(psb[7].t[:, 256:512].rearrange("p (g d) -> p g d", g=2)))
    norm_transpose(x_s[:, :], 32, xnT, 0)
    kv_tile(xnT, 0, 32, o_kv_s[:, :], None)
    kb.copy("pool", kvb.v(kvb.t[0:32, :]), kvf.v(kvf.t[0:32, :]))
    kb.dma("sp", scr_kvs[:], kvb.v(kvb.t[0:32, :]))
    for q in range(4):
        kb.dma("sp", o_win_s.v(o_win_s.t[q, 0:504, :]), cwin_in.v(cwin_in.t[q, 8:512, :]))
        kb.dma("sp", o_win_s.v(o_win_s.t[q, 504:512, :]), kvf.v(kvf.t[8 * q:8 * q + 8, 1024:1536]))
    hs = kb.sb("hs", [128, 4, 8], F32)
    kb.dma("sp", hs[:], st_h_in[:])
    xps = [[kb.sb("xps%d_%d" % (q, m), [128, 16], F32) for m in range(8)] for q in range(4)]
    stc = kb.sb("stc", [128, 4, 8, 3], F32)
    kb.dma("sp", stc[:], st_conv_in[:])
    for q in range(4):
        for m in range(8):
            kb.copy("pool", xps[q][m].v(xps[q][m].t[:, 0:3]), stc.v(stc.t[:, q, m, :]))
        hq = T(hs.t[:, q, :], "hsq")
        hq.buf = hs.buf

        def keep(m, rh, q=q):
            kb.copy("pool", h_s_all.v(h_s_all.t[:, m, 8 * q:8 * q + 8]), rh.v(rh.t[:, 0:8]))
        rnn_group(xnT, 8, hq, xps[q], keep, c0=8 * q)
        for m in range(8):
            kb.dma("sp", o_conv_s.v(o_conv_s.t[:, q, m, :]), xps[q][m].v(xps[q][m].t[:, 0:3]))
    kb.dma("sp", o_h_s[:], hs[:])
    kb.phase_end()
    stage_gate(2)

    kb.phase_begin()
    W2 = kb.sb("W2", [128, 16, 2072], BF16)
    for k in range(16):
        kb.dma("pool", W2.v(W2.t[:, k, 0:1024]), w_in.v(w_view[:, k, C_Q:C_Q + 1024]))
        kb.dma("pool", W2.v(W2.t[:, k, 1024:1048]), w_in.v(w_view[:, k, C_GT:C_GT + 24]))
        kb.dma("pool", W2.v(W2.t[:, k, 1048:2072]), w_in.v(w_view[:, k, C_XG:C_XG + 1024]))
    qTt = kb.sb("qTt", [128, 8, 512], BF16)
    g_x = kb.sb("g_x", [128, 512], F32)
    g_t = kb.sb("g_t", [128, 512], F32)
    g_o = kb.sb("g_o", [128, 512], BF16)
    hsb = kb.sb("hsb", [128, 8, 32], BF16)
    kb.copy("pool", hsb[:], h_s_all[:])
    for G in range(3):
        n = 512 if G < 2 else 32
        t0 = G * 512
        if G < 2:
            for i in range(4):
                norm_transpose(x_own.v(x_own.t[t0 + i * 128:t0 + (i + 1) * 128, :]), 128, xnT, i * 128)
        else:
            norm_transpose(x_s[:, :], 32, xnT, 0)
        for h in range(8):
            pb = psb[4 + (h % 2)]
            for k in range(16):
                kb.mm(pb.v(pb.t[:, 0:n]), W2.v(W2.t[:, k, h * 128:(h + 1) * 128]), xnT.v(xnT.t[:, k, 0:n]),
                      start=(k == 0), stop=(k == 15), signal=(k == 15))
            kb.act(qTt.v(qTt.t[:, h, 0:n]), pb.v(pb.t[:, 0:n]), AF.Copy, scale=float(128 ** -0.5))
        kb.dma("sp", scr_q.v(scr_q.t[:, :, t0:t0 + n]), qTt.v(qTt.t[:, :, 0:n]))
        for i in range((n + 127) // 128):
            rows = min(128, n - i * 128)
            pb = psb[6]
            for k in range(16):
                kb.mm(pb.v(pb.t[0:rows, 0:24]), xnT.v(xnT.t[:, k, i * 128:i * 128 + rows]), W2.v(W2.t[:, k, 1024:1048]),
                      start=(k == 0), stop=(k == 15), signal=(k == 15))
            kb.act(gates_sb.v(gates_sb.t[0:rows, G * 4 + i, :]), pb.v(pb.t[0:rows, 0:24]), AF.Sigmoid)
        for m in range(8):
            pb = psb[4 + (m % 2)]
            for k in range(16):
                kb.mm(pb.v(pb.t[:, 0:n]), W2.v(W2.t[:, k, 1048 + m * 128:1048 + (m + 1) * 128]), xnT.v(xnT.t[:, k, 0:n]),
                      start=(k == 0), stop=(k == 15), signal=(k == 15))
            gx, gt, go = g_x.v(g_x.t[:, 0:n]), g_t.v(g_t.t[:, 0:n]), g_o.v(g_o.t[:, 0:n])
            kb.copy("act", gx, pb.v(pb.t[:, 0:n]))
            kb.tt("pool", gt, gx, gx, ALU.mult)
            kb.ts("dve", gt, gt, 0.044715, 1.0, ALU.mult, ALU.add)
            kb.tt("pool", gt, gt, gx, ALU.mult)
            kb.act(gt, gt, AF.Sigmoid, scale=1.5957691216057308)
            kb.tt("pool", gt, gt, gx, ALU.mult)
            hsrc = own_h.v(own_h.t[:, m, t0:t0 + n]) if G < 2 else hsb.v(hsb.t[:, m, :])
            kb.tt("pool", go, gt, hsrc, ALU.mult)
            kb.dma("sp", scr_cat.v(scr_cat.t[:, 8 + m, t0:t0 + n]), go)
    kb.dma("sp", scr_g[:], gates_sb.v(gates_sb.t[0:32, 8, :]))
    kb.phase_end()
    kb.phase_end()
    stage_gate(3)

    kb.phase_begin()
    iota_p = kb.sb("iota_p", [128, 4096], F32)
    kb.dma("sp", iota_p[:], iota_in.v(iota_in.t[0:1, :].partition_broadcast(128)))
    iota_ce = kb.sb("iota_ce_sb", [128, 128], F32)
    kb.dma("sp", iota_ce[:], iotace_in.v(iotace_in.t[0:1, :].partition_broadcast(128)))
    iota_blk = kb.sb("iota_blk_sb", [128, 64], F32)
    kb.dma("sp", iota_blk[:], iotablk_in.v(iotablk_in.t[0:1, :].partition_broadcast(128)))
    tpos = kb.sb("tpos_sb", [128, 8], F32)
    kb.dma("sp", tpos[:], tpos_in[:])
    curb = kb.sb("curb_sb", [128, 8], F32)
    kb.dma("sp", curb[:], curb_in[:])
    curm1 = kb.sb("curm1", [128, 8], F32)
    kb.ts("dve", curm1[:], curb[:], -1.0, None, ALU.add)
    negdw = kb.sb("negdw_sb", [128, 640], F32)
    kb.dma("sp", negdw[:], negdw_in[:])
    bandw = kb.sb("bandw_sb", [128, 640], F32)
    kb.dma("sp", bandw[:], bandw_in[:])
    kselT = kb.sb("kselT", [128, 2, SEQ], BF16)
    vsel = kb.sb("vsel", [128, 32, 2, 129], BF16)
    kwin_loc = kb.sb("kwin_loc", [128, 2, 1536], BF16)
    vwin_loc = kb.sb("vwin_loc", [128, 12, 2, 129], BF16)
    kb.memset("pool", vsel[:], 1.0)
    kb.memset("pool", vwin_loc[:], 1.0)
    kb.memset("pool", vwin_loc.v(vwin_loc.t[:, :, :, 0:128]), 0.0)
    kb.memset("pool", kwin_loc[:], 0.0)
    for g in range(2):
        kb.dma("sp", kselT.v(kselT.t[:, g, :]), scr_kT.v(scr_kT.t[0, g]))
    for t in range(32):
        kb.dma("sp", vsel.v(vsel.t[:, t, :, 0:128]), scr_v.v(scr_v.t[0, t]))
    negd = kb.sb("negd", [128, 4096], F32)
    Sb = kb.sb("Sb", [128, 4096], F32)
    kwfull = T(Sb.t[:].bitcast(BF16), "kwfull")
    kwfull.buf = Sb.buf
    vwfull = T(negd.t[:].bitcast(BF16), "vwfull")
    vwfull.buf = negd.buf
    for g in range(2):
        kb.dma("sp", kwfull.v(kwfull.t[:, g * 4096:(g + 1) * 4096]), scr_kT.v(scr_kT.t[1, g]))
    for q in range(4):
        lo = 8 * q - 4
        a = max(lo, 0)
        for g in range(2):
            dst = kwin_loc.v(kwin_loc.t[:, g, (a - lo) * 128:1536])
            kb.stt("dve", dst, kwfull.v(kwfull.t[:, g * 4096 + a * 128:g * 4096 + (lo + 12) * 128]), onehot.v(onehot.t[:, q:q + 1]), dst, ALU.mult, ALU.add)
    vw3 = vwfull.t[:, 0:8192].rearrange("p (t g d) -> p t g d", t=32, g=2)
    for t in range(32):
        kb.dma("sp", vwfull.v(vw3[:, t]), scr_v.v(scr_v.t[1, t]))
    for q in range(4):
        lo = 8 * q - 4
        a = max(lo, 0)
        dst = vwin_loc.v(vwin_loc.t[:, a - lo:12, :, 0:128])
        kb.stt("dve", dst, vwfull.v(vw3[:, a:lo + 12]), onehot.v(onehot.t[:, q:q + 1]), dst, ALU.mult, ALU.add)
    maskg = kb.sb("maskg", [128, 4096], BF16)
    Eb = kb.sb("Eb", [128, 4096], BF16)
    PT = kb.sb("PT", [128, 8, 128], BF16)
    Sb_b = kb.sb("Sb_b", [128, 4096], F32)
    Eb_b = kb.sb("Eb_b", [128, 4096], BF16)
    PT_b = kb.sb("PT_b", [128, 8, 128], BF16)
    Sb2, Eb2, PT2 = [Sb, Sb_b], [Eb, Eb_b], [PT, PT_b]
    qs = kb.sb("qs", [128, 8, 128], BF16)
    negdc = kb.sb("negdc", [128, 128], F32)
    validc = kb.sb("validc", [128, 128], F32)
    Sc = kb.sb("Sc", [128, 4, 128], F32)
    pcb = kb.sb("pcb", [128, 4, 128], BF16)
    mc = kb.sb("mc", [128, 4], F32)
    sc = kb.sb("sc", [128, 4], F32)
    imp = kb.sb("imp", [128, 64], F32)
    imp2 = kb.sb("imp2", [128, 64], F32)
    fm = kb.sb("fm", [128, 64], F32)
    validb = kb.sb("validb", [128, 64], F32)
    selb = kb.sb("selb", [128, 64], BF16)
    mx8 = kb.sb("mx8", [128, 8], F32)
    thr = kb.sb("thr", [128, 1], F32)
    m1_2 = [kb.sb("m1_%d" % i, [128, 1], F32) for i in range(4)]
    coef_2 = [kb.sb("coef_%d" % i, [128, 1], F32) for i in range(4)]
    Sw2 = [kb.sb("Sw%d" % i, [128, 640], F32) for i in range(2)]
    Ew2 = [kb.sb("Ew%d" % i, [128, 640], BF16) for i in range(2)]
    PTw2 = [kb.sb("PTw%d" % i, [128, 8, 128], BF16) for i in range(2)]
    wm = kb.sb("wm", [128, 640], BF16)
    wtmp = kb.sb("wtmp", [128, 640], F32)
    attn_tm = kb.sb("attn_tm", [128, 1024], F32)
    attn_bf = kb.sb("attn_bf", [128, 1024], BF16)
    aT = kb.sb("aT", [128, 8, 128], BF16)

    def softmax_A(S_t, width, E_t, mask_v, m1):
        kb.op("dve", lambda e: e.tensor_reduce(out=m1.t[:], in_=S_t.t[:, 0:width], axis=AX.X, op=ALU.max), [S_t[:]], [m1[:]])
        kb.ts("dve", m1[:], m1[:], -1.0, None, ALU.mult)
        kb.act(E_t.v(E_t.t[:, 0:width]), S_t.v(S_t.t[:, 0:width]), AF.Exp, bias=m1[:], scale=1.0)
        kb.tt("pool", E_t.v(E_t.t[:, 0:width]), E_t.v(E_t.t[:, 0:width]), mask_v, ALU.mult)

    def softmax_B(width, E_t, ntiles, vsrc, po, h, gate_col, s, first, PT, coef, tb):
        for rr in range((ntiles + 7) // 8):
            cnt = min(8, ntiles - rr * 8)
            pt = psb[tb + rr % 2]
            ptv = bview(pt)
            for i in range(cnt):
                kt = rr * 8 + i
                kb.tr(pt.v(ptv[:, i * 128:(i + 1) * 128]), E_t.v(E_t.t[:, kt * 128:(kt + 1) * 128]), ident_b[:], signal=(i == cnt - 1))
            kb.copy("act", PT.v(PT.t[:, 0:cnt, :]), pt.v(ptv[:, 0:cnt * 128].rearrange("p (a b) -> p a b", a=cnt)))
            for i in range(cnt):
                kt = rr * 8 + i
                kb.mm(po.v(po.t[:, 0:129]), PT.v(PT.t[:, i, :]), vsrc(kt), start=(kt == 0), stop=(kt == ntiles - 1),
                      signal=(kt == ntiles - 1))
        kb.ts("dve", coef[:], po.v(po.t[:, 128:129]), 1e-30, None, ALU.max)
        kb.op("dve", lambda e: e.reciprocal(out=coef.t[:], in_=coef.t[:]), [coef[:]], [coef[:]])
        kb.tt("dve", coef[:], coef[:], gates_sb.v(gates_sb.t[:, s, gate_col:gate_col + 1]), ALU.mult)
        dst = attn_tm.v(attn_tm.t[:, h * 128:(h + 1) * 128])
        if first:
            kb.ts("dve", dst, po.v(po.t[:, 0:128]), coef.v(coef.t[:, 0:1]), None, ALU.mult)
        else:
            kb.stt("dve", dst, po.v(po.t[:, 0:128]), coef.v(coef.t[:, 0:1]), dst, ALU.mult, ALU.add)

    for s in range(8):
        tq = tpos.v(tpos.t[:, s:s + 1])
        kb.dma("sp", qs[:], scr_q.v(scr_q.t[:, :, s * 128:(s + 1) * 128]))
        kb.ts("dve", negd[:], iota_p[:], tq, 0.0, ALU.subtract, ALU.min)
        kb.ts("dve", negdc[:], iota_ce[:], tq, 0.0, ALU.subtract, ALU.min)
        kb.ts("dve", validc[:], iota_ce[:], tq, None, ALU.is_le)
        kb.ts("dve", wtmp[:], iota_p.v(iota_p.t[:, 0:640]), float(512 - 128 * s), onehot.v(onehot.t[:, 0:1]), ALU.is_lt, ALU.mult)
        kb.ts("dve", wtmp[:], wtmp[:], -1.0, 1.0, ALU.mult, ALU.add)
        kb.tt("pool", wm[:], bandw[:], wtmp[:], ALU.mult)
        for g in range(2):
            pc = psb[0]
            for r in range(4):
                kb.mm(pc.v(pc.t[:, r * 128:(r + 1) * 128]), qs.v(qs.t[:, 4 * g + r, :]), kcmpT_sb.v(kcmpT_sb.t[:, g, :]), signal=(r == 3))
            for r in range(4):
                kb.stt("dve", Sc.v(Sc.t[:, r, :]), negdc[:], SLOPES[4 * g + r], pc.v(pc.t[:, r * 128:(r + 1) * 128]), ALU.mult, ALU.add)
            kb.op("dve", lambda e: e.tensor_reduce(out=mc.t[:], in_=Sc.t[:], axis=AX.X, op=ALU.max), [Sc[:]], [mc[:]])
            kb.ts("dve", mc[:], mc[:], -1.0, None, ALU.mult)
            for r in range(4):
                kb.act(Sc.v(Sc.t[:, r, :]), Sc.v(Sc.t[:, r, :]), AF.Exp, bias=mc.v(mc.t[:, r:r + 1]), scale=1.0)
            kb.tt("pool", Sc[:], Sc[:], validc.v(validc.t[:, :].unsqueeze(1).to_broadcast([128, 4, 128])), ALU.mult)
            kb.op("dve", lambda e: e.tensor_reduce(out=sc.t[:], in_=Sc.t[:], axis=AX.X, op=ALU.add), [Sc[:]], [sc[:]])
            kb.ts("dve", sc[:], sc[:], 1e-30, None, ALU.max)
            kb.op("dve", lambda e: e.reciprocal(out=sc.t[:], in_=sc.t[:]), [sc[:]], [sc[:]])
            kb.tt("dve", Sc[:], Sc[:], sc.v(sc.t[:, :].unsqueeze(2).to_broadcast([128, 4, 128])), ALU.mult)
            kb.op("dve", lambda e: e.tensor_reduce(out=imp.t[:], in_=Sc.t[:].rearrange("p h (j two) -> p j h two", two=2),
                                                    axis=AX.XY, op=ALU.add), [Sc[:]], [imp[:]])
            cur = curb.v(curb.t[:, s:s + 1])
            kb.ts("dve", fm[:], iota_blk[:], cur, None, ALU.is_equal)
            kb.ts("dve", imp2[:], iota_blk[:], curm1.v(curm1.t[:, s:s + 1]), None, ALU.is_equal)
            kb.tt("dve", fm[:], fm[:], imp2[:], ALU.max)
            kb.ts("dve", imp2[:], iota_blk[:], 0.0, None, ALU.is_equal)
            kb.tt("dve", fm[:], fm[:], imp2[:], ALU.max)
            kb.stt("dve", imp[:], fm[:], 1e4, imp[:], ALU.mult, ALU.max)
            kb.ts("dve", validb[:], iota_blk[:], cur, None, ALU.is_le)
            kb.tt("dve", imp[:], imp[:], validb[:], ALU.mult)
            kb.stt("dve", imp[:], validb[:], -1.0, imp[:], ALU.add, ALU.add)
            kb.op("dve", lambda e: e.max(out=mx8.t[:], in_=imp.t[:]), [imp[:]], [mx8[:]])
            kb.op("dve", lambda e: e.match_replace(out=imp2.t[:], in_to_replace=mx8.t[:], in_values=imp.t[:], imm_value=-2.0),
                  [mx8[:], imp[:]], [imp2[:]])
            kb.op("dve", lambda e: e.max(out=mx8.t[:], in_=imp2.t[:]), [imp2[:]], [mx8[:]])
            kb.op("dve", lambda e: e.tensor_reduce(out=thr.t[:], in_=mx8.t[:], axis=AX.X, op=ALU.min), [mx8[:]], [thr[:]])
            kb.ts("dve", imp2[:], imp[:], thr.v(thr.t[:, 0:1]), None, ALU.is_ge)
            kb.tt("dve", selb[:], imp2[:], validb[:], ALU.mult)
            kb.stt("dve", maskg.v(maskg.t[:].rearrange("p (b k) -> p b k", k=64)), iota_p.v(iota_p.t[:].rearrange("p (b k) -> p b k", k=64)),
                   tq, selb.v(selb.t[:, :].unsqueeze(2).to_broadcast([128, 64, 64])), ALU.is_le, ALU.mult)
            kb.copy("pool", pcb[:], Sc[:])
            pt = psb[1]
            ptv = bview(pt)
            for r in range(4):
                kb.tr(pt.v(ptv[:, r * 128:(r + 1) * 128]), pcb.v(pcb.t[:, r, :]), ident_b[:], signal=(r == 3))
            kb.copy("act", PT.v(PT.t[:, 0:4, :]), pt.v(ptv[:, 0:512].rearrange("p (a b) -> p a b", a=4)))
            po = psb[0]
            for r in range(4):
                kb.mm(po.v(po.t[:, r * 128:(r + 1) * 128]), PT.v(PT.t[:, r, :]), vcmp_sb.v(vcmp_sb.t[:, g, :]), signal=(r == 3))
            for r in range(4):
                h = 4 * g + r
                kb.ts("dve", attn_tm.v(attn_tm.t[:, h * 128:(h + 1) * 128]), po.v(po.t[:, r * 128:(r + 1) * 128]),
                      gates_sb.v(gates_sb.t[:, s, 3 * h:3 * h + 1]), None, ALU.mult)
            def mk(r, g=g, s=s):
                h = 4 * g + r
                par = r % 2
                Sbx, Sw, Ew = Sb2[par], Sw2[par], Ew2[par]

                def A_sel():
                    for c4 in range(8):
                        ps = psb[2 + c4 % 2]
                        kb.mm(ps[:], qs.v(qs.t[:, h, :]), kselT.v(kselT.t[:, g, c4 * 512:(c4 + 1) * 512]))
                        kb.stt("dve", Sbx.v(Sbx.t[:, c4 * 512:(c4 + 1) * 512]), negd.v(negd.t[:, c4 * 512:(c4 + 1) * 512]), SLOPES[h], ps[:], ALU.mult, ALU.add)
                    softmax_A(Sbx, 4096, Eb2[par], maskg[:], m1_2[par])

                def B_sel():
                    softmax_B(4096, Eb2[par], 32, lambda kt: vsel.v(vsel.t[:, kt, g, :]), psb[6] if par == 0 else psb[0], h, 3 * h + 1, s, False,
                              PT2[par], coef_2[par], 4)

                def A_win():
                    kb.mm(psb[2][:], qs.v(qs.t[:, h, :]), kwin_loc.v(kwin_loc.t[:, g, s * 128:s * 128 + 512]))
                    kb.mm(psb[3].v(psb[3].t[:, 0:128]), qs.v(qs.t[:, h, :]), kwin_loc.v(kwin_loc.t[:, g, s * 128 + 512:s * 128 + 640]))
                    kb.stt("dve", Sw.v(Sw.t[:, 0:512]), negdw.v(negdw.t[:, 0:512]), SLOPES[h], psb[2][:], ALU.mult, ALU.add)
                    kb.stt("dve", Sw.v(Sw.t[:, 512:640]), negdw.v(negdw.t[:, 512:640]), SLOPES[h], psb[3].v(psb[3].t[:, 0:128]), ALU.mult, ALU.add)
                    softmax_A(Sw, 640, Ew, wm[:], m1_2[2 + par])

                def B_win():
                    softmax_B(640, Ew, 5, lambda kt: vwin_loc.v(vwin_loc.t[:, s + kt, g, :]), psb[7] if par == 0 else psb[1], h, 3 * h + 2, s, False,
                              PTw2[par], coef_2[2 + par], 4)
                return A_sel, B_sel, A_win, B_win
            jobs = [mk(r) for r in range(4)]
            jobs[0][0]()
            jobs[0][2]()
            for r in range(4):
                if r + 1 < 4:
                    jobs[r + 1][0]()
                jobs[r][1]()
                if r + 1 < 4:
                    jobs[r + 1][2]()
                jobs[r][3]()
        kb.copy("pool", attn_bf[:], attn_tm[:])
        pt = psb[1]
        ptv = bview(pt)
        for h in range(8):
            kb.tr(pt.v(ptv[:, h * 128:(h + 1) * 128]), attn_bf.v(attn_bf.t[:, h * 128:(h + 1) * 128]), ident_b[:], signal=(h == 7))
        kb.copy("act", aT[:], pt.v(ptv[:, 0:1024].rearrange("p (a b) -> p a b", a=8)))
        kb.dma("sp", scr_cat.v(scr_cat.t[:, 0:8, s * 128:(s + 1) * 128]), aT[:])
    kb.phase_end()
    stage_gate(35)

    kb.phase_begin()
    ptb = kb.sb("ptb", [128, 512], I32)
    kb.dma("sp", ptb[:], pt_in.v(pt_in.t[0:1, :].partition_broadcast(128)))
    pidx = kb.sb("pidx_sb", [128, 1], F32)
    kb.dma("sp", pidx[:], pidx_in[:])
    ptf = kb.sb("ptf", [128, 512], F32)
    kb.copy("dve", ptf[:], ptb[:])
    kb.ts("dve", ptf[:], ptf[:], 128.0, pidx.v(pidx.t[:, 0:1]), ALU.mult, ALU.add)
    pidx_i = kb.sb("pidx_i", [128, 512], I32)
    kb.copy("dve", pidx_i[:], ptf[:])
    cbs = kb.sb("cbs", [128, 2, 512], F32)
    kb.dma("sp", cbs[:], cbs_in[:])
    slrow = kb.sb("slrow", [128, 2], F32)
    kb.dma("sp", slrow[:], slrow_in[:])
    stok = kb.sb("stok_sb", [128, 2], F32)
    kb.dma("sp", stok[:], stok_in[:])
    kb.eng["pool"].wait(pidx_i.buf.w)
    wbs = kb.sb("wbs", [128, 2, 520], F32)
    kb.dma("sp", wbs[:], wbs_in[:])
    wms = kb.sb("wms", [128, 520], F32)
    kb.dma("sp", wms[:], wms_in[:])
    nbs = kb.sb("nbs", [128, 2, 8], F32)
    kb.dma("sp", nbs[:], nbs_in[:])
    nms = kb.sb("nms", [128, 8], F32)
    kb.dma("sp", nms[:], nms_in[:])
    rsum_f = kb.sb("rsum_f", [128, 128], F32)
    kb.dma("sp", rsum_f[:], rsum_in[:])
    rsum_b = kb.sb("rsum_b", [128, 128], BF16)
    kb.copy("pool", rsum_b[:], rsum_f[:])
    forced = kb.sb("forced", [128, 257], F32)
    kb.dma("sp", forced[:], forced_in.v(forced_in.t[0:1, :].partition_broadcast(128)))
    iota512 = kb.sb("iota512_sb", [128, 512], F32)
    kb.dma("sp", iota512[:], iota512_in.v(iota512_in.t[0:1, :].partition_broadcast(128)))
    poolk2 = kb.sb("poolk2", [128, 4], BF16)
    poolv2 = kb.sb("poolv2", [128, 252], BF16)
    kb.dma("pool", poolk2[:], poolk_in[:])
    kb.dma("pool", poolv2[:], poolv_in[:])
    qss = kb.sb("qss", [128, 2, 4, 32], BF16)
    for g in range(2):
        for b in range(4):
            kb.dma("sp", qss.v(qss.t[:, g, b, :].rearrange("p (r t) -> p r t", r=4)),
                   scr_q.v(scr_q.t[:, 4 * g:4 * g + 4, 1024 + 8 * b:1024 + 8 * b + 8]))
    grow = kb.sb("grow", [128, 2, 3], F32)
    for g in range(2):
        for b in range(4):
            for r in range(4):
                kb.dma("sp", grow.v(grow.t[32 * b + 8 * r:32 * b + 8 * r + 8, g, :]),
                       scr_g.v(scr_g.t[8 * b:8 * b + 8, (4 * g + r) * 3:(4 * g + r) * 3 + 3]))
    attn_s = kb.sb("attn_s", [128, 2, 128], F32)
    pg = [kb.sb("pg%d" % i, [128, 4, 512], F32) for i in range(6)]
    pgk2 = [kb.sb("pgk%d" % i, [128, 4, 2, 128], BF16) for i in range(2)]
    pgv2 = [kb.sb("pgv%d" % i, [128, 1, 2, 129], BF16) for i in range(2)]
    pgk = pgk2[0]
    for i in range(2):
        kb.memset("pool", pgv2[i][:], 1.0)
    vwS = kb.sb("vwS", [128, 4, 4, 2, 129], BF16)
    kb.memset("pool", vwS[:], 1.0)
    vsS2 = [kb.sb("vsS%d" % i, [128, 4, 4, 2, 129], BF16) for i in range(2)]
    for i in range(2):
        kb.memset("pool", vsS2[i][:], 1.0)
    kTs2 = [kb.sb("kTs%d" % i, [128, 8, 128], BF16) for i in range(2)]
    kTs = kTs2[0]
    kcmpT_s = kb.sb("kcmpT_s", [128, 4, 2, 512], BF16)
    vcmp_s = kb.sb("vcmp_s", [128, 4, 4, 2, 128], BF16)
    S_sb2 = [kb.sb("S_sb%d" % i, [128, 520], F32) for i in range(2)]
    E_sb2 = [kb.sb("E_sb%d" % i, [128, 520], BF16) for i in range(2)]
    PTs2 = [kb.sb("PTs%d" % i, [128, 5, 128], BF16) for i in range(2)]
    S_sb, E_sb, PTs = S_sb2[0], E_sb2[0], PTs2[0]
    mask_c2 = [kb.sb("mask_c%d" % i, [128, 512], BF16) for i in range(2)]
    p_hi = kb.sb("p_hi", [128, 512], BF16)
    p_lo = kb.sb("p_lo", [128, 512], BF16)
    imps = kb.sb("imps", [128, 264], F32)
    imps2 = kb.sb("imps2", [128, 264], F32)
    sels = [kb.sb("sels%d" % g, [128, 264], BF16) for g in range(2)]
    mrun = [kb.sb("mrun%d" % g, [128, 1], F32) for g in range(2)]
    oacc = [kb.sb("oacc%d" % g, [128, 129], F32) for g in range(2)]
    mc_ = kb.sb("mc_", [128, 1], F32)
    mn_ = kb.sb("mn_", [128, 1], F32)
    al_ = kb.sb("al_", [128, 1], F32)
    kc_ = kb.sb("kc_", [128, 1], F32)
    bi_ = kb.sb("bi_", [128, 1], F32)
    cf_ = kb.sb("cf_", [128, 1], F32)
    mx8s = kb.sb("mx8s", [128, 8], F32)
    thrs = kb.sb("thrs", [128, 1], F32)
    sm_ = kb.sb("sm_", [128, 1], F32)
    mask_c = kb.sb("mask_c", [128, 512], BF16)

    def gather_pages(buf, cache, lp):
        for b in range(4):
            col = b * 128 + lp
            kb.dma("pool", buf.v(buf.t[:, b, :]), cache[:, :],
                   fn=lambda e, b=b, col=col: e.indirect_dma_start(out=buf.t[:, b, :], out_offset=None, in_=cache.t[:, :],
                                                                    in_offset=bass.IndirectOffsetOnAxis(ap=pidx_i.t[:, col:col + 1], axis=0)))

    for b in range(4):
        for bank in (4, 5, 6, 7):
            kb.memset("dve", psb[bank][:], 0.0)
        for lp in range(128):
            buf = pg[lp % 6]
            col = b * 128 + lp
            kb.dma("pool", buf.v(buf.t[:, 0, :]), cache_c[:, :],
                   fn=lambda e, buf=buf, col=col: e.indirect_dma_start(out=buf.t[:, 0, :], out_offset=None, in_=cache_c.t[:, :],
                                                                        in_offset=bass.IndirectOffsetOnAxis(ap=pidx_i.t[:, col:col + 1], axis=0)))
            src4 = buf.t[:, 0, :].rearrange("p (g c d) -> p g c d", g=2, c=2)
            pgk, pgv = pgk2[lp % 2], pgv2[lp % 2]
            kb.copy("act", pgk.v(pgk.t[:, 0, :, :]), buf.v(src4[:, :, 0, :]))
            kb.copy("dve", pgv.v(pgv.t[:, 0, :, 0:128]), buf.v(src4[:, :, 1, :]))
            for g in range(2):
                pk = psb[4 + g]
                kb.mm(pk.v(pk.t[:, 4 * lp:4 * lp + 4]), pgk.v(pgk.t[:, 0, g, :]), poolk2[:], start=False, stop=False, signal=False)
            tl, off = lp // 32, lp % 32
            pv = psb[6 + tl // 2]
            kb.mm(pv.v(pv.t[:, (tl % 2) * 256:(tl % 2) * 256 + 256].rearrange("p (g d) -> p g d", g=2)),
                  poolv2.v(poolv2.t[:, 124 - 4 * off:252 - 4 * off]), pgv.v(pgv.t[:, 0, :, 0:128]), start=False, stop=False, signal=True)
        for g in range(2):
            kb.copy("act", kcmpT_s.v(kcmpT_s.t[:, b, g, :]), psb[4 + g][:])
        for tl in range(4):
            pv = psb[6 + tl // 2]
            kb.copy("dve", vcmp_s.v(vcmp_s.t[:, b, tl, :, :]), pv.v(pv.t[:, (tl % 2) * 256:(tl % 2) * 256 + 256].rearrange("p (g d) -> p g d", g=2)))

    def exact_softmax(S_ps_list, width, bias_v, mask_v):
        c0 = 0
        for (ps_v, w) in S_ps_list:
            kb.tt("dve", S_sb.v(S_sb.t[:, c0:c0 + w]), ps_v, V(bias_v.ap[:, c0:c0 + w], bias_v.buf), ALU.add)
            c0 += w
        kb.op("dve", lambda e: e.tensor_reduce(out=mc_.t[:], in_=S_sb.t[:, 0:width], axis=AX.X, op=ALU.max), [S_sb[:]], [mc_[:]])
        kb.ts("dve", mc_[:], mc_[:], -1.0, None, ALU.mult)
        kb.act(E_sb.v(E_sb.t[:, 0:width]), S_sb.v(S_sb.t[:, 0:width]), AF.Exp, bias=mc_[:], scale=1.0)
        if mask_v is not None:
            kb.tt("dve", E_sb.v(E_sb.t[:, 0:width]), E_sb.v(E_sb.t[:, 0:width]), mask_v, ALU.mult)

    def transposeE(ntiles, width, E_sb=E_sb, PTs=PTs):
        pt = psb[4]
        ptv = bview(pt)
        for i in range(ntiles):
            w = min(128, width - i * 128)
            kb.tr(pt.v(ptv[0:w, i * 128:(i + 1) * 128]), E_sb.v(E_sb.t[:, i * 128:i * 128 + w]), ident_b[:], signal=(i == ntiles - 1))
        kb.copy("act", PTs.v(PTs.t[:, 0:ntiles, :]), pt.v(ptv[:, 0:ntiles * 128].rearrange("p (a b) -> p a b", a=ntiles)))

    kvn = kb.sb("kvn", [8, 4, 3 * KVW], BF16)
    for b in range(4):
        kb.dma("sp", kvn.v(kvn.t[:, b, :]), scr_kvs.v(scr_kvs.t[8 * b:8 * b + 8, :]))
    vnew = kb.sb("vnew", [8, 2, 4, 2, 129], BF16)
    kb.memset("pool", vnew[:], 1.0)
    knT = kb.sb("knT", [128, 2, 4, 2, 8], BF16)
    for br in range(2):
        base = 512 * (br + 1)
        for b in range(4):
            src4 = kvn.t[:, b, base:base + 512].rearrange("p (g c d) -> p g c d", g=2, c=2)
            kb.copy("pool", vnew.v(vnew.t[:, br, b, :, 0:128]), kvn.v(src4[:, :, 1, :]))
            pt = psb[3]
            ptv = bview(pt)
            for g in range(2):
                kb.tr(pt.v(ptv[:, g * 8:g * 8 + 8]), kvn.v(kvn.t[:, b, base + g * 256:base + g * 256 + 128]), ident_b.v(ident_b.t[0:8, 0:8]), signal=(g == 1))
            kb.copy("act", knT.v(knT.t[:, br, b, :, :]), pt.v(ptv[:, 0:16].rearrange("p (g k) -> p g k", g=2)))

    for g in range(2):
        pc = psb[0]
        for b in range(4):
            kb.mm(pc.v(pc.t[32 * b:32 * b + 32, :]), qss.v(qss.t[:, g, b, :]), kcmpT_s.v(kcmpT_s.t[:, b, g, :]), signal=(b == 3))
        exact_softmax([(pc[:], 512)], 512, cbs.v(cbs.t[:, g, :]), None)
        kb.op("dve", lambda e: e.tensor_reduce(out=sm_.t[:], in_=E_sb.t[:, 0:512], axis=AX.X, op=ALU.add), [E_sb[:]], [sm_[:]])
        kb.op("dve", lambda e: e.reciprocal(out=sm_.t[:], in_=sm_.t[:]), [sm_[:]], [sm_[:]])
        kb.act(S_sb.v(S_sb.t[:, 0:512]), S_sb.v(S_sb.t[:, 0:512]), AF.Exp, bias=mc_[:], scale=1.0)
        kb.ts("dve", S_sb.v(S_sb.t[:, 0:512]), S_sb.v(S_sb.t[:, 0:512]), sm_.v(sm_.t[:, 0:1]), None, ALU.mult)
        kb.ts("dve", E_sb.v(E_sb.t[:, 0:512]), E_sb.v(E_sb.t[:, 0:512]), sm_.v(sm_.t[:, 0:1]), None, ALU.mult)
        kb.copy("pool", p_hi[:], S_sb.v(S_sb.t[:, 0:512]))
        kb.tt("pool", p_lo[:], S_sb.v(S_sb.t[:, 0:512]), p_hi[:], ALU.subtract)
        pi = psb[1]
        kb.mm(pi[:], rsum_b[:], p_hi[:], start=True, stop=False, signal=False)
        kb.mm(pi[:], rsum_b[:], p_lo[:], start=False, stop=True, signal=True)
        kb.memset("pool", imps[:], 0.0)
        kb.copy("act", S_sb.v(S_sb.t[:, 0:512]), pi[:])
        kb.tt("dve", imps.v(imps.t[:, 0:256]), S_sb.v(S_sb.t[:, 0:512].rearrange("p (j two) -> p j two", two=2)[:, :, 0]),
              S_sb.v(S_sb.t[:, 0:512].rearrange("p (j two) -> p j two", two=2)[:, :, 1]), ALU.add)
        kb.stt("dve", imps.v(imps.t[:, 0:257]), forced[:], 1e4, imps.v(imps.t[:, 0:257]), ALU.mult, ALU.max)
        kb.memset("pool", imps.v(imps.t[:, 257:264]), -1.0)
        kb.op("dve", lambda e: e.max(out=mx8s.t[:], in_=imps.t[:]), [imps[:]], [mx8s[:]])
        kb.op("dve", lambda e: e.match_replace(out=imps2.t[:], in_to_replace=mx8s.t[:], in_values=imps.t[:], imm_value=-2.0),
              [mx8s[:], imps[:]], [imps2[:]])
        kb.op("dve", lambda e: e.max(out=mx8s.t[:], in_=imps2.t[:]), [imps2[:]], [mx8s[:]])
        kb.op("dve", lambda e: e.tensor_reduce(out=thrs.t[:], in_=mx8s.t[:], axis=AX.X, op=ALU.min), [mx8s[:]], [thrs[:]])
        kb.ts("dve", sels[g][:], imps[:], thrs.v(thrs.t[:, 0:1]), None, ALU.is_ge)
        transposeE(4, 512)
        po = psb[5]
        for b in range(4):
            for tl in range(4):
                kb.mm(po.v(po.t[32 * b:32 * b + 32, 0:128]), PTs.v(PTs.t[:, tl, 32 * b:32 * b + 32]), vcmp_s.v(vcmp_s.t[:, b, tl, g, :]),
                      start=(tl == 0), stop=(tl == 3), signal=(b == 3 and tl == 3))
        kb.ts("dve", attn_s.v(attn_s.t[:, g, :]), po.v(po.t[:, 0:128]), grow.v(grow.t[:, g, 0:1]), None, ALU.mult)
        for tl in range(4):
            for b in range(4):
                kb.dma("sp", pg[0].v(pg[0].t[:, b, :]), cwin_in.v(cwin_in.t[b, tl * 128:(tl + 1) * 128, :]))
            src5 = pg[0].t[:].rearrange("p b (g c d) -> p b g c d", g=2, c=2)
            kb.copy("act", pgk[:], pg[0].v(src5[:, :, :, 0, :]))
            kb.copy("dve", vwS.v(vwS.t[:, tl, :, :, 0:128]), pg[0].v(src5[:, :, :, 1, :]))
            pt = psb[2]
            ptv = bview(pt)
            for b in range(4):
                kb.tr(pt.v(ptv[:, b * 128:(b + 1) * 128]), pgk.v(pgk.t[:, b, g, :]), ident_b[:], signal=(b == 3))
            kb.copy("act", kTs.v(kTs.t[:, 0:4, :]), pt.v(ptv[:, 0:512].rearrange("p (a b) -> p a b", a=4)))
            pw = psb[0]
            for b in range(4):
                kb.mm(pw.v(pw.t[32 * b:32 * b + 32, tl * 128:(tl + 1) * 128]), qss.v(qss.t[:, g, b, :]), kTs.v(kTs.t[:, b, :]), signal=(b == 3))
        pn = psb[1]
        for b in range(4):
            kb.mm(pn.v(pn.t[32 * b:32 * b + 32, 0:8]), qss.v(qss.t[:, g, b, :]), knT.v(knT.t[:, 1, b, g, :]), signal=(b == 3))
        exact_softmax([(psb[0][:], 512), (pn.v(pn.t[:, 0:8]), 8)], 520, wbs.v(wbs.t[:, g, :]), wms[:])
        transposeE(5, 520)
        po = psb[5]
        for b in range(4):
            for tl in range(5):
                if tl < 4:
                    rhs = vwS.v(vwS.t[:, tl, b, g, :])
                    lhs = PTs.v(PTs.t[:, tl, 32 * b:32 * b + 32])
                else:
                    rhs = vnew.v(vnew.t[:, 1, b, g, :])
                    lhs = PTs.v(PTs.t[0:8, 4, 32 * b:32 * b + 32])
                kb.mm(po.v(po.t[32 * b:32 * b + 32, 0:129]), lhs, rhs, start=(tl == 0), stop=(tl == 4), signal=(b == 3 and tl == 4))
        kb.ts("dve", cf_[:], po.v(po.t[:, 128:129]), 1e-30, None, ALU.max)
        kb.op("dve", lambda e: e.reciprocal(out=cf_.t[:], in_=cf_.t[:]), [cf_[:]], [cf_[:]])
        kb.tt("dve", cf_[:], cf_[:], grow.v(grow.t[:, g, 2:3]), ALU.mult)
        kb.stt("dve", attn_s.v(attn_s.t[:, g, :]), po.v(po.t[:, 0:128]), cf_.v(cf_.t[:, 0:1]), attn_s.v(attn_s.t[:, g, :]), ALU.mult, ALU.add)
        kb.memset("dve", mrun[g][:], -1e30)
        kb.memset("dve", oacc[g][:], 0.0)

    def online_update(g, ps_list, width, kc_val, mask_v, pv_fn, ntiles):
        S_sb, E_sb, PTs = S_sb2[g], E_sb2[g], PTs2[g]
        c0 = 0
        for (ps_v, w, add_v, is_iota) in ps_list:
            if is_iota:
                kb.stt("dve", S_sb.v(S_sb.t[:, c0:c0 + w]), V(add_v.ap[:, 0:w], add_v.buf), slrow.v(slrow.t[:, g:g + 1]), ps_v, ALU.mult, ALU.add)
            else:
                kb.tt("dve", S_sb.v(S_sb.t[:, c0:c0 + w]), ps_v, add_v, ALU.add)
            c0 += w
        kb.op("dve", lambda e: e.tensor_reduce(out=mc_.t[:], in_=S_sb.t[:, 0:width], axis=AX.X, op=ALU.max), [S_sb[:]], [mc_[:]])
        if kc_val is not None:
            kb.tt("dve", mc_[:], mc_[:], kc_val, ALU.add)
        kb.tt("dve", mn_[:], mc_[:], mrun[g][:], ALU.max)
        kb.tt("dve", al_[:], mrun[g][:], mn_[:], ALU.subtract)
        kb.act(al_[:], al_[:], AF.Exp)
        kb.copy("dve", mrun[g][:], mn_[:])
        if kc_val is not None:
            kb.tt("dve", bi_[:], kc_val, mn_[:], ALU.subtract)
        else:
            kb.ts("dve", bi_[:], mn_[:], -1.0, None, ALU.mult)
        kb.act(E_sb.v(E_sb.t[:, 0:width]), S_sb.v(S_sb.t[:, 0:width]), AF.Exp, bias=bi_[:], scale=1.0)
        kb.tt("dve", E_sb.v(E_sb.t[:, 0:width]), E_sb.v(E_sb.t[:, 0:width]), mask_v, ALU.mult)
        transposeE(ntiles, width, E_sb, PTs)
        po = psb[5]
        pv_fn(po, PTs)
        kb.stt("dve", oacc[g][:], oacc[g][:], al_.v(al_.t[:, 0:1]), po.v(po.t[:, 0:129]), ALU.mult, ALU.add)

    def prep_chunk(c):
        sb0 = 0 if c % 2 == 0 else 6
        for i in range(4):
            lp = 4 * c + i
            buf = pg[lp % 6]
            gather_pages(buf, cache_s, lp)
            src5 = buf.t[:].rearrange("p b (g c d) -> p b g c d", g=2, c=2)
            pgk, kTs, vsS = pgk2[lp % 2], kTs2[lp % 2], vsS2[c % 2]
            kb.copy("act", pgk[:], buf.v(src5[:, :, :, 0, :]))
            kb.copy("dve", vsS.v(vsS.t[:, i, :, :, 0:128]), buf.v(src5[:, :, :, 1, :]))
            for half in range(2):
                pt = psb[2 + half]
                ptv = bview(pt)
                for bb in range(2):
                    for g in range(2):
                        b = half * 2 + bb
                        kb.tr(pt.v(ptv[:, (bb * 2 + g) * 128:(bb * 2 + g + 1) * 128]), pgk.v(pgk.t[:, b, g, :]), ident_b[:], signal=(bb == 1 and g == 1))
                kb.copy("act" if half == 0 else "dve", kTs.v(kTs.t[:, half * 4:half * 4 + 4, :]), pt.v(ptv[:, 0:512].rearrange("p (a b) -> p a b", a=4)))
            for g in range(2):
                for b in range(4):
                    kb.mm(psb[sb0 + g].v(psb[sb0 + g].t[32 * b:32 * b + 32, i * 128:(i + 1) * 128]), qss.v(qss.t[:, g, b, :]), kTs.v(kTs.t[:, b * 2 + g, :]),
                          signal=(b == 3))

    def finish_chunk(c):
        sb0 = 0 if c % 2 == 0 else 6
        for g in range(2):
            mask_c = mask_c2[g]
            kb.ts("dve", kc_[:], slrow.v(slrow.t[:, g:g + 1]), float(512 * c - 16384), stok.v(stok.t[:, g:g + 1]), ALU.mult, ALU.add)
            kb.copy("dve", mask_c.v(mask_c.t[:].rearrange("p (b k) -> p b k", k=64)),
                    sels[g].v(sels[g].t[:, 8 * c:8 * c + 8].unsqueeze(2).to_broadcast([128, 8, 64])))

            def pv_fn(po, PTs, g=g, vsS=vsS2[c % 2]):
                for b in range(4):
                    for i in range(4):
                        kb.mm(po.v(po.t[32 * b:32 * b + 32, 0:129]), PTs.v(PTs.t[:, i, 32 * b:32 * b + 32]), vsS.v(vsS.t[:, i, b, g, :]),
                              start=(i == 0), stop=(i == 3), signal=(b == 3 and i == 3))
            online_update(g, [(psb[sb0 + g][:], 512, iota512[:], True)], 512, kc_[:], mask_c[:], pv_fn, 4)

    prep_chunk(0)
    for c in range(32):
        if c + 1 < 32:
            prep_chunk(c + 1)
        finish_chunk(c)
    for g in range(2):
        pn = psb[g]
        for b in range(4):
            kb.mm(pn.v(pn.t[32 * b:32 * b + 32, 0:8]), qss.v(qss.t[:, g, b, :]), knT.v(knT.t[:, 0, b, g, :]), signal=(b == 3))

        def pv_fn2(po, PTs, g=g):
            for b in range(4):
                kb.mm(po.v(po.t[32 * b:32 * b + 32, 0:129]), PTs.v(PTs.t[0:8, 0, 32 * b:32 * b + 32]), vnew.v(vnew.t[:, 0, b, g, :]),
                      start=True, stop=True, signal=(b == 3))
        online_update(g, [(pn.v(pn.t[:, 0:8]), 8, nbs.v(nbs.t[:, g, :]), False)], 8, None, nms[:], pv_fn2, 1)
        kb.ts("dve", cf_[:], oacc[g].v(oacc[g].t[:, 128:129]), 1e-30, None, ALU.max)
        kb.op("dve", lambda e: e.reciprocal(out=cf_.t[:], in_=cf_.t[:]), [cf_[:]], [cf_[:]])
        kb.tt("dve", cf_[:], cf_[:], grow.v(grow.t[:, g, 1:2]), ALU.mult)
        kb.stt("dve", attn_s.v(attn_s.t[:, g, :]), oacc[g].v(oacc[g].t[:, 0:128]), cf_.v(cf_.t[:, 0:1]), attn_s.v(attn_s.t[:, g, :]), ALU.mult, ALU.add)
    asb = kb.sb("asb", [128, 2, 128], BF16)
    kb.copy("pool", asb[:], attn_s[:])
    aTs = kb.sb("aTs", [128, 2, 128], BF16)
    pt = psb[2]
    ptv = bview(pt)
    for g in range(2):
        kb.tr(pt.v(ptv[:, g * 128:(g + 1) * 128]), asb.v(asb.t[:, g, :]), ident_b[:], signal=(g == 1))
    kb.copy("act", aTs[:], pt.v(ptv[:, 0:256].rearrange("p (a b) -> p a b", a=2)))
    for g in range(2):
        for b in range(4):
            for r in range(4):
                kb.dma("sp", scr_cat.v(scr_cat.t[:, 4 * g + r, 1024 + 8 * b:1024 + 8 * b + 8]),
                       aTs.v(aTs.t[:, g, 32 * b + 8 * r:32 * b + 8 * r + 8]))
    kb.phase_end()
    stage_gate(4)

    TILES = [(i * 128, 128) for i in range(8)] + [(1024, 32)]
    kb.phase_begin()
    acc = kb.sb("acc", [128, 9, D], F32)
    comb = kb.sb("comb", [128, 9, 16], F32)
    kb.phase_begin()
    catT = kb.sb("catT", [128, 16, NTOK], BF16)
    for k in range(16):
        kb.dma("sp", catT.v(catT.t[:, k, :]), scr_cat.v(scr_cat.t[:, k, :]))
    Wo = kb.sb("Wo", [128, 16, D], BF16)
    wo_view = w_out.t.rearrange("(k p) n -> p k n", p=128)
    for k in range(16):
        kb.dma("pool", Wo.v(Wo.t[:, k, :]), w_out.v(wo_view[:, k, :]))
    xs4 = kb.sb("xs4", [128, D], F32)
    for ti, (t0, rows) in enumerate(TILES):
        src = x_own.v(x_own.t[t0:t0 + rows, :]) if ti < 8 else x_s[:, :]
        kb.dma("sp", xs4.v(xs4.t[0:rows, :]), src)
        for cb in range(4):
            pb = psb[cb % 4]
            for k in range(16):
                kb.mm(pb.v(pb.t[0:rows, :]), catT.v(catT.t[:, k, t0:t0 + rows]), Wo.v(Wo.t[:, k, cb * 512:(cb + 1) * 512]),
                      start=(k == 0), stop=(k == 15), signal=(k == 15))
            kb.tt("dve", acc.v(acc.t[0:rows, ti, cb * 512:(cb + 1) * 512]), pb.v(pb.t[0:rows, :]), xs4.v(xs4.t[0:rows, cb * 512:(cb + 1) * 512]), ALU.add)
    kb.phase_end()
    stage_gate(5)
    kb.phase_begin()
    hnT = kb.sb("hnT", [128, 16, NTOK], BF16)
    kb.phase_begin()
    gffn = kb.sb("gffn", [128, D], F32)
    kb.dma("sp", gffn[:], norm_ffn.v(norm_ffn.t[0:1, :].partition_broadcast(128)))
    hn32 = kb.sb("hn32", [128, D], F32)
    junk = kb.sb("junk", [128, D], BF16)
    hn_hi = kb.sb("hn_hi", [128, D], BF16)
    hn_lo = kb.sb("hn_lo", [128, D], BF16)
    loT = kb.sb("loT", [128, 16, 128], BF16)
    wr = kb.sb("wr", [128, 16, 20], F32)
    wr_view = w_router.t.rearrange("(k p) n -> p k n", p=128)
    for k in range(16):
        kb.dma("sp", wr.v(wr.t[:, k, :]), w_router.v(wr_view[:, k, :]))
    whi = kb.sb("whi", [128, 16, 20], BF16)
    wlo = kb.sb("wlo", [128, 16, 20], BF16)
    kb.copy("pool", whi[:], wr[:])
    kb.tt("pool", wlo[:], wr[:], whi[:], ALU.subtract)
    br_t = kb.sb("br_t", [128, 20], F32)
    kb.dma("sp", br_t[:], b_router.v(b_router.t[0:1, :].partition_broadcast(128)))
    lg = kb.sb("lg", [128, 20], F32)
    gmax = kb.sb("gmax", [128, 1], F32)
    goh = kb.sb("goh", [128, 4], F32)
    gex = kb.sb("gex", [128, 4], F32)
    gsum = kb.sb("gsum", [128, 1], F32)
    el = kb.sb("el", [128, 4], F32)
    etmp = kb.sb("etmp", [128, 4, 4], F32)
    e1 = kb.sb("e1", [128, 1], F32)
    e2 = kb.sb("e2", [128, 1], F32)
    mk1 = kb.sb("mk1", [128, 4], F32)
    mk2 = kb.sb("mk2", [128, 4], F32)
    el2 = kb.sb("el2", [128, 4], F32)
    w1 = kb.sb("w1", [128, 1], F32)
    w2 = kb.sb("w2", [128, 1], F32)
    wv = kb.sb("wv", [128, 4], F32)
    for ti, (t0, rows) in enumerate(TILES):
        at = T(acc.t[:, ti, :], "acc_t")
        at.buf = acc.buf
        kb.act(junk.v(junk.t[0:rows, :]), at.v(at.t[0:rows, :]), AF.Square, accum=ssq.v(ssq.t[0:rows, :]))
        kb.act(rstd.v(rstd.t[0:rows, :]), ssq.v(ssq.t[0:rows, :]), AF.Sqrt, bias=eps_t.v(eps_t.t[0:rows, :]), scale=1.0 / D)
        kb.op("dve", lambda e: e.reciprocal(out=rstd.t[0:rows, :], in_=rstd.t[0:rows, :]), [rstd[:]], [rstd[:]])
        kb.stt("dve", hn32.v(hn32.t[0:rows, :]), at.v(at.t[0:rows, :]), rstd.v(rstd.t[0:rows, 0:1]), gffn.v(gffn.t[0:rows, :]), ALU.mult, ALU.mult)
        kb.copy("pool", hn_hi.v(hn_hi.t[0:rows, :]), hn32.v(hn32.t[0:rows, :]))
        kb.tt("pool", hn_lo.v(hn_lo.t[0:rows, :]), hn32.v(hn32.t[0:rows, :]), hn_hi.v(hn_hi.t[0:rows, :]), ALU.subtract)
        transpose16(hn_hi, rows, hnT, t0)
        transpose16(hn_lo, rows, loT, 0)
        pl = psb[4]
        for k in range(16):
            kb.mm(pl.v(pl.t[0:rows, 0:20]), hnT.v(hnT.t[:, k, t0:t0 + rows]), whi.v(whi.t[:, k, :]), start=(k == 0), stop=False, signal=False)
        for k in range(16):
            kb.mm(pl.v(pl.t[0:rows, 0:20]), hnT.v(hnT.t[:, k, t0:t0 + rows]), wlo.v(wlo.t[:, k, :]), start=False, stop=False, signal=False)
        for k in range(16):
            kb.mm(pl.v(pl.t[0:rows, 0:20]), loT.v(loT.t[:, k, 0:rows]), whi.v(whi.t[:, k, :]), start=False, stop=(k == 15), signal=(k == 15))
        R = slice(0, rows)
        kb.tt("dve", lg.v(lg.t[R, :]), pl.v(pl.t[R, 0:20]), br_t.v(br_t.t[R, :]), ALU.add)
        kb.op("dve", lambda e: e.tensor_reduce(out=gmax.t[R, :], in_=lg.t[R, 0:4], axis=AX.X, op=ALU.max), [lg[:]], [gmax[:]])
        kb.ts("dve", goh.v(goh.t[R, :]), lg.v(lg.t[R, 0:4]), gmax.v(gmax.t[R, 0:1]), None, ALU.is_ge)
        kb.ts("dve", gmax.v(gmax.t[R, :]), gmax.v(gmax.t[R, :]), -1.0, None, ALU.mult)
        kb.act(gex.v(gex.t[R, :]), lg.v(lg.t[R, 0:4]), AF.Exp, bias=gmax.v(gmax.t[R, 0:1]), scale=1.0)
        kb.op("dve", lambda e: e.tensor_reduce(out=gsum.t[R, :], in_=gex.t[R, :], axis=AX.X, op=ALU.add), [gex[:]], [gsum[:]])
        kb.op("dve", lambda e: e.reciprocal(out=gsum.t[R, :], in_=gsum.t[R, :]), [gsum[:]], [gsum[:]])
        kb.tt("dve", etmp.v(etmp.t[R]), lg.v(lg.t[R, 4:20].rearrange("p (g i) -> p g i", g=4)),
              goh.v(goh.t[R, :].unsqueeze(2).to_broadcast([rows, 4, 4])), ALU.mult)
        kb.op("dve", lambda e: e.tensor_reduce(out=el.t[R, :], in_=etmp.t[R].rearrange("p g i -> p i g"), axis=AX.X, op=ALU.add), [etmp[:]], [el[:]])
        kb.op("dve", lambda e: e.tensor_reduce(out=e1.t[R, :], in_=el.t[R, :], axis=AX.X, op=ALU.max), [el[:]], [e1[:]])
        kb.ts("dve", mk1.v(mk1.t[R, :]), el.v(el.t[R, :]), e1.v(e1.t[R, 0:1]), None, ALU.is_ge)
        kb.stt("dve", el2.v(el2.t[R, :]), mk1.v(mk1.t[R, :]), -1e30, el.v(el.t[R, :]), ALU.mult, ALU.add)
        kb.op("dve", lambda e: e.tensor_reduce(out=e2.t[R, :], in_=el2.t[R, :], axis=AX.X, op=ALU.max), [el2[:]], [e2[:]])
        kb.ts("dve", mk2.v(mk2.t[R, :]), el2.v(el2.t[R, :]), e2.v(e2.t[R, 0:1]), None, ALU.is_ge)
        kb.tt("dve", w2.v(w2.t[R, :]), e2.v(e2.t[R, :]), e1.v(e1.t[R, :]), ALU.subtract)
        kb.act(w2.v(w2.t[R, :]), w2.v(w2.t[R, :]), AF.Exp)
        kb.ts("dve", w1.v(w1.t[R, :]), w2.v(w2.t[R, :]), 1.0, None, ALU.add)
        kb.op("dve", lambda e: e.reciprocal(out=w1.t[R, :], in_=w1.t[R, :]), [w1[:]], [w1[:]])
        kb.tt("dve", w2.v(w2.t[R, :]), w2.v(w2.t[R, :]), w1.v(w1.t[R, :]), ALU.mult)
        kb.tt("dve", w1.v(w1.t[R, :]), w1.v(w1.t[R, :]), gsum.v(gsum.t[R, :]), ALU.mult)
        kb.tt("dve", w2.v(w2.t[R, :]), w2.v(w2.t[R, :]), gsum.v(gsum.t[R, :]), ALU.mult)
        kb.ts("dve", wv.v(wv.t[R, :]), mk1.v(mk1.t[R, :]), w1.v(w1.t[R, 0:1]), None, ALU.mult)
        kb.stt("dve", wv.v(wv.t[R, :]), mk2.v(mk2.t[R, :]), w2.v(w2.t[R, 0:1]), wv.v(wv.t[R, :]), ALU.mult, ALU.add)
        kb.tt("dve", comb.v(comb.t[R, ti, :].rearrange("p (g i) -> p g i", g=4)),
              goh.v(goh.t[R, :].unsqueeze(2).to_broadcast([rows, 4, 4])), wv.v(wv.t[R, :].unsqueeze(1).to_broadcast([rows, 4, 4])), ALU.mult)
    kb.phase_end()
    stage_gate(6)
    kb.phase_begin()
    wg2 = [kb.sb("wg%d" % i, [128, 16, 512], BF16) for i in range(2)]
    wu2 = [kb.sb("wu%d" % i, [128, 16, 512], BF16) for i in range(2)]
    wd = kb.sb("wd", [128, 4, D], BF16)
    hT = kb.sb("hT", [128, 4, NTOK], BF16)
    sgt = kb.sb("sgt", [128, 512], F32)
    for e_ in range(16):
        wg, wu = wg2[e_ % 2], wu2[e_ % 2]
        wgv = w_gate.t[e_].rearrange("(k p) n -> p k n", p=128)
        wuv = w_up.t[e_].rearrange("(k p) n -> p k n", p=128)
        wdv = w_down.t[e_].rearrange("(k p) n -> p k n", p=128)
        for k in range(16):
            kb.dma("pool", wg.v(wg.t[:, k, :]), w_gate.v(wgv[:, k, :]))
            kb.dma("pool", wu.v(wu.t[:, k, :]), w_up.v(wuv[:, k, :]))
        for k in range(4):
            kb.dma("pool", wd.v(wd.t[:, k, :]), w_down.v(wdv[:, k, :]))
        for fc in range(4):
            for (c0, n) in ((0, 512), (512, 512), (1024, 32)):
                pg, pu = psb[0 + (fc % 2) * 2], psb[1 + (fc % 2) * 2]
                for k in range(16):
                    kb.mm(pg.v(pg.t[:, 0:n]), wg.v(wg.t[:, k, fc * 128:(fc + 1) * 128]), hnT.v(hnT.t[:, k, c0:c0 + n]),
                          start=(k == 0), stop=(k == 15), signal=(k == 15))
                for k in range(16):
                    kb.mm(pu.v(pu.t[:, 0:n]), wu.v(wu.t[:, k, fc * 128:(fc + 1) * 128]), hnT.v(hnT.t[:, k, c0:c0 + n]),
                          start=(k == 0), stop=(k == 15), signal=(k == 15))
                kb.act(sgt.v(sgt.t[:, 0:n]), pg.v(pg.t[:, 0:n]), AF.Silu)
                kb.tt("dve", hT.v(hT.t[:, fc, c0:c0 + n]), sgt.v(sgt.t[:, 0:n]), pu.v(pu.t[:, 0:n]), ALU.mult)
        for ti, (t0, rows) in enumerate(TILES):
            for cb in range(4):
                po = psb[4 + cb % 4]
                for fc in range(4):
                    kb.mm(po.v(po.t[0:rows, :]), hT.v(hT.t[:, fc, t0:t0 + rows]), wd.v(wd.t[:, fc, cb * 512:(cb + 1) * 512]),
                          start=(fc == 0), stop=(fc == 3), signal=(fc == 3))
                dst = acc.v(acc.t[0:rows, ti, cb * 512:(cb + 1) * 512])
                kb.stt("dve", dst, po.v(po.t[0:rows, :]), comb.v(comb.t[0:rows, ti, e_:e_ + 1]), dst, ALU.mult, ALU.add)
    kb.phase_end()
    kb.phase_end()
    stage_gate(7)
    kb.phase_begin()
    gfin = kb.sb("gfin", [128, D], F32)
    kb.dma("sp", gfin[:], norm_final.v(norm_final.t[0:1, :].partition_broadcast(128)))
    yo = kb.sb("yo", [128, D], F32)
    junk2 = kb.sb("junk2", [128, D], BF16)
    for ti, (t0, rows) in enumerate(TILES):
        at = T(acc.t[:, ti, :], "acc_t2")
        at.buf = acc.buf
        kb.act(junk2.v(junk2.t[0:rows, :]), at.v(at.t[0:rows, :]), AF.Square, accum=ssq.v(ssq.t[0:rows, :]))
        kb.act(rstd.v(rstd.t[0:rows, :]), ssq.v(ssq.t[0:rows, :]), AF.Sqrt, bias=eps_t.v(eps_t.t[0:rows, :]), scale=1.0 / D)
        kb.op("dve", lambda e: e.reciprocal(out=rstd.t[0:rows, :], in_=rstd.t[0:rows, :]), [rstd[:]], [rstd[:]])
        kb.stt("dve", yo.v(yo.t[0:rows, :]), at.v(at.t[0:rows, :]), rstd.v(rstd.t[0:rows, 0:1]), gfin.v(gfin.t[0:rows, :]), ALU.mult, ALU.mult)
        kb.dma("sp", o_y.v(o_y.t[t0:t0 + rows, :]), yo.v(yo.t[0:rows, :]))
    kb.phase_end()
    kb.phase_end()


_CACHE = {}


def kernel(**inputs):
    g = lambda k: np.ascontiguousarray(inputs[k], dtype=np.float32)
    x_prompt = g("x_prompt")
    x_sample = g("x_sample")
    w_in = g("w_in")[0]
    if "nc" not in _CACHE:
        _CACHE["nc"] = build_program()
    nc = _CACHE["nc"]
    ident = np.eye(128, dtype=np.float32)
    chan = lambda v: v.reshape(8, 128).T
    rnn_par = np.zeros((128, 8, 8), np.float32)
    cw = g("conv_w")[0]
    for k in range(4):
        rnn_par[:, :, k] = chan(cw[k])
    rnn_par[:, :, 4] = chan(g("conv_b")[0])
    rnn_par[:, :, 5] = chan(g("lru_ba")[0])
    rnn_par[:, :, 6] = chan(g("lru_bx")[0])
    rnn_par[:, :, 7] = chan(g("lru_lambda")[0])

    def bd(w):
        o = np.zeros((128, 8, 128), np.float32)
        for m in range(8):
            o[0:64, m, 0:64] = w[2 * m]
            o[64:128, m, 64:128] = w[2 * m + 1]
        return o
    wa_bd, wx_bd = bd(g("lru_wa")[0]), bd(g("lru_wx")[0])
    wc = g("cmp_pool_w")[0]
    poolk = np.zeros((128, 4), np.float32)
    poolv = np.zeros((128, 252), np.float32)
    for t in range(128):
        poolk[t, t // 32] = wc[t % 32, 0]
        poolv[t, 124 + t // 32] = wc[t % 32, 1]
    ii = np.arange(128, dtype=np.float32)[:, None]
    cc = np.arange(640, dtype=np.float32)[None, :]
    distw = 512.0 + ii - cc
    negdw = np.minimum(-distw, 0.0).astype(np.float32)
    bandw = ((distw >= 0) & (distw <= 512)).astype(np.float32)
    w_router = np.ascontiguousarray(np.concatenate([g("w_router_group")[0], g("w_router_expert")[0]], axis=1))
    b_router = np.ascontiguousarray(np.concatenate([g("b_router_group")[0], g("b_router_expert")[0]], axis=0)[None, :])
    shared = {
        "w_in": w_in, "norm_mix": g("norm_mix"), "ident": ident,
        "rnn_par": rnn_par, "wa_bd": wa_bd, "wx_bd": wx_bd, "poolk": poolk, "poolv": poolv,
        "iota4096": np.arange(4096, dtype=np.float32)[None, :],
        "iota_ce": (np.arange(128, dtype=np.float32) * 32 + 31)[None, :],
        "iota_blk": np.arange(64, dtype=np.float32)[None, :],
        "negdw": negdw, "bandw": bandw,
        "w_out": g("w_out")[0], "norm_ffn": g("norm_ffn"), "norm_final": g("norm_final")[None, :],
        "w_router": w_router, "b_router": b_router,
        "w_gate": g("w_exp_gate")[0], "w_up": g("w_exp_up")[0], "w_down": g("w_exp_down")[0],
    }
    rows = np.arange(128)
    rb, rr, rt = rows // 32, (rows // 8) % 4, rows % 8
    sl = np.stack([2.0 ** (-(4 * gg + rr + 1)) for gg in range(2)], 1).astype(np.float64)
    jj = np.arange(512)
    cb_s = (-sl[:, :, None] * (16384 + rt[:, None, None] - (32 * jj[None, None, :] + 31))).astype(np.float32)
    cwi = np.arange(520)
    distw_s = np.where(cwi[None, :] < 512, 512 + rt[:, None] - cwi[None, :], rt[:, None] - (cwi[None, :] - 512))
    wm_s = ((distw_s >= 0) & (distw_s <= 512)).astype(np.float32)
    wb_s = (-sl[:, :, None] * np.maximum(distw_s, 0)[:, None, :]).astype(np.float32)
    nn = np.arange(8)
    nm_s = (nn[None, :] <= rt[:, None]).astype(np.float32)
    nb_s = (-sl[:, :, None] * np.maximum(rt[:, None] - nn[None, :], 0)[:, None, :]).astype(np.float32)
    rsum = ((rb[:, None] == rb[None, :]) & (rt[:, None] == rt[None, :])).astype(np.float32)
    forced_s = np.zeros((1, 257), np.float32)
    forced_s[0, [0, 255, 256]] = 1.0
    shared.update({
        "cache_c": np.asarray(inputs["cache_cmp_kv"], dtype=np.float32).reshape(5120 * 128, 512),
        "cache_s": np.asarray(inputs["cache_sel_kv"], dtype=np.float32).reshape(5120 * 128, 512),
        "pidx": np.arange(128, dtype=np.float32)[:, None],
        "cb_s": cb_s, "slope_rows": sl.astype(np.float32), "stok": (-sl * rt[:, None]).astype(np.float32),
        "wb_s": wb_s, "wm_s": wm_s, "nb_s": nb_s, "nm_s": nm_s, "rsum": rsum, "forced_s": forced_s,
        "iota512": np.arange(512, dtype=np.float32)[None, :],
    })
    page_table = np.asarray(inputs["page_table"], dtype=np.int32)
    in_maps = []
    for c in range(8):
        b, j = c // 4, c % 4
        tpos = (1024 * j + 128 * np.arange(8, dtype=np.float32)[None, :] + ii).astype(np.float32)
        m = dict(shared)
        m.update({
            "x_full": x_prompt[b],
            "x_own": np.ascontiguousarray(x_prompt[b, 1024 * j:1024 * (j + 1)]),
            "x_s": np.ascontiguousarray(x_sample[4 * c:4 * c + 4].reshape(32, D)),
            "onehot": np.tile(np.eye(4, dtype=np.float32)[j][None, :], (128, 1)),
            "st_h": np.ascontiguousarray(g("state_h")[0, 4 * c:4 * c + 4].reshape(4, 8, 128).transpose(2, 0, 1)),
            "st_conv": np.ascontiguousarray(g("state_conv")[0, 4 * c:4 * c + 4].reshape(4, 3, 8, 128).transpose(3, 0, 2, 1)),
            "cwin": np.ascontiguousarray(g("cache_win_kv")[0, 4 * c:4 * c + 4].reshape(4, 512, 512)),
            "tpos": tpos, "curb": np.floor(tpos / 64.0).astype(np.float32),
            "pt": np.ascontiguousarray(page_table[4 * c:4 * c + 4].reshape(1, 512)),
        })
        in_maps.append(m)
    res = run_bass_kernel_spmd(nc, in_maps, core_ids=list(range(8)))
    r = res.results
    kvp = np.stack([r[0]["o_kv_p"], r[4]["o_kv_p"]], 0)
    kvs = np.concatenate([r[c]["o_kv_s"] for c in range(8)], 0).reshape(32, 8, 1536)
    f = lambda a, i: np.ascontiguousarray(a[..., i * 512:(i + 1) * 512]).reshape(a.shape[:-1] + (2, 2, 128))[None]
    y_prompt = np.concatenate([r[c]["o_y"][0:1024] for c in range(8)], 0).reshape(2, 4096, 2048)
    y_sample = np.concatenate([r[c]["o_y"][1024:1056] for c in range(8)], 0).reshape(32, 8, 2048)
    new_cmp_p, new_sel_p = f(kvp, 0), f(kvp, 1)
    new_win_p = np.ascontiguousarray(f(kvp, 2)[:, :, -512:])
    new_cmp_s, new_sel_s = f(kvs, 0), f(kvs, 1)
    new_win_s = np.concatenate([r[c]["o_win_s"] for c in range(8)], 0).reshape(1, 32, 512, 2, 2, 128)
    new_conv_p = np.stack([r[4 * b]["o_conv_p"].transpose(2, 1, 0).reshape(3, 1024) for b in range(2)], 0)[None]
    new_conv_s = np.concatenate([r[c]["o_conv_s"].transpose(1, 3, 2, 0).reshape(4, 3, 1024) for c in range(8)], 0)[None]
    new_h_p = np.stack([r[4 * b]["o_h_p"].T.reshape(1024) for b in range(2)], 0)[None]
    new_h_s = np.concatenate([r[c]["o_h_s"].transpose(1, 2, 0).reshape(4, 1024) for c in range(8)], 0)[None]
    return (y_prompt, y_sample, new_cmp_p, new_cmp_s, new_sel_p, new_sel_s, new_win_p, new_win_s,
            new_conv_p, new_conv_s, new_h_p, new_h_s)
```
